# Optimizing a Trainium2 kernel written in Bass

```python
import jax, jax.numpy as jnp
from jax import lax
import numpy as np

D_MODEL = 1024
BATCH = 8
SEQ = 2048
DEPTH = 2

N_MIXERS = 2
N_A_LAYERS = (DEPTH + 1) // 2
N_B_LAYERS = DEPTH // 2

ATT_HEADS = 16
ATT_KV_HEADS = 4
HEAD_DIM = D_MODEL // ATT_HEADS
GROUP = ATT_HEADS // ATT_KV_HEADS
IDX_HEADS = 8
IDX_DIM = 64
TOPK_MAX = 256
Q_BLOCK = 128
ROPE_THETA = 10000.0
Q_COLS = ATT_HEADS * HEAD_DIM
KV_COLS = ATT_KV_HEADS * HEAD_DIM
IQ_COLS = IDX_HEADS * IDX_DIM
IK_COLS = IDX_DIM
IW_COLS = IDX_HEADS
IN_COLS = Q_COLS + 2 * KV_COLS + IQ_COLS + IK_COLS + IW_COLS

RWKV_HEAD = 64
RWKV_HEADS = D_MODEL // RWKV_HEAD
DECAY_LORA = 64
ICL_LORA = 64
GATE_LORA = 160
LNX_EPS = 64e-5

D_FF = 4 * D_MODEL
NORM_EPS = 1e-6

kernel_name = "dsa_rwkv7_interleaved_hybrid"


def rms_norm(x, g):
    xf = x.astype(jnp.float32)
    y = xf * lax.rsqrt(jnp.mean(xf * xf, axis=-1, keepdims=True) + NORM_EPS)
    return (y * g.astype(jnp.float32)).astype(x.dtype)


def rope_tables(length, dim):
    inv = 1.0 / (ROPE_THETA ** (jnp.arange(0, dim, 2, dtype=jnp.float32) / dim))
    ang = jnp.arange(length, dtype=jnp.float32)[:, None] * inv[None, :]
    return jnp.cos(ang), jnp.sin(ang)


def apply_rope(x, cos, sin):
    shape = (1, cos.shape[0]) + (1,) * (x.ndim - 3) + (cos.shape[1],)
    c = cos.reshape(shape)
    s = sin.reshape(shape)
    xf = x.astype(jnp.float32)
    x1, x2 = jnp.split(xf, 2, axis=-1)
    return jnp.concatenate([x1 * c - x2 * s, x2 * c + x1 * s], axis=-1).astype(x.dtype)


def dsa_mixer(h, w_in, w_o):
    B, L, _ = h.shape
    top_k = min(TOPK_MAX, L // 4)
    qb_len = min(Q_BLOCK, L)
    n_blk = L // qb_len
    proj = h @ w_in
    o0 = Q_COLS
    o1 = o0 + KV_COLS
    o2 = o1 + KV_COLS
    o3 = o2 + IQ_COLS
    o4 = o3 + IK_COLS
    q = proj[..., :o0].reshape(B, L, ATT_HEADS, HEAD_DIM)
    k = proj[..., o0:o1].reshape(B, L, ATT_KV_HEADS, HEAD_DIM)
    v = proj[..., o1:o2].reshape(B, L, ATT_KV_HEADS, HEAD_DIM)
    qi = proj[..., o2:o3].reshape(B, L, IDX_HEADS, IDX_DIM)
    ki = proj[..., o3:o4]
    wi = proj[..., o4:] * (IDX_HEADS ** -0.5 * IDX_DIM ** -0.5)
    cos, sin = rope_tables(L, HEAD_DIM)
    q = apply_rope(q, cos, sin)
    k = apply_rope(k, cos, sin)
    cos_i, sin_i = rope_tables(L, IDX_DIM)
    qi = apply_rope(qi, cos_i, sin_i)
    ki = apply_rope(ki, cos_i, sin_i)

    def to_blocks(a):
        return jnp.moveaxis(a.reshape((B, n_blk, qb_len) + a.shape[2:]), 1, 0)

    s_pos = jnp.arange(L, dtype=jnp.int32)

    def block_fn(args):
        q_blk, qi_blk, wi_blk, start = args
        t_pos = start + jnp.arange(qb_len, dtype=jnp.int32)
        rel = jax.nn.relu(jnp.einsum('bthd,bsd->bths', qi_blk, ki).astype(jnp.float32))
        score = jnp.einsum('bths,bth->bts', rel, wi_blk.astype(jnp.float32))
        causal = s_pos[None, :] <= t_pos[:, None]
        score = jnp.where(causal[None], score, -jnp.inf)
        _, sel = lax.top_k(score, top_k)
        valid = sel <= t_pos[None, :, None]
        k_sel = jax.vmap(lambda kb, ib: kb[ib])(k, sel)
        v_sel = jax.vmap(lambda vb, ib: vb[ib])(v, sel)
        qg = q_blk.reshape(B, qb_len, ATT_KV_HEADS, GROUP, HEAD_DIM)
        logits = jnp.einsum('btngd,btknd->btngk', qg, k_sel).astype(jnp.float32) * (HEAD_DIM ** -0.5)
        logits = jnp.where(valid[:, :, None, None, :], logits, -jnp.inf)
        p = jax.nn.softmax(logits, axis=-1).astype(v_sel.dtype)
        o = jnp.einsum('btngk,btknd->btngd', p, v_sel)
        return o.reshape(B, qb_len, ATT_HEADS * HEAD_DIM)

    starts = jnp.arange(n_blk, dtype=jnp.int32) * qb_len
    o = lax.map(block_fn, (to_blocks(q), to_blocks(qi), to_blocks(wi), starts))
    o = jnp.moveaxis(o, 0, 1).reshape(B, L, ATT_HEADS * HEAD_DIM)
    return o @ w_o


def token_shift(x):
    return jnp.pad(x, ((0, 0), (1, 0), (0, 0)))[:, :-1]


def rwkv7_mixer(h, mu, w_rkv, w0, w1, w2, a0, a1, a2, g1, g2, k_k, k_a, r_k, lnx_w, lnx_b, w_o):
    B, L, D = h.shape
    H, N = RWKV_HEADS, RWKV_HEAD
    f32 = jnp.float32
    xx = token_shift(h) - h
    xs = h[None] + xx[None] * mu[:, None, None, :]
    r, k, v = jnp.einsum('cbld,cde->cble', xs[:3], w_rkv)
    xw, xa, xg = xs[3], xs[4], xs[5]
    w_log = -jax.nn.softplus(-(w0 + jnp.tanh(xw @ w1) @ w2).astype(f32)) - 0.5
    decay = jnp.exp(-jnp.exp(w_log))
    a = jax.nn.sigmoid((a0 + (xa @ a1) @ a2).astype(f32))
    g = jax.nn.sigmoid(xg @ g1) @ g2
    kf = k.astype(f32)
    kk = (kf * k_k.astype(f32)).reshape(B, L, H, N)
    kk = kk * lax.rsqrt(jnp.maximum(jnp.sum(kk * kk, axis=-1, keepdims=True), 1e-24))
    kf = kf * (1.0 + (a - 1.0) * k_a.astype(f32))

    def heads(z):
        return z.reshape(B, L, H, N)

    rh, wh, kh, vh, ah = heads(r.astype(f32)), heads(decay), heads(kf), heads(v.astype(f32)), heads(a)
    bh = kk * ah

    def step(state, inp):
        r_t, w_t, k_t, v_t, kk_t, b_t = inp
        sa = jnp.einsum('bhvk,bhk->bhv', state, -kk_t)
        state = (state * w_t[:, :, None, :] + sa[..., None] * b_t[:, :, None, :]
                 + v_t[..., None] * k_t[:, :, None, :])
        y = jnp.einsum('bhvk,bhk->bhv', state, r_t)
        return state, y

    def tm(z):
        return jnp.moveaxis(z, 1, 0)

    state0 = jnp.zeros((B, H, N, N), f32)
    _, y = lax.scan(step, state0, (tm(rh), tm(wh), tm(kh), tm(vh), tm(kk), tm(bh)))
    y = jnp.moveaxis(y, 0, 1)
    mean = jnp.mean(y, axis=-1, keepdims=True)
    var = jnp.mean(jnp.square(y - mean), axis=-1, keepdims=True)
    y = ((y - mean) * lax.rsqrt(var + LNX_EPS) * lnx_w.astype(f32).reshape(H, N)
         + lnx_b.astype(f32).reshape(H, N))
    bonus = jnp.sum(rh * kh * r_k.astype(f32), axis=-1, keepdims=True) * vh
    out = (y + bonus).reshape(B, L, D).astype(h.dtype) * g
    return out @ w_o


def sqrelu_mlp(h, w_up, w_down):
    u = jax.nn.relu(h @ w_up)
    return (u * u) @ w_down


def setup_inputs(seed: int = 0) -> dict:
    key = jax.random.key(seed)
    ks = jax.random.split(key, 32)
    D = D_MODEL

    def nrm(k, shape, scale):
        return jax.random.normal(k, shape, jnp.float32) * scale

    nA, nB = N_A_LAYERS, N_B_LAYERS
    return {
        "x": nrm(ks[0], (BATCH, SEQ, D), 1.0),
        "mixer_norm": 1.0 + nrm(ks[1], (DEPTH, D), 0.02),
        "mlp_norm": 1.0 + nrm(ks[2], (DEPTH, D), 0.02),
        "mlp_w_up": nrm(ks[3], (DEPTH, D, D_FF), D ** -0.5),
        "mlp_w_down": nrm(ks[4], (DEPTH, D_FF, D), D_FF ** -0.5),
        "final_norm": 1.0 + nrm(ks[5], (D,), 0.02),
        "dsa_w_in": nrm(ks[6], (nA, D, IN_COLS), D ** -0.5),
        "dsa_w_o": nrm(ks[7], (nA, Q_COLS, D), Q_COLS ** -0.5),
        "rwkv_mu": jax.random.uniform(ks[8], (nB, 6, D), jnp.float32),
        "rwkv_w_rkv": nrm(ks[9], (nB, 3, D, D), D ** -0.5),
        "rwkv_w0": jnp.linspace(-6.0, -1.0, D, dtype=jnp.float32)[None, :] + nrm(ks[10], (nB, D), 0.1),
        "rwkv_w1": nrm(ks[11], (nB, D, DECAY_LORA), D ** -0.5),
        "rwkv_w2": nrm(ks[12], (nB, DECAY_LORA, D), 0.1 * DECAY_LORA ** -0.5),
        "rwkv_a0": nrm(ks[13], (nB, D), 0.1),
        "rwkv_a1": nrm(ks[14], (nB, D, ICL_LORA), D ** -0.5),
        "rwkv_a2": nrm(ks[15], (nB, ICL_LORA, D), 0.1 * ICL_LORA ** -0.5),
        "rwkv_g1": nrm(ks[16], (nB, D, GATE_LORA), D ** -0.5),
        "rwkv_g2": nrm(ks[17], (nB, GATE_LORA, D), GATE_LORA ** -0.5),
        "rwkv_k_k": 0.85 + nrm(ks[18], (nB, D), 0.02),
        "rwkv_k_a": 1.0 + nrm(ks[19], (nB, D), 0.02),
        "rwkv_r_k": -0.04 + nrm(ks[20], (nB, RWKV_HEADS, RWKV_HEAD), 0.02),
        "rwkv_lnx_w": 1.0 + nrm(ks[21], (nB, D), 0.02),
        "rwkv_lnx_b": nrm(ks[22], (nB, D), 0.02),
        "rwkv_w_o": nrm(ks[23], (nB, D, D), D ** -0.5),
    }


def reference(x, mixer_norm, mlp_norm, mlp_w_up, mlp_w_down, final_norm, dsa_w_in, dsa_w_o,
              rwkv_mu, rwkv_w_rkv, rwkv_w0, rwkv_w1, rwkv_w2, rwkv_a0, rwkv_a1, rwkv_a2,
              rwkv_g1, rwkv_g2, rwkv_k_k, rwkv_k_a, rwkv_r_k, rwkv_lnx_w, rwkv_lnx_b, rwkv_w_o):
    for i in range(DEPTH):
        h = rms_norm(x, mixer_norm[i])
        j = i // N_MIXERS
        if i % N_MIXERS == 0:
            x = x + dsa_mixer(h, dsa_w_in[j], dsa_w_o[j])
        else:
            x = x + rwkv7_mixer(h, rwkv_mu[j], rwkv_w_rkv[j], rwkv_w0[j], rwkv_w1[j], rwkv_w2[j],
                                rwkv_a0[j], rwkv_a1[j], rwkv_a2[j], rwkv_g1[j], rwkv_g2[j],
                                rwkv_k_k[j], rwkv_k_a[j], rwkv_r_k[j], rwkv_lnx_w[j],
                                rwkv_lnx_b[j], rwkv_w_o[j])
        x = x + sqrelu_mlp(rms_norm(x, mlp_norm[i]), mlp_w_up[i], mlp_w_down[i])
    return rms_norm(x, final_norm)
```

```python
import math
import numpy as np
from contextlib import ExitStack
import concourse.bass as bass
import concourse.mybir as mybir
from concourse.bass_utils import run_bass_kernel_spmd

F32 = mybir.dt.float32
BF16 = mybir.dt.bfloat16
I32 = mybir.dt.int32
AF = mybir.ActivationFunctionType
ALU = mybir.AluOpType
AX = mybir.AxisListType

D = 1024
DC = 8
DFF = 4096
NCORES = 8
HEADS = 16
KVH = 4
IDXH = 8
IN_COLS = 2120
NORM_EPS = 1e-6
LNX_EPS = 64e-5
NEG = -30000.0


class Buf:
    __slots__ = ("w", "r", "name", "excl")

    def __init__(self, name="", excl=False):
        self.w = None
        self.r = {}
        self.name = name
        self.excl = excl


class K:
    N_DMA_SEMS = 24

    def __init__(self, nc, es):
        self.nc = nc
        self.es = es
        self.eng = {"pe": nc.tensor, "act": nc.scalar, "dve": nc.vector,
                    "pool": nc.gpsimd, "sp": nc.sync}
        self.sem = {}
        self.cnt = {}
        self.seen = {}
        self.hist = {}
        for e in self.eng:
            self.sem[e] = es.enter_context(nc.semaphore("s_" + e))
            self.cnt[e] = 0
            self.seen[e] = {}
            self.hist[e] = {}
        self.dsem = {"hw": [], "sw": []}
        for kind in ("hw", "sw"):
            for i in range(self.N_DMA_SEMS // 2):
                key = "d%s%d" % (kind, i)
                self.sem[key] = es.enter_context(nc.semaphore("s_" + key))
                self.cnt[key] = 0
                self.hist[key] = {}
                self.dsem[kind].append(key)
        self.dnext = {"hw": 0, "sw": 0}
        self.dead = False
        self.uid = 0
        self.names = []
        self.n_instr = 0
        self.n_wait = 0
        self.per_eng = {e: 0 for e in self.eng}

    def sb(self, name, shape, dtype, es=None):
        self.uid += 1
        self.names.append("%s_%d" % (name, self.uid))
        return (es or self.es).enter_context(self.nc.sbuf_tensor("%s_%d" % (name, self.uid), list(shape), dtype))

    def ps(self, name, shape, dtype, es=None):
        self.uid += 1
        self.names.append("%s_%d" % (name, self.uid))
        return (es or self.es).enter_context(self.nc.psum_tensor("%s_%d" % (name, self.uid), list(shape), dtype))

    def _need(self, e, deps):
        eng = self.eng[e]
        seen = self.seen[e]
        for (sk, v) in deps:
            if seen.get(sk, 0) >= v:
                continue
            eng.wait_ge(self.sem[sk], v)
            self.n_wait += 1
            seen[sk] = v
            h = self.hist[sk].get(v)
            if h:
                for k2, v2 in h.items():
                    if seen.get(k2, 0) < v2:
                        seen[k2] = v2

    def _deps(self, e, reads, writes):
        deps = []
        for b in reads:
            if b.w is not None:
                if b.w[0] == e and e == "pe":
                    continue
                deps.append(b.w)
        for b in writes:
            if b.w is not None and b.w[0] != e:
                deps.append(b.w)
            for sk, v in b.r.items():
                if sk != e or e != "pe":
                    deps.append((sk, v))
        return deps

    def op(self, e, fn, reads=(), writes=()):
        if self.dead:
            return None
        ex = [b for b in reads if b.excl]
        if ex:
            reads = [b for b in reads if not b.excl]
            writes = list(writes) + ex
        self._need(e, self._deps(e, reads, writes))
        ins = fn()
        self.cnt[e] += 1
        c = self.cnt[e]
        ins.then_inc(self.sem[e], 1)
        self.hist[e][c] = dict(self.seen[e])
        for b in reads:
            if b.r.get(e, 0) < c:
                b.r[e] = c
        for b in writes:
            b.w = (e, c)
            b.r = {}
        self.n_instr += 1
        self.per_eng[e] += 1
        return ins

    def dma(self, e, out, in_, reads=(), writes=(), **kw):
        if self.dead:
            return None
        kind = "sw" if e == "pool" else "hw"
        sk = self.dsem[kind][self.dnext[kind]]
        self.dnext[kind] = (self.dnext[kind] + 1) % len(self.dsem[kind])
        deps = self._deps(sk, reads, writes)
        if self.cnt[sk] > 0:
            deps.append((sk, self.cnt[sk]))
        self._need(e, deps)
        ins = self.eng[e].dma_start(out=out, in_=in_, **kw)
        self.cnt[sk] += 16
        c = self.cnt[sk]
        ins.then_inc(self.sem[sk], 16)
        self.hist[sk][c] = dict(self.seen[e])
        for b in reads:
            if b.r.get(sk, 0) < c:
                b.r[sk] = c
        for b in writes:
            b.w = (sk, c)
            b.r = {}
        self.n_instr += 1
        return ins

    def barrier(self):
        if self.dead:
            return
        allk = [(sk, v) for sk, v in self.cnt.items() if v > 0]
        for e in self.eng:
            self._need(e, [d for d in allk if d[0] != e])

    def finish(self, bufs, e="sp"):
        self._need(e, [b.w for b in bufs if b.w is not None])


class Rot:
    def __init__(self, k, name, n, shape, dtype, es=None, psum=False):
        self.items = []
        for i in range(n):
            t = (k.ps if psum else k.sb)("%s%d" % (name, i), shape, dtype, es)
            self.items.append((t, Buf(name)))
        self.i = 0

    def next(self):
        it = self.items[self.i]
        self.i = (self.i + 1) % len(self.items)
        return it


class NS:
    pass


class StopBuild(Exception):
    pass


def mark(S, name):
    if getattr(S, "stop_at", None) == name:
        S.k.dead = True


def setup_common(k, nc, S, L):
    S.L = L
    S.NT = L // 128
    S.X = k.sb("X", [128, S.NT, D], F32)
    S.bX = [Buf("X%d" % i) for i in range(S.NT)]
    S.ident = k.sb("ident", [128, 128], BF16)
    S.bident = Buf("ident")
    k.op("pool", lambda: nc.gpsimd.memset(S.ident[:], 1.0), writes=[S.bident])
    k.op("pool", lambda: nc.gpsimd.affine_select(
        out=S.ident[:], in_=S.ident[:], pattern=[[-1, 128]], compare_op=ALU.is_equal,
        fill=0.0, base=0, channel_multiplier=1), reads=[S.bident], writes=[S.bident])
    S.pb = [k.ps("pb%d" % i, [128, 512], F32) for i in range(7)]
    S.bpb = [Buf("pb%d" % i, excl=True) for i in range(7)]
    S.pT = k.ps("pT", [128, 1024], BF16)
    S.bpT = Buf("pT", excl=True)
    S.ss = Rot(k, "ss", 4, [128, 1], F32)
    S.rs = Rot(k, "rs", 4, [128, 1], F32)
    S.junk = k.sb("junk", [128, 1024], BF16)
    S.bjunk = Buf("junk")
    S.hb = Rot(k, "hb", 1, [128, 1024], BF16)
    S.gbc = Rot(k, "gbc", 1, [128, D], F32)


def load_gbc(k, nc, S, g_ap):
    t, b = S.gbc.next()
    k.dma("sp", t[:], g_ap.partition_broadcast(128), writes=[b])
    return t, b


def rstd_tile(k, nc, S, i):
    ss, bss = S.ss.next()
    rs, brs = S.rs.next()
    k.op("act", lambda: nc.scalar.activation(out=S.junk[:], in_=S.X[:, i, :], func=AF.Square,
                                             accum_out=ss[:]),
         reads=[S.bX[i]], writes=[S.bjunk, bss])
    k.op("act", lambda: nc.scalar.activation(out=ss[:], in_=ss[:], func=AF.Sqrt,
                                             scale=1.0 / D, bias=NORM_EPS),
         reads=[bss], writes=[bss])
    k.op("dve", lambda: nc.vector.reciprocal(out=rs[:], in_=ss[:]), reads=[bss], writes=[brs])
    return rs, brs


def norm_to_hT(k, nc, S, i, gbc, bgbc, dst_ap, bdst, evac_eng="act"):
    rs, brs = rstd_tile(k, nc, S, i)
    hb, bhb = S.hb.next()
    k.op("dve", lambda: nc.vector.scalar_tensor_tensor(
        out=hb[:], in0=S.X[:, i, :], scalar=rs[:], in1=gbc[:], op0=ALU.mult, op1=ALU.mult),
        reads=[S.bX[i], brs, bgbc], writes=[bhb])
    for c in range(DC):
        k.op("pe", lambda: nc.tensor.transpose(out=S.pT[:, c * 128:(c + 1) * 128],
                                               in_=hb[:, c * 128:(c + 1) * 128],
                                               identity=S.ident[:]),
             reads=[bhb, S.bident], writes=[S.bpT])
    src = S.pT[:].rearrange("p (c t) -> p c t", c=DC)
    if evac_eng == "act":
        k.op("act", lambda: nc.scalar.copy(out=dst_ap, in_=src), reads=[S.bpT], writes=[bdst])
    else:
        k.op("dve", lambda: nc.vector.tensor_copy(out=dst_ap, in_=src), reads=[S.bpT],
             writes=[bdst])


def phase_mlp(k, nc, S, g_ap, wup_ap, wdn_ap):
    L, NT = S.L, S.NT
    NTB = L // 512
    FP = 512
    NFP = DFF // FP
    FCP = FP // 128
    with ExitStack() as es:
        hT = k.sb("mlp_hT", [128, DC, L], BF16, es)
        bhT = [Buf("hT%d" % i) for i in range(NT)]
        uT = [k.sb("mlp_uT%d" % s, [128, FCP, L], BF16, es) for s in range(2)]
        buT = [[Buf("uT") for _ in range(NTB)] for s in range(2)]
        wup = [k.sb("mlp_wup%d" % s, [128, DC, FP], BF16, es) for s in range(2)]
        bwup = [Buf("wup") for s in range(2)]
        wdn = [k.sb("mlp_wdn%d" % s, [128, FCP, D], BF16, es) for s in range(2)]
        bwdn = [Buf("wdn") for s in range(2)]
        rtmp = Rot(k, "mlp_rt", 2, [128, 512], F32, es)

        def load_w(fp):
            s = fp % 2
            k.dma("pool", wup[s][:], wup_ap[:, fp * FP:(fp + 1) * FP].rearrange(
                "(c p) n -> p c n", p=128), writes=[bwup[s]])
            k.dma("pool", wdn[s][:], wdn_ap[fp * FP:(fp + 1) * FP, :].rearrange(
                "(c p) n -> p c n", p=128), writes=[bwdn[s]])

        load_w(0)
        load_w(1)
        gbc, bgbc = load_gbc(k, nc, S, g_ap)
        for i in range(NT):
            norm_to_hT(k, nc, S, i, gbc, bgbc, hT[:, :, i * 128:(i + 1) * 128], bhT[i],
                       evac_eng=("act" if i % 2 == 0 else "dve"))

        upb = [0, 1]
        dnb = [2, 3, 4, 5]
        cnt = {"u": 0, "d": 0}

        def up(fp):
            s = fp % 2
            for fc in range(FCP):
                for tb in range(NTB):
                    b = upb[cnt["u"] % 2]
                    cnt["u"] += 1
                    for dc in range(DC):
                        k.op("pe", lambda: nc.tensor.matmul(
                            out=S.pb[b][:], lhsT=wup[s][:, dc, fc * 128:(fc + 1) * 128],
                            rhs=hT[:, dc, tb * 512:(tb + 1) * 512],
                            start=(dc == 0), stop=(dc == DC - 1)),
                            reads=[bwup[s]] + bhT[tb * 4:(tb + 1) * 4], writes=[S.bpb[b]])
                    rt, brt = rtmp.next()
                    k.op("act", lambda: nc.scalar.activation(out=rt[:], in_=S.pb[b][:],
                                                             func=AF.Relu),
                         reads=[S.bpb[b]], writes=[brt])
                    k.op("dve", lambda: nc.vector.tensor_tensor(
                        out=uT[s][:, fc, tb * 512:(tb + 1) * 512], in0=rt[:], in1=rt[:],
                        op=ALU.mult), reads=[brt], writes=[buT[s][tb]])

        def down(fp):
            s = fp % 2
            for i in range(NT):
                for half in range(2):
                    b = dnb[cnt["d"] % 4]
                    cnt["d"] += 1
                    for fc in range(FCP):
                        k.op("pe", lambda: nc.tensor.matmul(
                            out=S.pb[b][:], lhsT=uT[s][:, fc, i * 128:(i + 1) * 128],
                            rhs=wdn[s][:, fc, half * 512:(half + 1) * 512],
                            start=(fc == 0), stop=(fc == FCP - 1)),
                            reads=[bwdn[s], buT[s][i // 4]], writes=[S.bpb[b]])
                    xs = S.X[:, i, half * 512:(half + 1) * 512]
                    k.op("dve", lambda: nc.vector.tensor_tensor(out=xs, in0=S.pb[b][:], in1=xs,
                                                                op=ALU.add),
                         reads=[S.bpb[b], S.bX[i]], writes=[S.bX[i]])
            if fp + 2 < NFP:
                load_w(fp + 2)

        up(0)
        for fp in range(NFP):
            if fp + 1 < NFP:
                up(fp + 1)
            down(fp)
        k.barrier()


def phase_final(k, nc, S, g_ap, out_ap, bout):
    with ExitStack() as es:
        ot = Rot(k, "fin_o", 2, [128, D], F32, es)
        gbc, bgbc = load_gbc(k, nc, S, g_ap)
        for i in range(S.NT):
            rs, brs = rstd_tile(k, nc, S, i)
            o, bo = ot.next()
            k.op("dve", lambda: nc.vector.scalar_tensor_tensor(
                out=o[:], in0=S.X[:, i, :], scalar=rs[:], in1=gbc[:], op0=ALU.mult,
                op1=ALU.mult), reads=[S.bX[i], brs, bgbc], writes=[bo])
            k.dma("sp", out_ap[i * 128:(i + 1) * 128, :], o[:], reads=[bo], writes=[bout])
        k.finish([bout], "sp")
        k.barrier()


PARAM_SHAPES = {
    "mixer_norm": [2, D], "mlp_norm": [2, D], "mlp_w_up": [2, D, DFF], "mlp_w_down": [2, DFF, D],
    "final_norm": [D], "dsa_w_in": [1, D, IN_COLS], "dsa_w_o": [1, D, D],
    "rwkv_mu": [1, 6, D], "rwkv_w_rkv": [1, 3, D, D], "rwkv_w0": [1, D], "rwkv_w1": [1, D, 64],
    "rwkv_w2": [1, 64, D], "rwkv_a0": [1, D], "rwkv_a1": [1, D, 64], "rwkv_a2": [1, 64, D],
    "rwkv_g1": [1, D, 160], "rwkv_g2": [1, 160, D], "rwkv_k_k": [1, D], "rwkv_k_a": [1, D],
    "rwkv_r_k": [1, 16, 64], "rwkv_lnx_w": [1, D], "rwkv_lnx_b": [1, D], "rwkv_w_o": [1, D, D],
}


def dbg(k, S, name, ap, reads):
    if name in S.dbg_t:
        t = S.dbg_t[name]
        b = Buf("dbg")
        k.dma("sp", t, ap, reads=reads, writes=[b])
        S.dbg_b.append(b)


def build(L=2048, phases=("dsa", "mlp0", "rwkv", "mlp1", "final"), debug=None, stop_at=None):
    nc = bass.Bass("TRN2", target_bir_lowering=False)
    P = NS()
    P.x = nc.dram_tensor("x", [L, D], F32, kind="ExternalInput").ap()
    for name, shp in PARAM_SHAPES.items():
        setattr(P, name, nc.dram_tensor(name, shp, F32, kind="ExternalInput").ap())
    P.out = nc.dram_tensor("out", [L, D], F32, kind="ExternalOutput").ap()
    with ExitStack() as es:
        k = K(nc, es)
        global LAST_K
        LAST_K = k
        S = NS()
        S.P = P
        S.dbg_t = {}
        S.stop_at = stop_at
        S.k = k
        S.dbg_b = []
        for name, (shp, dt_) in (debug or {}).items():
            S.dbg_t[name] = nc.dram_tensor("dbg_" + name, list(shp), dt_, kind="ExternalOutput").ap()
        setup_common(k, nc, S, L)
        for i in range(S.NT):
            k.dma("sp", S.X[:, i, :], P.x[i * 128:(i + 1) * 128, :], writes=[S.bX[i]])
        for ph in phases:
            if ph == "dsa":
                phase_dsa(k, nc, S)
            elif ph == "mlp0":
                phase_mlp(k, nc, S, P.mlp_norm[0], P.mlp_w_up[0], P.mlp_w_down[0])
            elif ph == "rwkv":
                phase_rwkv(k, nc, S)
                k.dead = False
                k.barrier()
            elif ph == "mlp1":
                phase_mlp(k, nc, S, P.mlp_norm[1], P.mlp_w_up[1], P.mlp_w_down[1])
            elif ph == "final":
                bout = Buf("out")
                phase_final(k, nc, S, P.final_norm, P.out, bout)
                k.finish([bout], "sp")
            elif ph == "dump":
                bout = Buf("out")
                for i in range(S.NT):
                    k.dma("sp", P.out[i * 128:(i + 1) * 128, :], S.X[:, i, :],
                          reads=[S.bX[i]], writes=[bout])
                k.finish([bout], "sp")
        k.finish(S.dbg_b, "sp")
        print("instr", k.n_instr, "waits", k.n_wait, k.per_eng)
    return nc


def phase_dsa(k, nc, S):
    L, NT, P = S.L, S.NT, S.P
    TOPK = min(256, L // 4)
    NIT = 14
    w_in = P.dsa_w_in[0]
    w_o = P.dsa_w_o[0]
    o0, o1, o2, o3, o4 = 1024, 1280, 1536, 2048, 2112
    NHALF = 2 if L >= 1024 else 1
    LH = L // NHALF
    NTH = LH // 128
    NTBH = LH // 512
    with ExitStack() as es:
        qT = k.sb("qT", [128, 8, L], BF16, es)
        bqT = [Buf("qT%d" % i) for i in range(NT)]
        kT = k.sb("kT", [128, 2, L], BF16, es)
        bkT = [Buf("kT%d" % i) for i in range(NT)]
        qiT = k.sb("qiT", [128, 4, L], BF16, es)
        bqiT = [Buf("qiT%d" % i) for i in range(NT)]
        kiT = k.sb("kiT", [128, 1, L], BF16, es)
        bkiT = [Buf("kiT%d" % i) for i in range(NT)]
        Vaug = k.sb("Vaug", [128, NT, KVH, 65], BF16, es)
        bV = [Buf("V%d" % i) for i in range(NT)]
        wi = k.sb("wi", [128, NT, 8], F32, es)
        bwi = [Buf("wi%d" % i) for i in range(NT)]
        ident4 = k.sb("ident4", [128, 4, 128], BF16, es)
        bident4 = Buf("ident4")
        pw2 = k.sb("pw2", [128, NIT + 2], F32, es)
        bpw2 = Buf("pw2")
        for g in range(4):
            k.op("pool", lambda: nc.gpsimd.tensor_copy(out=ident4[:, g, :], in_=S.ident[:]),
                 reads=[S.bident], writes=[bident4])
        for n in range(NIT + 2):
            k.op("pool", lambda: nc.gpsimd.memset(pw2[:, n:n + 1], 2.0 ** (-(n + 1))),
                 writes=[bpw2])
        k.op("pool", lambda: nc.gpsimd.memset(Vaug[:, :, :, 64:65], 1.0), writes=bV)

        with ExitStack() as es2:
            cosT = k.sb("cosT", [128, LH], F32, es2)
            sinT = k.sb("sinT", [128, LH], F32, es2)
            btab = Buf("tab")
            bhT = [Buf("hT%d" % i) for i in range(NTH)]
            wn = [k.sb("dsa_wn%d" % s, [128, DC, 2, 2, 64], BF16, es2) for s in range(2)]
            wr = [k.sb("dsa_wr%d" % s, [128, DC, 2, 2, 2, 32], BF16, es2) for s in range(2)]
            bwn = [Buf("wn") for s in range(2)]
            bwr = [Buf("wr") for s in range(2)]
            wv = k.sb("dsa_wv", [128, DC, 264], BF16, es2)
            bwv = Buf("wv")
            t1r = Rot(k, "rope_t1", 2, [128, 512], F32, es2)
            t2r = Rot(k, "rope_t2", 2, [128, 512], F32, es2)
            k.dma("pool", wv[:, :, 0:256], w_in[:, o1:o2].rearrange("(c p) n -> p c n", p=128),
                  writes=[bwv])
            k.dma("pool", wv[:, :, 256:264], w_in[:, o4:o4 + 8].rearrange("(c p) n -> p c n", p=128),
                  writes=[bwv])
            gbc, bgbc = load_gbc(k, nc, S, P.mixer_norm[0])

            with ExitStack() as es3:
                pidx = k.sb("pidx", [128, 1], I32, es3)
                pj = k.sb("pj", [128, 1], I32, es3)
                pjf = k.sb("pjf", [128, 1], F32, es3)
                inv = k.sb("rinv", [128, 1], F32, es3)
                sgn = k.sb("rsgn", [128, 1], F32, es3)
                bc = Buf("ropec")
                k.op("pool", lambda: nc.gpsimd.iota(out=pidx[:], pattern=[[0, 1]], base=0,
                                                    channel_multiplier=1), writes=[bc])
                k.op("dve", lambda: nc.vector.tensor_single_scalar(out=pj[:], in_=pidx[:], scalar=31,
                                                                   op=ALU.bitwise_and),
                     reads=[bc], writes=[bc])
                k.op("dve", lambda: nc.vector.tensor_copy(out=pjf[:], in_=pj[:]), reads=[bc], writes=[bc])
                k.op("act", lambda: nc.scalar.activation(out=inv[:], in_=pjf[:], func=AF.Exp,
                                                         scale=-2.0 * math.log(10000.0) / 64.0),
                     reads=[bc], writes=[bc])
                k.op("dve", lambda: nc.vector.tensor_single_scalar(out=pj[:], in_=pidx[:], scalar=32,
                                                                   op=ALU.bitwise_and),
                     reads=[bc], writes=[bc])
                k.op("dve", lambda: nc.vector.tensor_copy(out=pjf[:], in_=pj[:]), reads=[bc], writes=[bc])
                k.op("dve", lambda: nc.vector.tensor_scalar(out=sgn[:], in0=pjf[:], scalar1=2.0 / 32.0,
                                                            scalar2=-1.0, op0=ALU.mult, op1=ALU.add),
                     reads=[bc], writes=[bc])

                def make_tables(hf):
                    with ExitStack() as es4:
                        ti = k.sb("tpos_i", [128, LH], I32, es4)
                        ang = k.sb("ang", [128, LH], F32, es4)
                        rr = k.sb("rr", [128, LH], F32, es4)
                        ki_ = ti
                        mm = k.sb("mm", [128, LH], F32, es4)
                        bt = Buf("tabtmp")
                        TWO_PI = 2.0 * math.pi
                        k.op("pool", lambda: nc.gpsimd.iota(out=ti[:], pattern=[[1, LH]], base=hf * LH,
                                                            channel_multiplier=0), writes=[bt])
                        k.op("dve", lambda: nc.vector.tensor_copy(out=ang[:], in_=ti[:]), reads=[bt], writes=[bt])
                        k.op("dve", lambda: nc.vector.tensor_scalar(out=ang[:], in0=ang[:], scalar1=inv[:],
                                                                    scalar2=None, op0=ALU.mult),
                             reads=[bt, bc], writes=[bt])
                        for which, dst in (("sin", sinT), ("cos", cosT)):
                            off = 0.0 if which == "sin" else math.pi / 2
                            k.op("dve", lambda: nc.vector.tensor_scalar(
                                out=ki_[:], in0=ang[:], scalar1=off, scalar2=1.0 / TWO_PI,
                                op0=ALU.add, op1=ALU.mult), reads=[bt], writes=[bt])
                            k.op("dve", lambda: nc.vector.tensor_copy(out=mm[:], in_=ki_[:]),
                                 reads=[bt], writes=[bt])
                            k.op("dve", lambda: nc.vector.scalar_tensor_tensor(
                                out=rr[:], in0=mm[:], scalar=-TWO_PI, in1=ang[:], op0=ALU.mult,
                                op1=ALU.add), reads=[bt], writes=[bt])
                            if off != 0.0:
                                k.op("dve", lambda: nc.vector.tensor_scalar(
                                    out=rr[:], in0=rr[:], scalar1=off, scalar2=None, op0=ALU.add),
                                    reads=[bt], writes=[bt])
                            k.op("dve", lambda: nc.vector.tensor_scalar(
                                out=mm[:], in0=rr[:], scalar1=math.pi, scalar2=-TWO_PI,
                                op0=ALU.is_gt, op1=ALU.mult), reads=[bt], writes=[bt])
                            k.op("dve", lambda: nc.vector.tensor_tensor(out=rr[:], in0=rr[:], in1=mm[:],
                                                                        op=ALU.add), reads=[bt], writes=[bt])
                            k.op("dve", lambda: nc.vector.tensor_scalar(
                                out=mm[:], in0=rr[:], scalar1=-math.pi, scalar2=TWO_PI,
                                op0=ALU.is_lt, op1=ALU.mult), reads=[bt], writes=[bt])
                            k.op("dve", lambda: nc.vector.tensor_tensor(out=rr[:], in0=rr[:], in1=mm[:],
                                                                        op=ALU.add), reads=[bt], writes=[bt])
                            k.op("dve", lambda: nc.vector.tensor_scalar(
                                out=rr[:], in0=rr[:], scalar1=-3.14159, scalar2=3.14159,
                                op0=ALU.max, op1=ALU.min), reads=[bt], writes=[bt])
                            k.op("act", lambda: nc.scalar.activation(out=dst[:], in_=rr[:], func=AF.Sin),
                                 reads=[bt], writes=[btab, bt])
                        k.op("dve", lambda: nc.vector.tensor_scalar(out=sinT[:], in0=sinT[:], scalar1=sgn[:],
                                                                    scalar2=None, op0=ALU.mult),
                             reads=[btab, bc], writes=[btab])
                        k.barrier()

                specs = []
                for c in range(8):
                    specs.append((qT, c, bqT, c * 64, (c + 8) * 64))
                for c in range(2):
                    specs.append((kT, c, bkT, o0 + c * 64, o0 + (c + 2) * 64))
                for c in range(4):
                    specs.append((qiT, c, bqiT, o2 + c * 64, o2 + (c + 4) * 64))
                specs.append((kiT, 0, bkiT, o3, o3))
                blocks = [specs[i:i + 2] for i in range(0, len(specs), 2)]

                def load_block(bi):
                    s = bi % 2
                    for cc, (_, _, _, lo, up) in enumerate(blocks[bi]):
                        for h, col in enumerate((lo, up)):
                            k.dma("pool", wn[s][:, :, cc, h, :],
                                  w_in[:, col:col + 64].rearrange("(c p) n -> p c n", p=128),
                                  writes=[bwn[s]])
                            for r in range(2):
                                k.dma("pool", wr[s][:, :, cc, h, r, :],
                                      w_in[:, col + (1 - r) * 32:col + (1 - r) * 32 + 32].rearrange(
                                          "(c p) n -> p c n", p=128), writes=[bwr[s]])

                cntp = {"a": 0}
                for hf in range(NHALF):
                    make_tables(hf)
                    es5 = ExitStack()
                    hT = k.sb("dsa_hT", [128, DC, LH], BF16, es5)
                    load_block(0)
                    load_block(1)
                    for it in range(NTH):
                        gi = hf * NTH + it
                        norm_to_hT(k, nc, S, gi, gbc, bgbc, hT[:, :, it * 128:(it + 1) * 128], bhT[it],
                                   evac_eng=("act" if it % 2 == 0 else "dve"))
                    for it in range(NTH):
                        gi = hf * NTH + it
                        for dc in range(DC):
                            k.op("pe", lambda: nc.tensor.matmul(
                                out=S.pb[4][:, 0:264], lhsT=hT[:, dc, it * 128:(it + 1) * 128],
                                rhs=wv[:, dc, :], start=(dc == 0), stop=(dc == DC - 1)),
                                reads=[bhT[it], bwv], writes=[S.bpb[4]])
                        k.op("act", lambda: nc.scalar.copy(
                            out=Vaug[:, gi, :, 0:64],
                            in_=S.pb[4][:, 0:256].rearrange("p (n e) -> p n e", n=KVH)),
                            reads=[S.bpb[4]], writes=[bV[gi]])
                        k.op("act", lambda: nc.scalar.mul(out=wi[:, gi, :], in_=S.pb[4][:, 256:264],
                                                          mul=(8.0 ** -0.5) * (64.0 ** -0.5)),
                             reads=[S.bpb[4]], writes=[bwi[gi]])
                    for bi, blk in enumerate(blocks):
                        s = bi % 2
                        for cc, (dst, dci, bdst, lo, up) in enumerate(blk):
                            for tb in range(NTBH):
                                ba = cntp["a"] % 2
                                bb = 2 + cntp["a"] % 2
                                cntp["a"] += 1
                                for dc in range(DC):
                                    k.op("pe", lambda: nc.tensor.matmul(
                                        out=S.pb[ba][:], lhsT=wn[s][:, dc, cc, :, :],
                                        rhs=hT[:, dc, tb * 512:(tb + 1) * 512],
                                        start=(dc == 0), stop=(dc == DC - 1)),
                                        reads=[bwn[s]] + bhT[tb * 4:(tb + 1) * 4], writes=[S.bpb[ba]])
                                for dc in range(DC):
                                    k.op("pe", lambda: nc.tensor.matmul(
                                        out=S.pb[bb][:], lhsT=wr[s][:, dc, cc, :, :, :],
                                        rhs=hT[:, dc, tb * 512:(tb + 1) * 512],
                                        start=(dc == 0), stop=(dc == DC - 1)),
                                        reads=[bwr[s]] + bhT[tb * 4:(tb + 1) * 4], writes=[S.bpb[bb]])
                                t1, bt1 = t1r.next()
                                t2, bt2 = t2r.next()
                                tsl = slice(tb * 512, (tb + 1) * 512)
                                k.op("dve", lambda: nc.vector.tensor_tensor(
                                    out=t1[:], in0=S.pb[ba][:], in1=cosT[:, tsl], op=ALU.mult),
                                    reads=[S.bpb[ba], btab], writes=[bt1])
                                k.op("dve", lambda: nc.vector.tensor_tensor(
                                    out=t2[:], in0=S.pb[bb][:], in1=sinT[:, tsl], op=ALU.mult),
                                    reads=[S.bpb[bb], btab], writes=[bt2])
                                g0 = hf * LH + tb * 512
                                gt = g0 // 128
                                k.op("pool", lambda: nc.gpsimd.tensor_tensor(
                                    out=dst[:, dci, g0:g0 + 512], in0=t1[:], in1=t2[:], op=ALU.add),
                                    reads=[bt1, bt2], writes=bdst[gt:gt + 4])
                        if bi + 2 < len(blocks):
                            load_block(bi + 2)
                    k.barrier()
                    es5.close()

        k.barrier()
        dbg(k, S, "qT", qT[:], bqT)
        dbg(k, S, "kT", kT[:], bkT)
        dbg(k, S, "qiT", qiT[:], bqiT)
        dbg(k, S, "kiT", kiT[:], bkiT)
        dbg(k, S, "Vaug", Vaug[:], bV)
        dbg(k, S, "wi", wi[:], bwi)
        with ExitStack() as es2:
            accR = Rot(k, "acc", 2, [128, L], F32, es2)
            amR = Rot(k, "amask", 2, [128, L], BF16, es2)
            junk2 = k.sb("junk2", [128, L], BF16, es2)
            bjunk2 = Buf("junk2")
            rlR = Rot(k, "rl", 2, [128, 512], F32, es2)
            ptR = Rot(k, "PT", 3, [128, 4, 128], BF16, es2)
            osR = Rot(k, "osb", 2, [128, D], BF16, es2)
            smR = Rot(k, "bis", 2, [128, 16], F32, es2)
            stR = Rot(k, "bstp", 2, [128, 2 * (NIT + 2)], F32, es2)
            recR = Rot(k, "rec", 2, [128, 4], F32, es2)
            cz = {"z": 0, "s": 0, "o": 0}
            for i in range(NT):
                W = (i + 1) * 128
                acc, bacc = accR.next()
                for h in range(IDXH):
                    c = h % 4
                    plo = 64 * (h // 4)
                    for w0 in range(0, W, 512):
                        w1 = min(W, w0 + 512)
                        zb = cz["z"] % 2
                        cz["z"] += 1
                        k.op("pe", lambda: nc.tensor.matmul(
                            out=S.pb[zb][:, 0:w1 - w0], lhsT=qiT[plo:plo + 64, c, i * 128:(i + 1) * 128],
                            rhs=kiT[plo:plo + 64, 0, w0:w1], start=True, stop=True),
                            reads=[bqiT[i]] + bkiT[w0 // 128:w1 // 128], writes=[S.bpb[zb]])
                        rl, brl = rlR.next()
                        k.op("act", lambda: nc.scalar.activation(out=rl[:, 0:w1 - w0],
                                                                 in_=S.pb[zb][:, 0:w1 - w0], func=AF.Relu),
                             reads=[S.bpb[zb]], writes=[brl])
                        if h == 0:
                            k.op("dve", lambda: nc.vector.tensor_scalar(
                                out=acc[:, w0:w1], in0=rl[:, 0:w1 - w0], scalar1=wi[:, i, 0:1],
                                scalar2=None, op0=ALU.mult), reads=[brl, bwi[i]], writes=[bacc])
                        else:
                            k.op("dve", lambda: nc.vector.scalar_tensor_tensor(
                                out=acc[:, w0:w1], in0=rl[:, 0:w1 - w0], scalar=wi[:, i, h:h + 1],
                                in1=acc[:, w0:w1], op0=ALU.mult, op1=ALU.add),
                                reads=[brl, bwi[i], bacc], writes=[bacc])
                k.op("pool", lambda: nc.gpsimd.affine_select(
                    out=acc[:, i * 128:W], in_=acc[:, i * 128:W], pattern=[[-1, 128]],
                    compare_op=ALU.is_ge, fill=-1e30, base=0, channel_multiplier=1),
                    reads=[bacc], writes=[bacc])
                am, bam = amR.next()
                sm, bsm = smR.next()
                if W <= TOPK:
                    k.op("dve", lambda: nc.vector.tensor_scalar(
                        out=am[:, 0:W], in0=acc[:, 0:W], scalar1=-1e29, scalar2=NEG,
                        op0=ALU.is_lt, op1=ALU.mult), reads=[bacc], writes=[bam])
                else:
                    st, bst = stR.next()
                    hi_, lo_, rng_, mid, cntv, tt, thr = [sm[:, j:j + 1] for j in range(7)]
                    k.op("dve", lambda: nc.vector.tensor_reduce(out=hi_, in_=acc[:, 0:W], axis=AX.X,
                                                                op=ALU.max), reads=[bacc], writes=[bsm])
                    k.op("dve", lambda: nc.vector.tensor_reduce(out=lo_, in_=acc[:, 0:i * 128], axis=AX.X,
                                                                op=ALU.min), reads=[bacc], writes=[bsm])
                    k.op("dve", lambda: nc.vector.tensor_tensor(out=rng_, in0=hi_, in1=lo_,
                                                                op=ALU.subtract), reads=[bsm], writes=[bsm])
                    k.op("dve", lambda: nc.vector.tensor_scalar(
                        out=st[:, 0:NIT + 2], in0=pw2[:, :], scalar1=rng_, scalar2=None, op0=ALU.mult),
                        reads=[bsm, bpw2], writes=[bst])
                    k.op("dve", lambda: nc.vector.tensor_scalar(
                        out=st[:, NIT + 2:2 * (NIT + 2)], in0=pw2[:, :], scalar1=rng_, scalar2=2.0,
                        op0=ALU.mult, op1=ALU.mult), reads=[bsm, bpw2], writes=[bst])
                    k.op("dve", lambda: nc.vector.tensor_tensor(out=mid, in0=lo_, in1=st[:, 0:1],
                                                                op=ALU.add), reads=[bsm, bst], writes=[bsm])
                    for n in range(NIT + 1):
                        k.op("dve", lambda: nc.vector.tensor_scalar(
                            out=junk2[:, 0:W], in0=acc[:, 0:W], scalar1=mid, scalar2=0.0,
                            op0=ALU.is_ge, op1=ALU.add, accum_out=cntv),
                            reads=[bacc, bsm], writes=[bjunk2, bsm])
                        if n < NIT:
                            k.op("dve", lambda: nc.vector.tensor_scalar(
                                out=tt, in0=cntv, scalar1=TOPK - 0.5, scalar2=st[:, NIT + 2 + n + 1:NIT + 2 + n + 2],
                                op0=ALU.is_ge, op1=ALU.mult), reads=[bsm, bst], writes=[bsm])
                            k.op("dve", lambda: nc.vector.scalar_tensor_tensor(
                                out=mid, in0=tt, scalar=st[:, n + 1:n + 2], in1=mid, op0=ALU.subtract,
                                op1=ALU.add), reads=[bsm, bst], writes=[bsm])
                        else:
                            k.op("dve", lambda: nc.vector.tensor_scalar(
                                out=tt, in0=cntv, scalar1=TOPK - 0.5, scalar2=st[:, n:n + 1],
                                op0=ALU.is_lt, op1=ALU.mult), reads=[bsm, bst], writes=[bsm])
                            k.op("dve", lambda: nc.vector.tensor_tensor(out=thr, in0=mid, in1=tt,
                                                                        op=ALU.subtract),
                                 reads=[bsm], writes=[bsm])
                    k.op("dve", lambda: nc.vector.tensor_scalar(
                        out=am[:, 0:W], in0=acc[:, 0:W], scalar1=thr, scalar2=NEG,
                        op0=ALU.is_lt, op1=ALU.mult), reads=[bacc, bsm], writes=[bam])
                if i == 1:
                    dbg(k, S, "acc1", acc[:], [bacc])
                    dbg(k, S, "am1", am[:], [bam])
                    dbg(k, S, "sm1", sm[:], [bsm])
                osb, bosb = osR.next()
                for n in range(KVH):
                    plo = 64 * (n // 2)
                    qc0 = 4 * (n % 2)
                    ob = 4 + cz["o"] % 2
                    cz["o"] += 1
                    for j in range(i + 1):
                        sbk = 2 + cz["s"] % 2
                        cz["s"] += 1
                        k.op("pe", lambda: nc.tensor.matmul(
                            out=S.pb[sbk][:].rearrange("p (g t) -> p g t", g=4),
                            lhsT=kT[plo:plo + 64, n % 2, j * 128:(j + 1) * 128],
                            rhs=qT[plo:plo + 64, qc0:qc0 + 4, i * 128:(i + 1) * 128],
                            start=True, stop=False),
                            reads=[bkT[j], bqT[i]], writes=[S.bpb[sbk]])
                        k.op("pe", lambda: nc.tensor.matmul(
                            out=S.pb[sbk][:].rearrange("p (g t) -> p g t", g=4),
                            lhsT=am[:, j * 128:(j + 1) * 128], rhs=ident4[:, :, :],
                            start=False, stop=True),
                            reads=[bam, bident4], writes=[S.bpb[sbk]])
                        pt, bpt = ptR.next()
                        k.op("act", lambda: nc.scalar.activation(
                            out=pt[:].rearrange("p g t -> p (g t)"), in_=S.pb[sbk][:], func=AF.Exp,
                            scale=0.125), reads=[S.bpb[sbk]], writes=[bpt])
                        for g in range(4):
                            k.op("pe", lambda: nc.tensor.matmul(
                                out=S.pb[ob][:, g * 65:(g + 1) * 65], lhsT=pt[:, g, :],
                                rhs=Vaug[:, j, n, :], start=(j == 0 and g == 0), stop=(j == i),
                                skip_group_check=True),
                                reads=[bpt, bV[j]], writes=[S.bpb[ob]])
                    rec, brec = recR.next()
                    ov = S.pb[ob][:, 0:260].rearrange("p (g e) -> p g e", g=4)
                    k.op("dve", lambda: nc.vector.reciprocal(out=rec[:].rearrange("p (g o) -> p g o", o=1),
                                                             in_=ov[:, :, 64:65]),
                         reads=[S.bpb[ob]], writes=[brec])
                    k.op("dve", lambda: nc.vector.tensor_tensor(
                        out=osb[:, n * 256:(n + 1) * 256].rearrange("p (g e) -> p g e", g=4),
                        in0=ov[:, :, 0:64],
                        in1=rec[:].rearrange("p (g o) -> p g o", o=1).to_broadcast([128, 4, 64]),
                        op=ALU.mult), reads=[S.bpb[ob], brec], writes=[bosb])
                if i == 1:
                    dbg(k, S, "osb1", osb[:], [bosb])
                for c in range(DC):
                    k.op("pe", lambda: nc.tensor.transpose(out=S.pT[:, c * 128:(c + 1) * 128],
                                                           in_=osb[:, c * 128:(c + 1) * 128],
                                                           identity=S.ident[:]),
                         reads=[bosb, S.bident], writes=[S.bpT])
                k.op("act", lambda: nc.scalar.copy(out=qT[:, :, i * 128:(i + 1) * 128],
                                                   in_=S.pT[:].rearrange("p (c t) -> p c t", c=DC)),
                     reads=[S.bpT], writes=[bqT[i]])

        k.barrier()
        with ExitStack() as es2:
            wo = k.sb("dsa_wo", [128, DC, D], BF16, es2)
            bwo = Buf("wo")
            for c in range(DC):
                k.dma("pool", wo[:, c, :], w_o[c * 128:(c + 1) * 128, :], writes=[bwo])
            cc = 0
            for i in range(NT):
                for half in range(2):
                    b = 2 + cc % 4
                    cc += 1
                    for c in range(DC):
                        k.op("pe", lambda: nc.tensor.matmul(
                            out=S.pb[b][:], lhsT=qT[:, c, i * 128:(i + 1) * 128],
                            rhs=wo[:, c, half * 512:(half + 1) * 512], start=(c == 0), stop=(c == DC - 1)),
                            reads=[bqT[i], bwo], writes=[S.bpb[b]])
                    xs = S.X[:, i, half * 512:(half + 1) * 512]
                    k.op("dve", lambda: nc.vector.tensor_tensor(out=xs, in0=S.pb[b][:], in1=xs, op=ALU.add),
                         reads=[S.bpb[b], S.bX[i]], writes=[S.bX[i]])
            k.barrier()


def phase_rwkv(k, nc, S):
    L, NT, P = S.L, S.NT, S.P
    G, CH, NHG, FC = 2, 512, 8, 4
    CDEC = math.exp(-0.5)
    mu = P.rwkv_mu[0]

    def v3(ap, a):
        return ap.rearrange("p (a b) -> p a b", a=a)

    def bc3(ap2, a, b):
        return ap2.unsqueeze(2).to_broadcast([128, a, b])

    with ExitStack() as es:
        triI = k.sb("triI", [128, 128], F32, es)
        triS = k.sb("triS", [128, 128], F32, es)
        negc = k.sb("negc", [128, 1], F32, es)
        selw = k.sb("selw", [2, 128], F32, es)
        sela = k.sb("sela", [2, 128], F32, es)
        rows = k.sb("rows", [2, CH], F32, es)
        maskL = k.sb("maskL", [128, 128], BF16, es)
        maskU = k.sb("maskU", [128, 128], BF16, es)
        maskUi = k.sb("maskUi", [128, 128], BF16, es)
        muT = k.sb("muT", [128, 6, DC], F32, es)
        bcv = k.sb("bcv", [128, 5, CH], BF16, es)
        wl1 = k.sb("wl1", [128, DC, 288], BF16, es)
        yo0 = k.sb("yo0", [128, NT, CH], BF16, es)
        byo0 = [Buf("yo0") for _ in range(NT)]
        bconst = Buf("rconst")
        k.op("pool", lambda: nc.gpsimd.memset(triI[:], -CDEC), writes=[bconst])
        k.op("pool", lambda: nc.gpsimd.affine_select(out=triI[:], in_=triI[:], pattern=[[1, 128]],
                                                     compare_op=ALU.is_ge, fill=0.0, base=0,
                                                     channel_multiplier=-1), reads=[bconst], writes=[bconst])
        k.op("pool", lambda: nc.gpsimd.memset(triS[:], -CDEC), writes=[bconst])
        k.op("pool", lambda: nc.gpsimd.affine_select(out=triS[:], in_=triS[:], pattern=[[1, 128]],
                                                     compare_op=ALU.is_gt, fill=0.0, base=0,
                                                     channel_multiplier=-1), reads=[bconst], writes=[bconst])
        k.op("pool", lambda: nc.gpsimd.memset(negc[:], -CDEC), writes=[bconst])
        for sel_, base_ in ((selw, 0), (sela, -1)):
            k.op("pool", lambda: nc.gpsimd.memset(sel_[:], 1.0), writes=[bconst])
            k.op("pool", lambda: nc.gpsimd.affine_select(out=sel_[:], in_=sel_[:], pattern=[[0, 128]],
                                                         compare_op=ALU.is_equal, fill=0.0, base=base_,
                                                         channel_multiplier=1), reads=[bconst], writes=[bconst])
        for m_, (cmp_, cm, pat) in ((maskL, (ALU.is_gt, 1, -1)), (maskU, (ALU.is_gt, -1, 1)),
                                    (maskUi, (ALU.is_ge, -1, 1))):
            k.op("pool", lambda: nc.gpsimd.memset(m_[:], 1.0), writes=[bconst])
            k.op("pool", lambda: nc.gpsimd.affine_select(out=m_[:], in_=m_[:], pattern=[[pat, 128]],
                                                         compare_op=cmp_, fill=0.0, base=0,
                                                         channel_multiplier=cm), reads=[bconst], writes=[bconst])
        NLEVS = 7
        lmask = k.sb("lmask", [128, NLEVS, 2, 128], BF16, es)
        with ExitStack() as esm:
            iI = k.sb("iI", [128, 128], I32, esm)
            iJ = k.sb("iJ", [128, 128], I32, esm)
            iT = k.sb("iT", [128, 128], I32, esm)
            eqm = k.sb("eqm", [128, 128], BF16, esm)
            bm = Buf("lmtmp")
            k.op("pool", lambda: nc.gpsimd.iota(out=iI[:], pattern=[[0, 128]], base=0, channel_multiplier=1), writes=[bm])
            k.op("pool", lambda: nc.gpsimd.iota(out=iJ[:], pattern=[[1, 128]], base=0, channel_multiplier=0), writes=[bm])
            k.op("dve", lambda: nc.vector.tensor_tensor(out=iI[:], in0=iI[:], in1=iJ[:], op=ALU.bitwise_xor),
                 reads=[bm], writes=[bm])
            for l in range(1, NLEVS + 1):
                k.op("dve", lambda: nc.vector.tensor_single_scalar(out=iT[:], in_=iI[:], scalar=l - 1,
                                                                   op=ALU.logical_shift_right), reads=[bm], writes=[bm])
                k.op("dve", lambda: nc.vector.tensor_single_scalar(out=eqm[:], in_=iT[:], scalar=1, op=ALU.is_equal),
                     reads=[bm], writes=[bm])
                k.op("dve", lambda: nc.vector.tensor_tensor(out=lmask[:, l - 1, 0, :], in0=eqm[:], in1=maskL[:], op=ALU.mult),
                     reads=[bm, bconst], writes=[bconst])
                k.op("dve", lambda: nc.vector.tensor_tensor(out=lmask[:, l - 1, 1, :], in0=eqm[:], in1=maskU[:], op=ALU.mult),
                     reads=[bm, bconst], writes=[bconst])
            k.barrier()
        with nc.allow_non_contiguous_dma(reason="tiny per-channel vectors"):
            for c in range(6):
                k.dma("sp", muT[:, c, :], mu[c].rearrange("(dc p) -> p dc", p=128), writes=[bconst])
        k.dma("pool", wl1[:, :, 0:64], P.rwkv_w1[0].rearrange("(c p) n -> p c n", p=128), writes=[bconst])
        k.dma("pool", wl1[:, :, 64:128], P.rwkv_a1[0].rearrange("(c p) n -> p c n", p=128), writes=[bconst])
        k.dma("pool", wl1[:, :, 128:288], P.rwkv_g1[0].rearrange("(c p) n -> p c n", p=128), writes=[bconst])
        gbc, bgbc = load_gbc(k, nc, S, P.mixer_norm[1])

        mark(S, "const")
        wr_ = k.sb("rw_wr", [128, DC, CH], BF16, es)
        wk_ = k.sb("rw_wk", [128, DC, CH], BF16, es)
        wv_ = k.sb("rw_wv", [128, DC, CH], BF16, es)
        wa2 = k.sb("rw_wa2", [64, CH], BF16, es)
        a2g_ = k.sb("rw_a2g", [64, CH], BF16, es)
        g2a = k.sb("rw_g2a", [128, CH], BF16, es)
        g2b = k.sb("rw_g2b", [32, CH], BF16, es)
        bw = Buf("rw_w")
        wo = k.sb("rw_wo", [128, DC, D], BF16, es)
        bwo = Buf("rw_wo")
        hc1 = k.sb("rw_hc", [128, DC, 129], BF16, es)
        hc = [hc1, hc1]
        bhc1 = Buf("hc")
        bhc = [bhc1, bhc1]
        dx = k.sb("rw_dx", [128, DC, 128], BF16, es)
        bdx = Buf("dx")
        xsR = Rot(k, "rw_xs", 2, [128, DC, 128], BF16, es)
        Fp = [k.sb("rw_F%d" % i, [128, CH], F32, es) for i in range(8)]
        bF = [Buf("F%d" % i) for i in range(8)]
        small = k.sb("rw_small", [128, 64], F32, es)
        bsmall = Buf("small")
        lT = k.sb("rw_lT", [128, 3, 128], BF16, es)
        g1b = k.sb("rw_g1b", [32, 128], BF16, es)
        blT = Buf("lT")
        Vb = k.sb("rw_V", [128, CH], BF16, es)
        bVb = Buf("V")
        tok4 = k.sb("rw_tok4", [128, 4, CH], BF16, es)
        btok = [Buf("tok%d" % i) for i in range(4)]
        KRT = k.sb("rw_KRT", [128, FC, 2, 128], BF16, es)
        KBT = k.sb("rw_KBT", [128, 2, FC, 128], BF16, es)
        bKRT = Buf("KRT")
        bKBT = Buf("KBT")
        Pm = [k.sb("rw_P%d" % i, [128, NHG, 128], BF16, es) for i in range(2)]
        PTm = [k.sb("rw_PT%d" % i, [128, NHG, 128], BF16, es) for i in range(2)]
        bPm = [Buf("P") for _ in range(2)]
        bPTm = [Buf("PT") for _ in range(2)]
        Ym = [k.sb("rw_Y%d" % i, [128, NHG, 128], BF16, es) for i in range(2)]
        bYm = [Buf("Y") for _ in range(2)]
        QT = PTm[1]
        bQT = bPTm[1]
        ArbT = k.sb("rw_ArbT", [128, NHG, 128], BF16, es)
        AkkT = k.sb("rw_AkkT", [128, NHG, 128], BF16, es)
        ArkT = k.sb("rw_ArkT", [128, NHG, 128], BF16, es)
        bArb, bAkk, bArk = Buf("Arb"), Buf("Akk"), Buf("Ark")
        Hf = k.sb("rw_Hf", [128, FC * 64], F32, es)
        Hb = k.sb("rw_Hb", [128, FC * 64], BF16, es)
        bHf, bHb = Buf("Hf"), Buf("Hb")
        pC = k.sb("rw_pC", [128, FC], F32, es)
        bpC = Buf("pC")
        Zs = k.sb("rw_Zs", [128, CH], BF16, es)
        Us = k.sb("rw_Us", [128, CH], BF16, es)
        bZs, bUs = Buf("Zs"), Buf("Us")
        yo = Zs
        byo = bZs
        pb, bpb = S.pb, S.bpb
        prot = {"i": 0}

        def pbank():
            b = prot["i"] % 3
            prot["i"] += 1
            return b

        for hg in range(G):
            c0 = hg * CH
            for wt, ci in ((wr_, 0), (wk_, 1), (wv_, 2)):
                k.dma("pool", wt[:], P.rwkv_w_rkv[0][ci][:, c0:c0 + CH].rearrange("(c p) n -> p c n", p=128),
                      writes=[bw])
            k.dma("pool", wa2[0:64, :], P.rwkv_w2[0][:, c0:c0 + CH], writes=[bw])
            k.dma("pool", a2g_[:], P.rwkv_a2[0][:, c0:c0 + CH], writes=[bw])
            k.dma("pool", g2a[:], P.rwkv_g2[0][0:128, c0:c0 + CH], writes=[bw])
            k.dma("pool", g2b[:], P.rwkv_g2[0][128:160, c0:c0 + CH], writes=[bw])
            k.dma("sp", rows[0:1, :], P.rwkv_w0[0:1, c0:c0 + CH], writes=[bw])
            k.dma("sp", rows[1:2, :], P.rwkv_a0[0:1, c0:c0 + CH], writes=[bw])
            for j, vec in enumerate((P.rwkv_k_k[0], P.rwkv_k_a[0], P.rwkv_r_k[0].rearrange("h n -> (h n)"),
                                     P.rwkv_lnx_w[0], P.rwkv_lnx_b[0])):
                k.dma("pool", bcv[:, j, :], vec[c0:c0 + CH].partition_broadcast(128), writes=[bw])
            if hg == G - 1:
                for c in range(DC):
                    k.dma("pool", wo[:, c, :], P.rwkv_w_o[0][c * 128:(c + 1) * 128, :], writes=[bwo])
            k.op("dve", lambda: nc.vector.memset(Hf[:], 0.0), writes=[bHf])
            k.op("dve", lambda: nc.vector.memset(Hb[:], 0.0), writes=[bHb])
            k.op("dve", lambda: nc.vector.memset(hc[0][:, :, 0:1], 0.0), writes=[bhc[0]])

            for n in range(NT):
                cur, nxt = n % 2, (n + 1) % 2
                h_ = hc[cur]
                norm_to_hT(k, nc, S, n, gbc, bgbc, h_[:, :, 1:129], bhc[cur],
                           evac_eng=("act" if n % 2 == 0 else "dve"))
                mark(S, "P0")
                k.op("dve", lambda: nc.vector.tensor_tensor(out=dx[:], in0=h_[:, :, 0:128], in1=h_[:, :, 1:129],
                                                            op=ALU.subtract), reads=[bhc[cur]], writes=[bdx])
                if n + 1 < NT:
                    k.op("dve", lambda: nc.vector.tensor_copy(out=h_[:, :, 0:1], in_=h_[:, :, 128:129]),
                         reads=[bhc[cur]], writes=[bhc[cur]])

                def mix(c):
                    xs, bxs = xsR.next()
                    k.op("dve", lambda: nc.vector.tensor_tensor(out=xs[:], in0=dx[:],
                                                                in1=bc3(muT[:, c, :], DC, 128), op=ALU.mult),
                         reads=[bdx, bconst], writes=[bxs])
                    k.op("pool", lambda: nc.gpsimd.tensor_tensor(out=xs[:], in0=xs[:], in1=h_[:, :, 1:129],
                                                                 op=ALU.add), reads=[bxs, bhc[cur]], writes=[bxs])
                    return xs, bxs

                def proj_tok(xs, bxs, wt, extra=None):
                    b = pbank()
                    nmm = DC + (1 if extra else 0)
                    for dc in range(DC):
                        k.op("pe", lambda: nc.tensor.matmul(out=pb[b][:], lhsT=xs[:, dc, :], rhs=wt[:, dc, :],
                                                            start=(dc == 0), stop=(dc == nmm - 1)),
                             reads=[bxs, bw], writes=[bpb[b]])
                    return b

                mark(S, "P1")
                R_, K_, SG, A_, PP, PINV, GG, TA = range(8)
                PPREV = PP
                xs, bxs = mix(0)
                b = proj_tok(xs, bxs, wr_)
                k.op("act", lambda: nc.scalar.copy(out=Fp[R_][:], in_=pb[b][:]), reads=[bpb[b]], writes=[bF[R_]])
                xs, bxs = mix(1)
                b = proj_tok(xs, bxs, wk_)
                k.op("act", lambda: nc.scalar.copy(out=Fp[K_][:], in_=pb[b][:]), reads=[bpb[b]], writes=[bF[K_]])
                xs, bxs = mix(2)
                b = proj_tok(xs, bxs, wv_)
                k.op("act", lambda: nc.scalar.copy(out=Vb[:], in_=pb[b][:]), reads=[bpb[b]], writes=[bVb])
                mark(S, "P2a")
                b = pbank()
                xs, bxs = mix(3)
                for dc in range(DC):
                    k.op("pe", lambda: nc.tensor.matmul(out=pb[b][0:64, 0:128], lhsT=wl1[:, dc, 0:64], rhs=xs[:, dc, :],
                                                        start=(dc == 0), stop=(dc == DC - 1), skip_group_check=True),
                         reads=[bxs, bconst], writes=[bpb[b]])
                xs, bxs = mix(4)
                for dc in range(DC):
                    k.op("pe", lambda: nc.tensor.matmul(out=pb[b][0:64, 384:512], lhsT=wl1[:, dc, 64:128], rhs=xs[:, dc, :],
                                                        start=False, stop=(dc == DC - 1), skip_group_check=True),
                         reads=[bxs, bconst], writes=[bpb[b]])
                xs, bxs = mix(5)
                for dc in range(DC):
                    k.op("pe", lambda: nc.tensor.matmul(out=pb[b][:, 128:256], lhsT=wl1[:, dc, 128:256], rhs=xs[:, dc, :],
                                                        start=(dc == 0), stop=(dc == DC - 1), skip_group_check=True),
                         reads=[bxs, bconst], writes=[bpb[b]])
                for dc in range(DC):
                    k.op("pe", lambda: nc.tensor.matmul(out=pb[b][0:32, 256:384], lhsT=wl1[:, dc, 256:288], rhs=xs[:, dc, :],
                                                        start=False, stop=(dc == DC - 1), skip_group_check=True),
                         reads=[bxs, bconst], writes=[bpb[b]])
                k.op("act", lambda: nc.scalar.activation(out=lT[0:64, 0, :], in_=pb[b][0:64, 0:128], func=AF.Tanh),
                     reads=[bpb[b]], writes=[blT])
                k.op("act", lambda: nc.scalar.copy(out=lT[0:64, 2, :], in_=pb[b][0:64, 384:512]),
                     reads=[bpb[b]], writes=[blT])
                k.op("act", lambda: nc.scalar.activation(out=lT[:, 1, :], in_=pb[b][:, 128:256], func=AF.Sigmoid),
                     reads=[bpb[b]], writes=[blT])
                k.op("act", lambda: nc.scalar.activation(out=g1b[:], in_=pb[b][0:32, 256:384], func=AF.Sigmoid),
                     reads=[bpb[b]], writes=[blT])
                mark(S, "P2b")
                b = pbank()
                k.op("pe", lambda: nc.tensor.matmul(out=pb[b][:], lhsT=lT[0:64, 0, :], rhs=wa2[0:64, :], start=True, stop=False),
                     reads=[blT, bw], writes=[bpb[b]])
                k.op("pe", lambda: nc.tensor.matmul(out=pb[b][:], lhsT=selw[:], rhs=rows[:, :],
                                                    start=False, stop=True), reads=[bconst, bw], writes=[bpb[b]])
                k.op("act", lambda: nc.scalar.activation(out=Fp[SG][:], in_=pb[b][:], func=AF.Sigmoid),
                     reads=[bpb[b]], writes=[bF[SG]])
                b = pbank()
                k.op("pe", lambda: nc.tensor.matmul(out=pb[b][:], lhsT=lT[0:64, 2, :], rhs=a2g_[:], start=True, stop=False),
                     reads=[blT, bw], writes=[bpb[b]])
                k.op("pe", lambda: nc.tensor.matmul(out=pb[b][:], lhsT=sela[:], rhs=rows[:, :],
                                                    start=False, stop=True), reads=[bconst, bw], writes=[bpb[b]])
                k.op("act", lambda: nc.scalar.activation(out=Fp[A_][:], in_=pb[b][:], func=AF.Sigmoid),
                     reads=[bpb[b]], writes=[bF[A_]])
                b = pbank()
                k.op("pe", lambda: nc.tensor.matmul(out=pb[b][:], lhsT=lT[:, 1, :], rhs=g2a[:], start=True, stop=False),
                     reads=[blT, bw], writes=[bpb[b]])
                k.op("pe", lambda: nc.tensor.matmul(out=pb[b][:], lhsT=g1b[:], rhs=g2b[:], start=False, stop=True),
                     reads=[blT, bw], writes=[bpb[b]])
                k.op("act", lambda: nc.scalar.copy(out=Fp[GG][:], in_=pb[b][:]), reads=[bpb[b]], writes=[bF[GG]])
                mark(S, "P2c")
                b = pbank()
                k.op("pe", lambda: nc.tensor.matmul(out=pb[b][:], lhsT=triI[:], rhs=Fp[SG][:], start=True, stop=True),
                     reads=[bconst, bF[SG]], writes=[bpb[b]])
                k.op("act", lambda: nc.scalar.activation(out=Fp[PP][:], in_=pb[b][:], func=AF.Exp),
                     reads=[bpb[b]], writes=[bF[PP]])
                k.op("act", lambda: nc.scalar.activation(out=Fp[PINV][:], in_=pb[b][:], func=AF.Exp, scale=-1.0),
                     reads=[bpb[b]], writes=[bF[PINV]])
                k.op("pool", lambda: nc.gpsimd.tensor_tensor(out=tok4[:, 1, :], in0=Fp[R_][:], in1=Fp[PP][:],
                                                             op=ALU.mult), reads=[bF[R_], bF[PP]], writes=[btok[1]])
                b = pbank()
                k.op("pe", lambda: nc.tensor.matmul(out=pb[b][:], lhsT=triS[:], rhs=Fp[SG][:], start=True, stop=True),
                     reads=[bconst, bF[SG]], writes=[bpb[b]])
                k.op("act", lambda: nc.scalar.activation(out=Fp[PPREV][:], in_=pb[b][:], func=AF.Exp),
                     reads=[bpb[b]], writes=[bF[PPREV]])
                b = pbank()
                for fc in range(FC):
                    k.op("pe", lambda: nc.tensor.matmul(out=pb[b][:, fc:fc + 1],
                                                        lhsT=Fp[SG][:, fc * 128:(fc + 1) * 128], rhs=negc[:],
                                                        start=(fc == 0), stop=True, skip_group_check=True),
                         reads=[bconst, bF[SG]], writes=[bpb[b]])
                k.op("act", lambda: nc.scalar.activation(out=pC[:], in_=pb[b][:, 0:FC], func=AF.Exp),
                     reads=[bpb[b]], writes=[bpC])
                mark(S, "P3")
                kkb = bcv[:, 0, :]
                kab = bcv[:, 1, :]
                rkb = bcv[:, 2, :]
                TB, TC = SG, TA + 0
                k.op("dve", lambda: nc.vector.tensor_tensor(out=Fp[TA][:], in0=Fp[K_][:], in1=kkb, op=ALU.mult),
                     reads=[bF[K_], bw], writes=[bF[TA]])
                k.op("pool", lambda: nc.gpsimd.tensor_tensor(out=Fp[TB][:], in0=Fp[TA][:], in1=Fp[TA][:], op=ALU.mult),
                     reads=[bF[TA]], writes=[bF[TB]])
                k.op("dve", lambda: nc.vector.tensor_reduce(out=small[:, 0:8], in_=v3(Fp[TB][:], NHG), axis=AX.X,
                                                            op=ALU.add), reads=[bF[TB]], writes=[bsmall])
                k.op("dve", lambda: nc.vector.tensor_scalar(out=small[:, 0:8], in0=small[:, 0:8], scalar1=1e-24,
                                                            scalar2=None, op0=ALU.max), reads=[bsmall], writes=[bsmall])
                k.op("act", lambda: nc.scalar.activation(out=small[:, 0:8], in_=small[:, 0:8], func=AF.Sqrt),
                     reads=[bsmall], writes=[bsmall])
                k.op("dve", lambda: nc.vector.reciprocal(out=small[:, 8:16], in_=small[:, 0:8]),
                     reads=[bsmall], writes=[bsmall])
                k.op("dve", lambda: nc.vector.tensor_tensor(out=v3(Fp[TA][:], NHG), in0=v3(Fp[TA][:], NHG),
                                                            in1=bc3(small[:, 8:16], NHG, 64), op=ALU.mult),
                     reads=[bF[TA], bsmall], writes=[bF[TA]])
                k.op("pool", lambda: nc.gpsimd.tensor_tensor(out=tok4[:, 0, :], in0=Fp[TA][:], in1=Fp[PPREV][:],
                                                             op=ALU.mult), reads=[bF[TA], bF[PPREV]], writes=[btok[0]])
                k.op("dve", lambda: nc.vector.scalar_tensor_tensor(out=Fp[TB][:], in0=Fp[TA][:], scalar=-1.0,
                                                                   in1=Fp[A_][:], op0=ALU.mult, op1=ALU.mult),
                     reads=[bF[TA], bF[A_]], writes=[bF[TB]])
                k.op("pool", lambda: nc.gpsimd.tensor_tensor(out=tok4[:, 3, :], in0=Fp[TB][:], in1=Fp[PINV][:],
                                                             op=ALU.mult), reads=[bF[TB], bF[PINV]], writes=[btok[3]])
                k.op("dve", lambda: nc.vector.scalar_tensor_tensor(out=Fp[TB][:], in0=Fp[A_][:], scalar=-1.0,
                                                                   in1=kab, op0=ALU.add, op1=ALU.mult),
                     reads=[bF[A_], bw], writes=[bF[TB]])
                k.op("dve", lambda: nc.vector.scalar_tensor_tensor(out=Fp[TB][:], in0=Fp[TB][:], scalar=1.0,
                                                                   in1=Fp[K_][:], op0=ALU.add, op1=ALU.mult),
                     reads=[bF[TB], bF[K_]], writes=[bF[TB]])
                k.op("pool", lambda: nc.gpsimd.tensor_tensor(out=tok4[:, 2, :], in0=Fp[TB][:], in1=Fp[PINV][:],
                                                             op=ALU.mult), reads=[bF[TB], bF[PINV]], writes=[btok[2]])
                k.op("dve", lambda: nc.vector.tensor_tensor(out=Fp[TA][:], in0=Fp[R_][:], in1=rkb, op=ALU.mult),
                     reads=[bF[R_], bw], writes=[bF[TA]])
                k.op("pool", lambda: nc.gpsimd.tensor_tensor(out=Fp[TA][:], in0=Fp[TA][:], in1=Fp[TB][:], op=ALU.mult),
                     reads=[bF[TA], bF[TB]], writes=[bF[TA]])
                k.op("dve", lambda: nc.vector.tensor_reduce(out=small[:, 16:24], in_=v3(Fp[TA][:], NHG), axis=AX.X,
                                                            op=ALU.add), reads=[bF[TA]], writes=[bsmall])
                mark(S, "P4")
                for j, (src_i, dst_ap) in enumerate(((0, 0), (1, 1))):
                    for fc in range(FC):
                        k.op("pe", lambda: nc.tensor.transpose(
                            out=S.pT[:, (fc * 2 + j) * 128:(fc * 2 + j + 1) * 128],
                            in_=tok4[:, src_i, fc * 128:(fc + 1) * 128], identity=S.ident[:]),
                            reads=[btok[src_i], S.bident], writes=[S.bpT])
                mark(S, "P5a")
                k.op("act", lambda: nc.scalar.copy(out=KRT[:].rearrange("p f j t -> p (f j t)"), in_=S.pT[:]),
                     reads=[S.bpT], writes=[bKRT])
                mark(S, "P5b")
                for j, src_i in enumerate((2, 3)):
                    for fc in range(FC):
                        k.op("pe", lambda: nc.tensor.transpose(
                            out=S.pT[:, (j * FC + fc) * 128:(j * FC + fc + 1) * 128],
                            in_=tok4[:, src_i, fc * 128:(fc + 1) * 128], identity=S.ident[:]),
                            reads=[btok[src_i], S.bident], writes=[S.bpT])
                mark(S, "P5c")
                k.op("act", lambda: nc.scalar.copy(out=KBT[:].rearrange("p j f t -> p (j f t)"), in_=S.pT[:]),
                     reads=[S.bpT], writes=[bKBT])
                mark(S, "P5")
                for h4 in range(2):
                    b = pbank()
                    for hh in range(4):
                        plo, fc = 64 * h4, hh
                        k.op("pe", lambda: nc.tensor.matmul(out=pb[b][:, hh * 128:(hh + 1) * 128],
                                                            lhsT=KRT[plo:plo + 64, fc, 0, :], rhs=KBT[plo:plo + 64, 1, fc, :],
                                                            start=True, stop=True, skip_group_check=True),
                             reads=[bKRT, bKBT], writes=[bpb[b]])
                    mark(S, "P6m")
                    k.op("dve", lambda: nc.vector.tensor_tensor(out=Pm[0][:, h4 * 4:(h4 + 1) * 4, :], in0=v3(pb[b][:], 4),
                                                                in1=maskL[:].unsqueeze(1).to_broadcast([128, 4, 128]),
                                                                op=ALU.mult), reads=[bpb[b], bconst], writes=[bPm[0]])
                mark(S, "P6a")
                for h2 in range(4):
                    b = pbank()
                    for hh in range(2):
                        s_ = h2 * 2 + hh
                        plo, fc = 64 * (s_ // 4), s_ % 4
                        k.op("pe", lambda: nc.tensor.matmul(out=pb[b][:, hh * 256:(hh + 1) * 256],
                                                            lhsT=KBT[plo:plo + 64, 1, fc, :], rhs=KRT[plo:plo + 64, fc, :, :],
                                                            start=True, stop=True, skip_group_check=True),
                             reads=[bKRT, bKBT], writes=[bpb[b]])
                    pv = pb[b][:].rearrange("p (h j t) -> p h j t", h=2, j=2)
                    k.op("dve", lambda: nc.vector.tensor_tensor(out=PTm[0][:, h2 * 2:(h2 + 1) * 2, :], in0=pv[:, :, 0, :],
                                                                in1=maskU[:].unsqueeze(1).to_broadcast([128, 2, 128]),
                                                                op=ALU.mult), reads=[bpb[b], bconst], writes=[bPTm[0]])
                    k.op("act", lambda: nc.scalar.copy(out=ArbT[:, h2 * 2:(h2 + 1) * 2, :], in_=pv[:, :, 1, :]),
                         reads=[bpb[b]], writes=[bArb])
                mark(S, "P6b")
                for h2 in range(4):
                    b = pbank()
                    for hh in range(2):
                        s_ = h2 * 2 + hh
                        plo, fc = 64 * (s_ // 4), s_ % 4
                        k.op("pe", lambda: nc.tensor.matmul(out=pb[b][:, hh * 256:(hh + 1) * 256],
                                                            lhsT=KBT[plo:plo + 64, 0, fc, :], rhs=KRT[plo:plo + 64, fc, :, :],
                                                            start=True, stop=True, skip_group_check=True),
                             reads=[bKRT, bKBT], writes=[bpb[b]])
                    pv = pb[b][:].rearrange("p (h j t) -> p h j t", h=2, j=2)
                    k.op("dve", lambda: nc.vector.tensor_tensor(out=AkkT[:, h2 * 2:(h2 + 1) * 2, :], in0=pv[:, :, 0, :],
                                                                in1=maskU[:].unsqueeze(1).to_broadcast([128, 2, 128]),
                                                                op=ALU.mult), reads=[bpb[b], bconst], writes=[bAkk])
                    k.op("act", lambda: nc.scalar.copy(out=ArkT[:, h2 * 2:(h2 + 1) * 2, :], in_=pv[:, :, 1, :]),
                         reads=[bpb[b]], writes=[bArk])
                mark(S, "P6c")
                k.op("pool", lambda: nc.gpsimd.tensor_tensor(out=ArbT[:], in0=ArbT[:],
                                                             in1=maskUi[:].unsqueeze(1).to_broadcast([128, NHG, 128]),
                                                             op=ALU.mult), reads=[bArb, bconst], writes=[bArb])
                k.op("pool", lambda: nc.gpsimd.tensor_tensor(out=ArkT[:], in0=ArkT[:],
                                                             in1=maskUi[:].unsqueeze(1).to_broadcast([128, NHG, 128]),
                                                             op=ALU.mult), reads=[bArk, bconst], writes=[bArk])
                mark(S, "P6")
                Dm, DTm, bD_, bDT_ = Pm[1], PTm[1], bPm[1], bPTm[1]
                k.op("pool", lambda: nc.gpsimd.tensor_tensor(out=Dm[:], in0=Pm[0][:],
                                                             in1=lmask[:, 0, 0, :].unsqueeze(1).to_broadcast([128, NHG, 128]),
                                                             op=ALU.mult), reads=[bPm[0], bconst], writes=[bD_])
                k.op("pool", lambda: nc.gpsimd.tensor_tensor(out=Dm[:], in0=Dm[:],
                                                             in1=S.ident[:].unsqueeze(1).to_broadcast([128, NHG, 128]),
                                                             op=ALU.add), reads=[bD_, S.bident], writes=[bD_])
                k.op("pool", lambda: nc.gpsimd.tensor_tensor(out=DTm[:], in0=PTm[0][:],
                                                             in1=lmask[:, 0, 1, :].unsqueeze(1).to_broadcast([128, NHG, 128]),
                                                             op=ALU.mult), reads=[bPTm[0], bconst], writes=[bDT_])
                k.op("pool", lambda: nc.gpsimd.tensor_tensor(out=DTm[:], in0=DTm[:],
                                                             in1=S.ident[:].unsqueeze(1).to_broadcast([128, NHG, 128]),
                                                             op=ALU.add), reads=[bDT_, S.bident], writes=[bDT_])
                for l in range(2, NLEVS + 1):
                    last = (l == NLEVS)
                    for h4 in range(2):
                        if not last:
                            b1 = pbank()
                            for hh in range(4):
                                h = h4 * 4 + hh
                                k.op("pe", lambda: nc.tensor.matmul(out=pb[b1][:, hh * 128:(hh + 1) * 128],
                                                                    lhsT=PTm[0][:, h, :], rhs=Dm[:, h, :],
                                                                    start=True, stop=True, skip_group_check=True),
                                     reads=[bPTm[0], bD_], writes=[bpb[b1]])
                            k.op("dve", lambda: nc.vector.tensor_tensor(
                                out=Ym[0][:, h4 * 4:(h4 + 1) * 4, :], in0=v3(pb[b1][:], 4),
                                in1=lmask[:, l - 1, 0, :].unsqueeze(1).to_broadcast([128, 4, 128]), op=ALU.mult),
                                reads=[bpb[b1], bconst], writes=[bYm[0]])
                        b2 = pbank()
                        for hh in range(4):
                            h = h4 * 4 + hh
                            k.op("pe", lambda: nc.tensor.matmul(out=pb[b2][:, hh * 128:(hh + 1) * 128],
                                                                lhsT=Pm[0][:, h, :], rhs=DTm[:, h, :],
                                                                start=True, stop=True, skip_group_check=True),
                                 reads=[bPm[0], bDT_], writes=[bpb[b2]])
                        k.op("dve", lambda: nc.vector.tensor_tensor(
                            out=Ym[1][:, h4 * 4:(h4 + 1) * 4, :], in0=v3(pb[b2][:], 4),
                            in1=lmask[:, l - 1, 1, :].unsqueeze(1).to_broadcast([128, 4, 128]), op=ALU.mult),
                            reads=[bpb[b2], bconst], writes=[bYm[1]])
                    zb = []
                    for h4 in range(2):
                        if not last:
                            b1 = pbank()
                            for hh in range(4):
                                h = h4 * 4 + hh
                                k.op("pe", lambda: nc.tensor.matmul(out=pb[b1][:, hh * 128:(hh + 1) * 128],
                                                                    lhsT=DTm[:, h, :], rhs=Ym[0][:, h, :],
                                                                    start=True, stop=True, skip_group_check=True),
                                     reads=[bDT_, bYm[0]], writes=[bpb[b1]])
                            zb.append((0, h4, b1))
                        b2 = pbank()
                        for hh in range(4):
                            h = h4 * 4 + hh
                            k.op("pe", lambda: nc.tensor.matmul(out=pb[b2][:, hh * 128:(hh + 1) * 128],
                                                                lhsT=Dm[:, h, :], rhs=Ym[1][:, h, :],
                                                                start=True, stop=True, skip_group_check=True),
                                 reads=[bD_, bYm[1]], writes=[bpb[b2]])
                        zb.append((1, h4, b2))
                        if len(zb) >= 2 or last:
                            for (which, hq, bb) in zb:
                                tgt, btgt = (Dm, bD_) if which == 0 else (DTm, bDT_)
                                k.op("dve", lambda: nc.vector.tensor_tensor(
                                    out=tgt[:, hq * 4:(hq + 1) * 4, :], in0=v3(pb[bb][:], 4),
                                    in1=tgt[:, hq * 4:(hq + 1) * 4, :], op=ALU.add),
                                    reads=[bpb[bb], btgt], writes=[btgt])
                            zb = []
                mark(S, "P7")
                bZ, bU, bD, bY = 3, 4, 5, 6
                for s_ in range(NHG):
                    plo, fc = 64 * (s_ // 4), s_ % 4
                    h = 2 * fc + s_ // 4
                    k.op("pe", lambda: nc.tensor.matmul(out=pb[bZ][:, h * 64:(h + 1) * 64], lhsT=KRT[plo:plo + 64, fc, 0, :],
                                                        rhs=Hb[plo:plo + 64, fc * 64:(fc + 1) * 64],
                                                        start=(s_ == 0), stop=False, skip_group_check=True),
                         reads=[bKRT, bHb], writes=[bpb[bZ]])
                    k.op("pe", lambda: nc.tensor.matmul(out=pb[bZ][:, h * 64:(h + 1) * 64], lhsT=AkkT[:, s_, :],
                                                        rhs=Vb[:, h * 64:(h + 1) * 64],
                                                        start=False, stop=True, skip_group_check=True),
                         reads=[bAkk, bVb], writes=[bpb[bZ]])
                k.op("act", lambda: nc.scalar.copy(out=Zs[:], in_=pb[bZ][:]), reads=[bpb[bZ]], writes=[bZs])
                for s_ in range(NHG):
                    h = 2 * (s_ % 4) + s_ // 4
                    k.op("pe", lambda: nc.tensor.matmul(out=pb[bU][:, h * 64:(h + 1) * 64], lhsT=QT[:, s_, :],
                                                        rhs=Zs[:, h * 64:(h + 1) * 64],
                                                        start=(s_ == 0), stop=True, skip_group_check=True),
                         reads=[bQT, bZs], writes=[bpb[bU]])
                k.op("act", lambda: nc.scalar.copy(out=Us[:], in_=pb[bU][:]), reads=[bpb[bU]], writes=[bUs])
                for s_ in range(NHG):
                    plo, fc = 64 * (s_ // 4), s_ % 4
                    h = 2 * fc + s_ // 4
                    k.op("pe", lambda: nc.tensor.matmul(out=pb[bY][:, h * 64:(h + 1) * 64], lhsT=KRT[plo:plo + 64, fc, 1, :],
                                                        rhs=Hb[plo:plo + 64, fc * 64:(fc + 1) * 64],
                                                        start=(s_ == 0), stop=False, skip_group_check=True),
                         reads=[bKRT, bHb], writes=[bpb[bY]])
                    k.op("pe", lambda: nc.tensor.matmul(out=pb[bY][:, h * 64:(h + 1) * 64], lhsT=ArkT[:, s_, :],
                                                        rhs=Vb[:, h * 64:(h + 1) * 64],
                                                        start=False, stop=False, skip_group_check=True),
                         reads=[bArk, bVb], writes=[bpb[bY]])
                    k.op("pe", lambda: nc.tensor.matmul(out=pb[bY][:, h * 64:(h + 1) * 64], lhsT=ArbT[:, s_, :],
                                                        rhs=Us[:, h * 64:(h + 1) * 64],
                                                        start=False, stop=True, skip_group_check=True),
                         reads=[bArb, bUs], writes=[bpb[bY]])
                for s_ in range(NHG):
                    plo, fc = 64 * (s_ // 4), s_ % 4
                    h = 2 * fc + s_ // 4
                    bDD = bD if s_ < 4 else bZ
                    k.op("pe", lambda: nc.tensor.matmul(out=pb[bDD][plo:plo + 64, fc * 64:(fc + 1) * 64],
                                                        lhsT=tok4[:, 2, h * 64:(h + 1) * 64], rhs=Vb[:, h * 64:(h + 1) * 64],
                                                        start=(s_ % 4 == 0), stop=False, skip_group_check=True),
                         reads=[btok[2], bVb], writes=[bpb[bDD]])
                    k.op("pe", lambda: nc.tensor.matmul(out=pb[bDD][plo:plo + 64, fc * 64:(fc + 1) * 64],
                                                        lhsT=tok4[:, 3, h * 64:(h + 1) * 64], rhs=Us[:, h * 64:(h + 1) * 64],
                                                        start=False, stop=True, skip_group_check=True),
                         reads=[btok[3], bUs], writes=[bpb[bDD]])
                k.op("dve", lambda: nc.vector.tensor_tensor(out=Hf[0:64, :], in0=pb[bD][0:64, 0:FC * 64], in1=Hf[0:64, :],
                                                            op=ALU.add), reads=[bpb[bD], bHf], writes=[bHf])
                k.op("dve", lambda: nc.vector.tensor_tensor(out=Hf[64:128, :], in0=pb[bZ][64:128, 0:FC * 64],
                                                            in1=Hf[64:128, :], op=ALU.add),
                     reads=[bpb[bZ], bHf], writes=[bHf])
                k.op("dve", lambda: nc.vector.tensor_tensor(out=v3(Hf[:], FC), in0=v3(Hf[:], FC), in1=bc3(pC[:], FC, 64),
                                                            op=ALU.mult), reads=[bHf, bpC], writes=[bHf])
                k.op("act", lambda: nc.scalar.copy(out=Hb[:], in_=Hf[:]), reads=[bHf], writes=[bHb])
                mark(S, "scan")
                YS, YQ = PP, PINV
                k.op("act", lambda: nc.scalar.copy(out=Fp[YS][:], in_=pb[bY][:]), reads=[bpb[bY]], writes=[bF[YS]])
                k.op("dve", lambda: nc.vector.tensor_reduce(out=small[:, 24:32], in_=v3(Fp[YS][:], NHG), axis=AX.X,
                                                            op=ALU.add), reads=[bF[YS]], writes=[bsmall])
                k.op("pool", lambda: nc.gpsimd.tensor_tensor(out=Fp[YQ][:], in0=Fp[YS][:], in1=Fp[YS][:], op=ALU.mult),
                     reads=[bF[YS]], writes=[bF[YQ]])
                k.op("dve", lambda: nc.vector.tensor_reduce(out=small[:, 32:40], in_=v3(Fp[YQ][:], NHG), axis=AX.X,
                                                            op=ALU.add), reads=[bF[YQ]], writes=[bsmall])
                k.op("dve", lambda: nc.vector.tensor_scalar(out=small[:, 24:32], in0=small[:, 24:32], scalar1=1.0 / 64,
                                                            scalar2=None, op0=ALU.mult), reads=[bsmall], writes=[bsmall])
                k.op("dve", lambda: nc.vector.tensor_tensor(out=small[:, 40:48], in0=small[:, 24:32], in1=small[:, 24:32],
                                                            op=ALU.mult), reads=[bsmall], writes=[bsmall])
                k.op("dve", lambda: nc.vector.scalar_tensor_tensor(out=small[:, 32:40], in0=small[:, 32:40], scalar=1.0 / 64,
                                                                   in1=small[:, 40:48], op0=ALU.mult, op1=ALU.subtract),
                     reads=[bsmall], writes=[bsmall])
                k.op("act", lambda: nc.scalar.activation(out=small[:, 32:40], in_=small[:, 32:40], func=AF.Sqrt,
                                                         bias=LNX_EPS, scale=1.0), reads=[bsmall], writes=[bsmall])
                k.op("dve", lambda: nc.vector.reciprocal(out=small[:, 40:48], in_=small[:, 32:40]),
                     reads=[bsmall], writes=[bsmall])
                k.op("dve", lambda: nc.vector.tensor_tensor(out=v3(Fp[YS][:], NHG), in0=v3(Fp[YS][:], NHG),
                                                            in1=bc3(small[:, 24:32], NHG, 64), op=ALU.subtract),
                     reads=[bF[YS], bsmall], writes=[bF[YS]])
                k.op("dve", lambda: nc.vector.tensor_tensor(out=v3(Fp[YS][:], NHG), in0=v3(Fp[YS][:], NHG),
                                                            in1=bc3(small[:, 40:48], NHG, 64), op=ALU.mult),
                     reads=[bF[YS], bsmall], writes=[bF[YS]])
                k.op("pool", lambda: nc.gpsimd.tensor_tensor(out=Fp[YS][:], in0=Fp[YS][:], in1=bcv[:, 3, :],
                                                             op=ALU.mult), reads=[bF[YS], bw], writes=[bF[YS]])
                k.op("pool", lambda: nc.gpsimd.tensor_tensor(out=Fp[YS][:], in0=Fp[YS][:], in1=bcv[:, 4, :],
                                                             op=ALU.add), reads=[bF[YS], bw], writes=[bF[YS]])
                k.op("dve", lambda: nc.vector.tensor_tensor(out=v3(Fp[YQ][:], NHG), in0=v3(Vb[:], NHG),
                                                            in1=bc3(small[:, 16:24], NHG, 64), op=ALU.mult),
                     reads=[bVb, bsmall], writes=[bF[YQ]])
                k.op("pool", lambda: nc.gpsimd.tensor_tensor(out=Fp[YS][:], in0=Fp[YS][:], in1=Fp[YQ][:], op=ALU.add),
                     reads=[bF[YS], bF[YQ]], writes=[bF[YS]])
                if hg < G - 1:
                    k.op("dve", lambda: nc.vector.tensor_tensor(out=yo0[:, n, :], in0=Fp[YS][:], in1=Fp[GG][:], op=ALU.mult),
                         reads=[bF[YS], bF[GG]], writes=[byo0[n]])
                else:
                    k.op("dve", lambda: nc.vector.tensor_tensor(out=yo[:], in0=Fp[YS][:], in1=Fp[GG][:], op=ALU.mult),
                         reads=[bF[YS], bF[GG]], writes=[byo])
                    for fc in range(FC):
                        k.op("pe", lambda: nc.tensor.transpose(out=S.pT[:, fc * 128:(fc + 1) * 128],
                                                               in_=yo0[:, n, fc * 128:(fc + 1) * 128], identity=S.ident[:]),
                             reads=[byo0[n], S.bident], writes=[S.bpT])
                    for fc in range(FC):
                        k.op("pe", lambda: nc.tensor.transpose(out=S.pT[:, (FC + fc) * 128:(FC + fc + 1) * 128],
                                                               in_=yo[:, fc * 128:(fc + 1) * 128], identity=S.ident[:]),
                             reads=[byo, S.bident], writes=[S.bpT])
                    yoT = KBT[:].rearrange("p j f t -> p (j f) t")
                    byoT = bKBT
                    k.op("act", lambda: nc.scalar.copy(out=KBT[:].rearrange("p j f t -> p (j f t)"), in_=S.pT[:]),
                         reads=[S.bpT], writes=[byoT])
                    for half in range(2):
                        b = pbank()
                        for c in range(DC):
                            k.op("pe", lambda: nc.tensor.matmul(out=pb[b][:], lhsT=yoT[:, c, :],
                                                                rhs=wo[:, c, half * 512:(half + 1) * 512],
                                                                start=(c == 0), stop=(c == DC - 1)),
                                 reads=[byoT, bwo], writes=[bpb[b]])
                        xsl = S.X[:, n, half * 512:(half + 1) * 512]
                        k.op("dve", lambda: nc.vector.tensor_tensor(out=xsl, in0=pb[b][:], in1=xsl, op=ALU.add),
                             reads=[bpb[b], S.bX[n]], writes=[S.bX[n]])
        k.barrier()


LAST_K = None
_NC_CACHE = {}


def kernel(**inputs):
    x = np.asarray(inputs["x"], dtype=np.float32)
    B, L, _ = x.shape
    if L not in _NC_CACHE:
        _NC_CACHE[L] = build(L)
    nc = _NC_CACHE[L]
    params = {n: np.ascontiguousarray(np.asarray(inputs[n], dtype=np.float32))
              for n in PARAM_SHAPES}
    in_maps = []
    for b in range(B):
        m = {"x": np.ascontiguousarray(x[b])}
        m.update(params)
        in_maps.append(m)
    res = run_bass_kernel_spmd(nc, in_maps, core_ids=list(range(B)))
    return np.stack([res.results[b]["out"] for b in range(B)], axis=0)
```

```python
import math
import numpy as np
from contextlib import ExitStack
import concourse.bass as bass
import concourse.mybir as mybir
from concourse.bass_utils import run_bass_kernel_spmd

F32 = mybir.dt.float32
BF16 = mybir.dt.bfloat16
I32 = mybir.dt.int32
AF = mybir.ActivationFunctionType
ALU = mybir.AluOpType
AX = mybir.AxisListType

D = 1024
DC = 8
DFF = 4096
NCORES = 8
HEADS = 16
KVH = 4
IDXH = 8
IN_COLS = 2120
NORM_EPS = 1e-6
LNX_EPS = 64e-5
NEG = -30000.0


class Buf:
    __slots__ = ("w", "r", "name", "excl")

    def __init__(self, name="", excl=False):
        self.w = None
        self.r = {}
        self.name = name
        self.excl = excl


class K:
    N_DMA_SEMS = 24

    def __init__(self, nc, es):
        self.nc = nc
        self.es = es
        self.eng = {"pe": nc.tensor, "act": nc.scalar, "dve": nc.vector,
                    "pool": nc.gpsimd, "sp": nc.sync}
        self.sem = {}
        self.cnt = {}
        self.seen = {}
        self.hist = {}
        for e in self.eng:
            self.sem[e] = es.enter_context(nc.semaphore("s_" + e))
            self.cnt[e] = 0
            self.seen[e] = {}
            self.hist[e] = {}
        self.dsem = {"hw": [], "sw": []}
        for kind in ("hw", "sw"):
            for i in range(self.N_DMA_SEMS // 2):
                key = "d%s%d" % (kind, i)
                self.sem[key] = es.enter_context(nc.semaphore("s_" + key))
                self.cnt[key] = 0
                self.hist[key] = {}
                self.dsem[kind].append(key)
        self.dnext = {"hw": 0, "sw": 0}
        self.dead = False
        self.uid = 0
        self.names = []
        self.n_instr = 0
        self.n_wait = 0
        self.per_eng = {e: 0 for e in self.eng}

    def sb(self, name, shape, dtype, es=None):
        self.uid += 1
        self.names.append("%s_%d" % (name, self.uid))
        return (es or self.es).enter_context(self.nc.sbuf_tensor("%s_%d" % (name, self.uid), list(shape), dtype))

    def ps(self, name, shape, dtype, es=None):
        self.uid += 1
        self.names.append("%s_%d" % (name, self.uid))
        return (es or self.es).enter_context(self.nc.psum_tensor("%s_%d" % (name, self.uid), list(shape), dtype))

    def _need(self, e, deps):
        eng = self.eng[e]
        seen = self.seen[e]
        for (sk, v) in deps:
            if seen.get(sk, 0) >= v:
                continue
            eng.wait_ge(self.sem[sk], v)
            self.n_wait += 1
            seen[sk] = v
            h = self.hist[sk].get(v)
            if h:
                for k2, v2 in h.items():
                    if seen.get(k2, 0) < v2:
                        seen[k2] = v2

    def _deps(self, e, reads, writes):
        deps = []
        for b in reads:
            if b.w is not None:
                if b.w[0] == e and e == "pe":
                    continue
                deps.append(b.w)
        for b in writes:
            if b.w is not None and b.w[0] != e:
                deps.append(b.w)
            for sk, v in b.r.items():
                if sk != e or e != "pe":
                    deps.append((sk, v))
        return deps

    def op(self, e, fn, reads=(), writes=()):
        if self.dead:
            return None
        ex = [b for b in reads if b.excl]
        if ex:
            reads = [b for b in reads if not b.excl]
            writes = list(writes) + ex
        self._need(e, self._deps(e, reads, writes))
        ins = fn()
        self.cnt[e] += 1
        c = self.cnt[e]
        ins.then_inc(self.sem[e], 1)
        self.hist[e][c] = dict(self.seen[e])
        for b in reads:
            if b.r.get(e, 0) < c:
                b.r[e] = c
        for b in writes:
            b.w = (e, c)
            b.r = {}
        self.n_instr += 1
        self.per_eng[e] += 1
        return ins

    def dma(self, e, out, in_, reads=(), writes=(), **kw):
        if self.dead:
            return None
        kind = "sw" if e == "pool" else "hw"
        sk = self.dsem[kind][self.dnext[kind]]
        self.dnext[kind] = (self.dnext[kind] + 1) % len(self.dsem[kind])
        deps = self._deps(sk, reads, writes)
        if self.cnt[sk] > 0:
            deps.append((sk, self.cnt[sk]))
        self._need(e, deps)
        ins = self.eng[e].dma_start(out=out, in_=in_, **kw)
        self.cnt[sk] += 16
        c = self.cnt[sk]
        ins.then_inc(self.sem[sk], 16)
        self.hist[sk][c] = dict(self.seen[e])
        for b in reads:
            if b.r.get(sk, 0) < c:
                b.r[sk] = c
        for b in writes:
            b.w = (sk, c)
            b.r = {}
        self.n_instr += 1
        return ins

    def barrier(self):
        if self.dead:
            return
        allk = [(sk, v) for sk, v in self.cnt.items() if v > 0]
        for e in self.eng:
            self._need(e, [d for d in allk if d[0] != e])

    def finish(self, bufs, e="sp"):
        self._need(e, [b.w for b in bufs if b.w is not None])


class Rot:
    def __init__(self, k, name, n, shape, dtype, es=None, psum=False):
        self.items = []
        for i in range(n):
            t = (k.ps if psum else k.sb)("%s%d" % (name, i), shape, dtype, es)
            self.items.append((t, Buf(name)))
        self.i = 0

    def next(self):
        it = self.items[self.i]
        self.i = (self.i + 1) % len(self.items)
        return it


class NS:
    pass


class StopBuild(Exception):
    pass


def mark(S, name):
    if getattr(S, "stop_at", None) == name:
        S.k.dead = True


def setup_common(k, nc, S, L):
    S.L = L
    S.NT = L // 128
    S.X = k.sb("X", [128, S.NT, D], F32)
    S.bX = [Buf("X%d" % i) for i in range(S.NT)]
    S.ident = k.sb("ident", [128, 128], BF16)
    S.bident = Buf("ident")
    k.op("pool", lambda: nc.gpsimd.memset(S.ident[:], 1.0), writes=[S.bident])
    k.op("pool", lambda: nc.gpsimd.affine_select(
        out=S.ident[:], in_=S.ident[:], pattern=[[-1, 128]], compare_op=ALU.is_equal,
        fill=0.0, base=0, channel_multiplier=1), reads=[S.bident], writes=[S.bident])
    S.pb = [k.ps("pb%d" % i, [128, 512], F32) for i in range(7)]
    S.bpb = [Buf("pb%d" % i, excl=True) for i in range(7)]
    S.pT = k.ps("pT", [128, 1024], BF16)
    S.bpT = Buf("pT", excl=True)
    S.ss = Rot(k, "ss", 4, [128, 1], F32)
    S.rs = Rot(k, "rs", 4, [128, 1], F32)
    S.junk = k.sb("junk", [128, 1024], BF16)
    S.bjunk = Buf("junk")
    S.hb = Rot(k, "hb", 1, [128, 1024], BF16)
    S.gbc = Rot(k, "gbc", 1, [128, D], F32)


def load_gbc(k, nc, S, g_ap):
    t, b = S.gbc.next()
    k.dma("sp", t[:], g_ap.partition_broadcast(128), writes=[b])
    return t, b


def rstd_tile(k, nc, S, i):
    ss, bss = S.ss.next()
    rs, brs = S.rs.next()
    k.op("act", lambda: nc.scalar.activation(out=S.junk[:], in_=S.X[:, i, :], func=AF.Square,
                                             accum_out=ss[:]),
         reads=[S.bX[i]], writes=[S.bjunk, bss])
    k.op("act", lambda: nc.scalar.activation(out=ss[:], in_=ss[:], func=AF.Sqrt,
                                             scale=1.0 / D, bias=NORM_EPS),
         reads=[bss], writes=[bss])
    k.op("dve", lambda: nc.vector.reciprocal(out=rs[:], in_=ss[:]), reads=[bss], writes=[brs])
    return rs, brs


def norm_to_hT(k, nc, S, i, gbc, bgbc, dst_ap, bdst, evac_eng="act"):
    rs, brs = rstd_tile(k, nc, S, i)
    hb, bhb = S.hb.next()
    k.op("dve", lambda: nc.vector.scalar_tensor_tensor(
        out=hb[:], in0=S.X[:, i, :], scalar=rs[:], in1=gbc[:], op0=ALU.mult, op1=ALU.mult),
        reads=[S.bX[i], brs, bgbc], writes=[bhb])
    for c in range(DC):
        k.op("pe", lambda: nc.tensor.transpose(out=S.pT[:, c * 128:(c + 1) * 128],
                                               in_=hb[:, c * 128:(c + 1) * 128],
                                               identity=S.ident[:]),
             reads=[bhb, S.bident], writes=[S.bpT])
    src = S.pT[:].rearrange("p (c t) -> p c t", c=DC)
    if evac_eng == "act":
        k.op("act", lambda: nc.scalar.copy(out=dst_ap, in_=src), reads=[S.bpT], writes=[bdst])
    else:
        k.op("dve", lambda: nc.vector.tensor_copy(out=dst_ap, in_=src), reads=[S.bpT],
             writes=[bdst])


def phase_mlp(k, nc, S, g_ap, wup_ap, wdn_ap):
    L, NT = S.L, S.NT
    NTB = L // 512
    FP = 512
    NFP = DFF // FP
    FCP = FP // 128
    with ExitStack() as es:
        hT = k.sb("mlp_hT", [128, DC, L], BF16, es)
        bhT = [Buf("hT%d" % i) for i in range(NT)]
        uT = [k.sb("mlp_uT%d" % s, [128, FCP, L], BF16, es) for s in range(2)]
        buT = [[Buf("uT") for _ in range(NTB)] for s in range(2)]
        wup = [k.sb("mlp_wup%d" % s, [128, DC, FP], BF16, es) for s in range(2)]
        bwup = [Buf("wup") for s in range(2)]
        wdn = [k.sb("mlp_wdn%d" % s, [128, FCP, D], BF16, es) for s in range(2)]
        bwdn = [Buf("wdn") for s in range(2)]
        rtmp = Rot(k, "mlp_rt", 2, [128, 512], F32, es)

        def load_w(fp):
            s = fp % 2
            k.dma("pool", wup[s][:], wup_ap[:, fp * FP:(fp + 1) * FP].rearrange(
                "(c p) n -> p c n", p=128), writes=[bwup[s]])
            k.dma("pool", wdn[s][:], wdn_ap[fp * FP:(fp + 1) * FP, :].rearrange(
                "(c p) n -> p c n", p=128), writes=[bwdn[s]])

        load_w(0)
        load_w(1)
        gbc, bgbc = load_gbc(k, nc, S, g_ap)
        for i in range(NT):
            norm_to_hT(k, nc, S, i, gbc, bgbc, hT[:, :, i * 128:(i + 1) * 128], bhT[i],
                       evac_eng=("act" if i % 2 == 0 else "dve"))

        upb = [0, 1]
        dnb = [2, 3, 4, 5]
        cnt = {"u": 0, "d": 0}

        def up(fp):
            s = fp % 2
            for fc in range(FCP):
                for tb in range(NTB):
                    b = upb[cnt["u"] % 2]
                    cnt["u"] += 1
                    for dc in range(DC):
                        k.op("pe", lambda: nc.tensor.matmul(
                            out=S.pb[b][:], lhsT=wup[s][:, dc, fc * 128:(fc + 1) * 128],
                            rhs=hT[:, dc, tb * 512:(tb + 1) * 512],
                            start=(dc == 0), stop=(dc == DC - 1)),
                            reads=[bwup[s]] + bhT[tb * 4:(tb + 1) * 4], writes=[S.bpb[b]])
                    rt, brt = rtmp.next()
                    k.op("act", lambda: nc.scalar.activation(out=rt[:], in_=S.pb[b][:],
                                                             func=AF.Relu),
                         reads=[S.bpb[b]], writes=[brt])
                    k.op("dve", lambda: nc.vector.tensor_tensor(
                        out=uT[s][:, fc, tb * 512:(tb + 1) * 512], in0=rt[:], in1=rt[:],
                        op=ALU.mult), reads=[brt], writes=[buT[s][tb]])

        def down(fp):
            s = fp % 2
            for i in range(NT):
                for half in range(2):
                    b = dnb[cnt["d"] % 4]
                    cnt["d"] += 1
                    for fc in range(FCP):
                        k.op("pe", lambda: nc.tensor.matmul(
                            out=S.pb[b][:], lhsT=uT[s][:, fc, i * 128:(i + 1) * 128],
                            rhs=wdn[s][:, fc, half * 512:(half + 1) * 512],
                            start=(fc == 0), stop=(fc == FCP - 1)),
                            reads=[bwdn[s], buT[s][i // 4]], writes=[S.bpb[b]])
                    xs = S.X[:, i, half * 512:(half + 1) * 512]
                    k.op("dve", lambda: nc.vector.tensor_tensor(out=xs, in0=S.pb[b][:], in1=xs,
                                                                op=ALU.add),
                         reads=[S.bpb[b], S.bX[i]], writes=[S.bX[i]])
            if fp + 2 < NFP:
                load_w(fp + 2)

        up(0)
        for fp in range(NFP):
            if fp + 1 < NFP:
                up(fp + 1)
            down(fp)
        k.barrier()


def phase_final(k, nc, S, g_ap, out_ap, bout):
    with ExitStack() as es:
        ot = Rot(k, "fin_o", 2, [128, D], F32, es)
        gbc, bgbc = load_gbc(k, nc, S, g_ap)
        for i in range(S.NT):
            rs, brs = rstd_tile(k, nc, S, i)
            o, bo = ot.next()
            k.op("dve", lambda: nc.vector.scalar_tensor_tensor(
                out=o[:], in0=S.X[:, i, :], scalar=rs[:], in1=gbc[:], op0=ALU.mult,
                op1=ALU.mult), reads=[S.bX[i], brs, bgbc], writes=[bo])
            k.dma("sp", out_ap[i * 128:(i + 1) * 128, :], o[:], reads=[bo], writes=[bout])
        k.finish([bout], "sp")
        k.barrier()


PARAM_SHAPES = {
    "mixer_norm": [2, D], "mlp_norm": [2, D], "mlp_w_up": [2, D, DFF], "mlp_w_down": [2, DFF, D],
    "final_norm": [D], "dsa_w_in": [1, D, IN_COLS], "dsa_w_o": [1, D, D],
    "rwkv_mu": [1, 6, D], "rwkv_w_rkv": [1, 3, D, D], "rwkv_w0": [1, D], "rwkv_w1": [1, D, 64],
    "rwkv_w2": [1, 64, D], "rwkv_a0": [1, D], "rwkv_a1": [1, D, 64], "rwkv_a2": [1, 64, D],
    "rwkv_g1": [1, D, 160], "rwkv_g2": [1, 160, D], "rwkv_k_k": [1, D], "rwkv_k_a": [1, D],
    "rwkv_r_k": [1, 16, 64], "rwkv_lnx_w": [1, D], "rwkv_lnx_b": [1, D], "rwkv_w_o": [1, D, D],
}


def dbg(k, S, name, ap, reads):
    if name in S.dbg_t:
        t = S.dbg_t[name]
        b = Buf("dbg")
        k.dma("sp", t, ap, reads=reads, writes=[b])
        S.dbg_b.append(b)


def build(L=2048, phases=("dsa", "mlp0", "rwkv", "mlp1", "final"), debug=None, stop_at=None):
    nc = bass.Bass("TRN2", target_bir_lowering=False)
    P = NS()
    P.x = nc.dram_tensor("x", [L, D], F32, kind="ExternalInput").ap()
    for name, shp in PARAM_SHAPES.items():
        setattr(P, name, nc.dram_tensor(name, shp, F32, kind="ExternalInput").ap())
    P.out = nc.dram_tensor("out", [L, D], F32, kind="ExternalOutput").ap()
    with ExitStack() as es:
        k = K(nc, es)
        global LAST_K
        LAST_K = k
        S = NS()
        S.P = P
        S.dbg_t = {}
        S.stop_at = stop_at
        S.k = k
        S.dbg_b = []
        for name, (shp, dt_) in (debug or {}).items():
            S.dbg_t[name] = nc.dram_tensor("dbg_" + name, list(shp), dt_, kind="ExternalOutput").ap()
        setup_common(k, nc, S, L)
        for i in range(S.NT):
            k.dma("sp", S.X[:, i, :], P.x[i * 128:(i + 1) * 128, :], writes=[S.bX[i]])
        for ph in phases:
            if ph == "dsa":
                phase_dsa(k, nc, S)
            elif ph == "mlp0":
                phase_mlp(k, nc, S, P.mlp_norm[0], P.mlp_w_up[0], P.mlp_w_down[0])
            elif ph == "rwkv":
                phase_rwkv(k, nc, S)
                k.dead = False
                k.barrier()
            elif ph == "mlp1":
                phase_mlp(k, nc, S, P.mlp_norm[1], P.mlp_w_up[1], P.mlp_w_down[1])
            elif ph == "final":
                bout = Buf("out")
                phase_final(k, nc, S, P.final_norm, P.out, bout)
                k.finish([bout], "sp")
            elif ph == "dump":
                bout = Buf("out")
                for i in range(S.NT):
                    k.dma("sp", P.out[i * 128:(i + 1) * 128, :], S.X[:, i, :],
                          reads=[S.bX[i]], writes=[bout])
                k.finish([bout], "sp")
        k.finish(S.dbg_b, "sp")
        print("instr", k.n_instr, "waits", k.n_wait, k.per_eng)
    return nc


def phase_dsa(k, nc, S):
    L, NT, P = S.L, S.NT, S.P
    TOPK = min(256, L // 4)
    NIT = 14
    w_in = P.dsa_w_in[0]
    w_o = P.dsa_w_o[0]
    o0, o1, o2, o3, o4 = 1024, 1280, 1536, 2048, 2112
    NHALF = 2 if L >= 1024 else 1
    LH = L // NHALF
    NTH = LH // 128
    NTBH = LH // 512
    with ExitStack() as es:
        qT = k.sb("qT", [128, 8, L], BF16, es)
        bqT = [Buf("qT%d" % i) for i in range(NT)]
        kT = k.sb("kT", [128, 2, L], BF16, es)
        bkT = [Buf("kT%d" % i) for i in range(NT)]
        qiT = k.sb("qiT", [128, 4, L], BF16, es)
        bqiT = [Buf("qiT%d" % i) for i in range(NT)]
        kiT = k.sb("kiT", [128, 1, L], BF16, es)
        bkiT = [Buf("kiT%d" % i) for i in range(NT)]
        Vaug = k.sb("Vaug", [128, NT, KVH, 65], BF16, es)
        bV = [Buf("V%d" % i) for i in range(NT)]
        wi = k.sb("wi", [128, NT, 8], F32, es)
        bwi = [Buf("wi%d" % i) for i in range(NT)]
        ident4 = k.sb("ident4", [128, 4, 128], BF16, es)
        bident4 = Buf("ident4")
        pw2 = k.sb("pw2", [128, NIT + 2], F32, es)
        bpw2 = Buf("pw2")
        for g in range(4):
            k.op("pool", lambda: nc.gpsimd.tensor_copy(out=ident4[:, g, :], in_=S.ident[:]),
                 reads=[S.bident], writes=[bident4])
        for n in range(NIT + 2):
            k.op("pool", lambda: nc.gpsimd.memset(pw2[:, n:n + 1], 2.0 ** (-(n + 1))),
                 writes=[bpw2])
        k.op("pool", lambda: nc.gpsimd.memset(Vaug[:, :, :, 64:65], 1.0), writes=bV)

        with ExitStack() as es2:
            cosT = k.sb("cosT", [128, LH], F32, es2)
            sinT = k.sb("sinT", [128, LH], F32, es2)
            btab = Buf("tab")
            bhT = [Buf("hT%d" % i) for i in range(NTH)]
            wn = [k.sb("dsa_wn%d" % s, [128, DC, 2, 2, 64], BF16, es2) for s in range(2)]
            wr = [k.sb("dsa_wr%d" % s, [128, DC, 2, 2, 2, 32], BF16, es2) for s in range(2)]
            bwn = [Buf("wn") for s in range(2)]
            bwr = [Buf("wr") for s in range(2)]
            wv = k.sb("dsa_wv", [128, DC, 264], BF16, es2)
            bwv = Buf("wv")
            t1r = Rot(k, "rope_t1", 2, [128, 512], F32, es2)
            t2r = Rot(k, "rope_t2", 2, [128, 512], F32, es2)
            k.dma("pool", wv[:, :, 0:256], w_in[:, o1:o2].rearrange("(c p) n -> p c n", p=128),
                  writes=[bwv])
            k.dma("pool", wv[:, :, 256:264], w_in[:, o4:o4 + 8].rearrange("(c p) n -> p c n", p=128),
                  writes=[bwv])
            gbc, bgbc = load_gbc(k, nc, S, P.mixer_norm[0])

            with ExitStack() as es3:
                pidx = k.sb("pidx", [128, 1], I32, es3)
                pj = k.sb("pj", [128, 1], I32, es3)
                pjf = k.sb("pjf", [128, 1], F32, es3)
                inv = k.sb("rinv", [128, 1], F32, es3)
                sgn = k.sb("rsgn", [128, 1], F32, es3)
                bc = Buf("ropec")
                k.op("pool", lambda: nc.gpsimd.iota(out=pidx[:], pattern=[[0, 1]], base=0,
                                                    channel_multiplier=1), writes=[bc])
                k.op("dve", lambda: nc.vector.tensor_single_scalar(out=pj[:], in_=pidx[:], scalar=31,
                                                                   op=ALU.bitwise_and),
                     reads=[bc], writes=[bc])
                k.op("dve", lambda: nc.vector.tensor_copy(out=pjf[:], in_=pj[:]), reads=[bc], writes=[bc])
                k.op("act", lambda: nc.scalar.activation(out=inv[:], in_=pjf[:], func=AF.Exp,
                                                         scale=-2.0 * math.log(10000.0) / 64.0),
                     reads=[bc], writes=[bc])
                k.op("dve", lambda: nc.vector.tensor_single_scalar(out=pj[:], in_=pidx[:], scalar=32,
                                                                   op=ALU.bitwise_and),
                     reads=[bc], writes=[bc])
                k.op("dve", lambda: nc.vector.tensor_copy(out=pjf[:], in_=pj[:]), reads=[bc], writes=[bc])
                k.op("dve", lambda: nc.vector.tensor_scalar(out=sgn[:], in0=pjf[:], scalar1=2.0 / 32.0,
                                                            scalar2=-1.0, op0=ALU.mult, op1=ALU.add),
                     reads=[bc], writes=[bc])

                def make_tables(hf):
                    with ExitStack() as es4:
                        ti = k.sb("tpos_i", [128, LH], I32, es4)
                        ang = k.sb("ang", [128, LH], F32, es4)
                        rr = k.sb("rr", [128, LH], F32, es4)
                        ki_ = ti
                        mm = k.sb("mm", [128, LH], F32, es4)
                        bt = Buf("tabtmp")
                        TWO_PI = 2.0 * math.pi
                        k.op("pool", lambda: nc.gpsimd.iota(out=ti[:], pattern=[[1, LH]], base=hf * LH,
                                                            channel_multiplier=0), writes=[bt])
                        k.op("dve", lambda: nc.vector.tensor_copy(out=ang[:], in_=ti[:]), reads=[bt], writes=[bt])
                        k.op("dve", lambda: nc.vector.tensor_scalar(out=ang[:], in0=ang[:], scalar1=inv[:],
                                                                    scalar2=None, op0=ALU.mult),
                             reads=[bt, bc], writes=[bt])
                        for which, dst in (("sin", sinT), ("cos", cosT)):
                            off = 0.0 if which == "sin" else math.pi / 2
                            k.op("dve", lambda: nc.vector.tensor_scalar(
                                out=ki_[:], in0=ang[:], scalar1=off, scalar2=1.0 / TWO_PI,
                                op0=ALU.add, op1=ALU.mult), reads=[bt], writes=[bt])
                            k.op("dve", lambda: nc.vector.tensor_copy(out=mm[:], in_=ki_[:]),
                                 reads=[bt], writes=[bt])
                            k.op("dve", lambda: nc.vector.scalar_tensor_tensor(
                                out=rr[:], in0=mm[:], scalar=-TWO_PI, in1=ang[:], op0=ALU.mult,
                                op1=ALU.add), reads=[bt], writes=[bt])
                            if off != 0.0:
                                k.op("dve", lambda: nc.vector.tensor_scalar(
                                    out=rr[:], in0=rr[:], scalar1=off, scalar2=None, op0=ALU.add),
                                    reads=[bt], writes=[bt])
                            k.op("dve", lambda: nc.vector.tensor_scalar(
                                out=mm[:], in0=rr[:], scalar1=math.pi, scalar2=-TWO_PI,
                                op0=ALU.is_gt, op1=ALU.mult), reads=[bt], writes=[bt])
                            k.op("dve", lambda: nc.vector.tensor_tensor(out=rr[:], in0=rr[:], in1=mm[:],
                                                                        op=ALU.add), reads=[bt], writes=[bt])
                            k.op("dve", lambda: nc.vector.tensor_scalar(
                                out=mm[:], in0=rr[:], scalar1=-math.pi, scalar2=TWO_PI,
                                op0=ALU.is_lt, op1=ALU.mult), reads=[bt], writes=[bt])
                            k.op("dve", lambda: nc.vector.tensor_tensor(out=rr[:], in0=rr[:], in1=mm[:],
                                                                        op=ALU.add), reads=[bt], writes=[bt])
                            k.op("dve", lambda: nc.vector.tensor_scalar(
                                out=rr[:], in0=rr[:], scalar1=-3.14159, scalar2=3.14159,
                                op0=ALU.max, op1=ALU.min), reads=[bt], writes=[bt])
                            k.op("act", lambda: nc.scalar.activation(out=dst[:], in_=rr[:], func=AF.Sin),
                                 reads=[bt], writes=[btab, bt])
                        k.op("dve", lambda: nc.vector.tensor_scalar(out=sinT[:], in0=sinT[:], scalar1=sgn[:],
                                                                    scalar2=None, op0=ALU.mult),
                             reads=[btab, bc], writes=[btab])
                        k.barrier()

                specs = []
                for c in range(8):
                    specs.append((qT, c, bqT, c * 64, (c + 8) * 64))
                for c in range(2):
                    specs.append((kT, c, bkT, o0 + c * 64, o0 + (c + 2) * 64))
                for c in range(4):
                    specs.append((qiT, c, bqiT, o2 + c * 64, o2 + (c + 4) * 64))
                specs.append((kiT, 0, bkiT, o3, o3))
                blocks = [specs[i:i + 2] for i in range(0, len(specs), 2)]

                def load_block(bi):
                    s = bi % 2
                    for cc, (_, _, _, lo, up) in enumerate(blocks[bi]):
                        for h, col in enumerate((lo, up)):
                            k.dma("pool", wn[s][:, :, cc, h, :],
                                  w_in[:, col:col + 64].rearrange("(c p) n -> p c n", p=128),
                                  writes=[bwn[s]])
                            for r in range(2):
                                k.dma("pool", wr[s][:, :, cc, h, r, :],
                                      w_in[:, col + (1 - r) * 32:col + (1 - r) * 32 + 32].rearrange(
                                          "(c p) n -> p c n", p=128), writes=[bwr[s]])

                cntp = {"a": 0}
                for hf in range(NHALF):
                    make_tables(hf)
                    es5 = ExitStack()
                    hT = k.sb("dsa_hT", [128, DC, LH], BF16, es5)
                    load_block(0)
                    load_block(1)
                    for it in range(NTH):
                        gi = hf * NTH + it
                        norm_to_hT(k, nc, S, gi, gbc, bgbc, hT[:, :, it * 128:(it + 1) * 128], bhT[it],
                                   evac_eng=("act" if it % 2 == 0 else "dve"))
                    for it in range(NTH):
                        gi = hf * NTH + it
                        for dc in range(DC):
                            k.op("pe", lambda: nc.tensor.matmul(
                                out=S.pb[4][:, 0:264], lhsT=hT[:, dc, it * 128:(it + 1) * 128],
                                rhs=wv[:, dc, :], start=(dc == 0), stop=(dc == DC - 1)),
                                reads=[bhT[it], bwv], writes=[S.bpb[4]])
                        k.op("act", lambda: nc.scalar.copy(
                            out=Vaug[:, gi, :, 0:64],
                            in_=S.pb[4][:, 0:256].rearrange("p (n e) -> p n e", n=KVH)),
                            reads=[S.bpb[4]], writes=[bV[gi]])
                        k.op("act", lambda: nc.scalar.mul(out=wi[:, gi, :], in_=S.pb[4][:, 256:264],
                                                          mul=(8.0 ** -0.5) * (64.0 ** -0.5)),
                             reads=[S.bpb[4]], writes=[bwi[gi]])
                    for bi, blk in enumerate(blocks):
                        s = bi % 2
                        for cc, (dst, dci, bdst, lo, up) in enumerate(blk):
                            for tb in range(NTBH):
                                ba = cntp["a"] % 2
                                bb = 2 + cntp["a"] % 2
                                cntp["a"] += 1
                                for dc in range(DC):
                                    k.op("pe", lambda: nc.tensor.matmul(
                                        out=S.pb[ba][:], lhsT=wn[s][:, dc, cc, :, :],
                                        rhs=hT[:, dc, tb * 512:(tb + 1) * 512],
                                        start=(dc == 0), stop=(dc == DC - 1)),
                                        reads=[bwn[s]] + bhT[tb * 4:(tb + 1) * 4], writes=[S.bpb[ba]])
                                for dc in range(DC):
                                    k.op("pe", lambda: nc.tensor.matmul(
                                        out=S.pb[bb][:], lhsT=wr[s][:, dc, cc, :, :, :],
                                        rhs=hT[:, dc, tb * 512:(tb + 1) * 512],
                                        start=(dc == 0), stop=(dc == DC - 1)),
                                        reads=[bwr[s]] + bhT[tb * 4:(tb + 1) * 4], writes=[S.bpb[bb]])
                                t1, bt1 = t1r.next()
                                t2, bt2 = t2r.next()
                                tsl = slice(tb * 512, (tb + 1) * 512)
                                k.op("dve", lambda: nc.vector.tensor_tensor(
                                    out=t1[:], in0=S.pb[ba][:], in1=cosT[:, tsl], op=ALU.mult),
                                    reads=[S.bpb[ba], btab], writes=[bt1])
                                k.op("dve", lambda: nc.vector.tensor_tensor(
                                    out=t2[:], in0=S.pb[bb][:], in1=sinT[:, tsl], op=ALU.mult),
                                    reads=[S.bpb[bb], btab], writes=[bt2])
                                g0 = hf * LH + tb * 512
                                gt = g0 // 128
                                k.op("pool", lambda: nc.gpsimd.tensor_tensor(
                                    out=dst[:, dci, g0:g0 + 512], in0=t1[:], in1=t2[:], op=ALU.add),
                                    reads=[bt1, bt2], writes=bdst[gt:gt + 4])
                        if bi + 2 < len(blocks):
                            load_block(bi + 2)
                    k.barrier()
                    es5.close()

        k.barrier()
        dbg(k, S, "qT", qT[:], bqT)
        dbg(k, S, "kT", kT[:], bkT)
        dbg(k, S, "qiT", qiT[:], bqiT)
        dbg(k, S, "kiT", kiT[:], bkiT)
        dbg(k, S, "Vaug", Vaug[:], bV)
        dbg(k, S, "wi", wi[:], bwi)
        with ExitStack() as es2:
            accR = Rot(k, "acc", 2, [128, L], F32, es2)
            amR = Rot(k, "amask", 2, [128, L], BF16, es2)
            junk2 = k.sb("junk2", [128, L], BF16, es2)
            bjunk2 = Buf("junk2")
            rlR = Rot(k, "rl", 2, [128, 512], F32, es2)
            ptR = Rot(k, "PT", 3, [128, 4, 128], BF16, es2)
            osR = Rot(k, "osb", 2, [128, D], BF16, es2)
            smR = Rot(k, "bis", 2, [128, 16], F32, es2)
            stR = Rot(k, "bstp", 2, [128, 2 * (NIT + 2)], F32, es2)
            recR = Rot(k, "rec", 2, [128, 4], F32, es2)
            cz = {"z": 0, "s": 0, "o": 0}
            tile_state = {}

            def gen_index(i):
                W = (i + 1) * 128
                acc, bacc = accR.next()
                am, bam = amR.next()
                sm, bsm = smR.next()
                tile_state[i] = (acc, bacc, am, bam, sm, bsm)
                for h in range(IDXH):
                    c = h % 4
                    plo = 64 * (h // 4)
                    for w0 in range(0, W, 512):
                        w1 = min(W, w0 + 512)
                        zb = cz["z"] % 2
                        cz["z"] += 1
                        k.op("pe", lambda: nc.tensor.matmul(
                            out=S.pb[zb][:, 0:w1 - w0], lhsT=qiT[plo:plo + 64, c, i * 128:(i + 1) * 128],
                            rhs=kiT[plo:plo + 64, 0, w0:w1], start=True, stop=True),
                            reads=[bqiT[i]] + bkiT[w0 // 128:w1 // 128], writes=[S.bpb[zb]])
                        rl, brl = rlR.next()
                        k.op("act", lambda: nc.scalar.activation(out=rl[:, 0:w1 - w0],
                                                                 in_=S.pb[zb][:, 0:w1 - w0], func=AF.Relu),
                             reads=[S.bpb[zb]], writes=[brl])
                        if h == 0:
                            k.op("dve", lambda: nc.vector.tensor_scalar(
                                out=acc[:, w0:w1], in0=rl[:, 0:w1 - w0], scalar1=wi[:, i, 0:1],
                                scalar2=None, op0=ALU.mult), reads=[brl, bwi[i]], writes=[bacc])
                        else:
                            k.op("dve", lambda: nc.vector.scalar_tensor_tensor(
                                out=acc[:, w0:w1], in0=rl[:, 0:w1 - w0], scalar=wi[:, i, h:h + 1],
                                in1=acc[:, w0:w1], op0=ALU.mult, op1=ALU.add),
                                reads=[brl, bwi[i], bacc], writes=[bacc])
                        yield
                k.op("pool", lambda: nc.gpsimd.affine_select(
                    out=acc[:, i * 128:W], in_=acc[:, i * 128:W], pattern=[[-1, 128]],
                    compare_op=ALU.is_ge, fill=-1e30, base=0, channel_multiplier=1),
                    reads=[bacc], writes=[bacc])
                yield

            def gen_bisect(i):
                W = (i + 1) * 128
                acc, bacc, am, bam, sm, bsm = tile_state[i]
                if W <= TOPK:
                    k.op("dve", lambda: nc.vector.tensor_scalar(
                        out=am[:, 0:W], in0=acc[:, 0:W], scalar1=-1e29, scalar2=NEG,
                        op0=ALU.is_lt, op1=ALU.mult), reads=[bacc], writes=[bam])
                    yield
                    return
                st, bst = stR.next()
                hi_, lo_, rng_, mid, cntv, tt, thr = [sm[:, j:j + 1] for j in range(7)]
                k.op("dve", lambda: nc.vector.tensor_reduce(out=hi_, in_=acc[:, 0:W], axis=AX.X,
                                                            op=ALU.max), reads=[bacc], writes=[bsm])
                k.op("dve", lambda: nc.vector.tensor_reduce(out=lo_, in_=acc[:, 0:i * 128], axis=AX.X,
                                                            op=ALU.min), reads=[bacc], writes=[bsm])
                k.op("dve", lambda: nc.vector.tensor_tensor(out=rng_, in0=hi_, in1=lo_,
                                                            op=ALU.subtract), reads=[bsm], writes=[bsm])
                k.op("dve", lambda: nc.vector.tensor_scalar(
                    out=st[:, 0:NIT + 2], in0=pw2[:, :], scalar1=rng_, scalar2=None, op0=ALU.mult),
                    reads=[bsm, bpw2], writes=[bst])
                k.op("dve", lambda: nc.vector.tensor_scalar(
                    out=st[:, NIT + 2:2 * (NIT + 2)], in0=pw2[:, :], scalar1=rng_, scalar2=2.0,
                    op0=ALU.mult, op1=ALU.mult), reads=[bsm, bpw2], writes=[bst])
                k.op("dve", lambda: nc.vector.tensor_tensor(out=mid, in0=lo_, in1=st[:, 0:1],
                                                            op=ALU.add), reads=[bsm, bst], writes=[bsm])
                yield
                for n in range(NIT + 1):
                    k.op("dve", lambda: nc.vector.tensor_scalar(
                        out=junk2[:, 0:W], in0=acc[:, 0:W], scalar1=mid, scalar2=0.0,
                        op0=ALU.is_ge, op1=ALU.add, accum_out=cntv),
                        reads=[bacc, bsm], writes=[bjunk2, bsm])
                    if n < NIT:
                        k.op("dve", lambda: nc.vector.tensor_scalar(
                            out=tt, in0=cntv, scalar1=TOPK - 0.5, scalar2=st[:, NIT + 2 + n + 1:NIT + 2 + n + 2],
                            op0=ALU.is_ge, op1=ALU.mult), reads=[bsm, bst], writes=[bsm])
                        k.op("dve", lambda: nc.vector.scalar_tensor_tensor(
                            out=mid, in0=tt, scalar=st[:, n + 1:n + 2], in1=mid, op0=ALU.subtract,
                            op1=ALU.add), reads=[bsm, bst], writes=[bsm])
                    else:
                        k.op("dve", lambda: nc.vector.tensor_scalar(
                            out=tt, in0=cntv, scalar1=TOPK - 0.5, scalar2=st[:, n:n + 1],
                            op0=ALU.is_lt, op1=ALU.mult), reads=[bsm, bst], writes=[bsm])
                        k.op("dve", lambda: nc.vector.tensor_tensor(out=thr, in0=mid, in1=tt,
                                                                    op=ALU.subtract),
                             reads=[bsm], writes=[bsm])
                    yield
                k.op("dve", lambda: nc.vector.tensor_scalar(
                    out=am[:, 0:W], in0=acc[:, 0:W], scalar1=thr, scalar2=NEG,
                    op0=ALU.is_lt, op1=ALU.mult), reads=[bacc, bsm], writes=[bam])
                yield

            def gen_attn(i):
                acc, bacc, am, bam, sm, bsm = tile_state[i]
                osb, bosb = osR.next()
                for n in range(KVH):
                    plo = 64 * (n // 2)
                    qc0 = 4 * (n % 2)
                    ob = 4 + cz["o"] % 2
                    cz["o"] += 1
                    for j in range(i + 1):
                        sbk = 2 + cz["s"] % 2
                        cz["s"] += 1
                        k.op("pe", lambda: nc.tensor.matmul(
                            out=S.pb[sbk][:].rearrange("p (g t) -> p g t", g=4),
                            lhsT=kT[plo:plo + 64, n % 2, j * 128:(j + 1) * 128],
                            rhs=qT[plo:plo + 64, qc0:qc0 + 4, i * 128:(i + 1) * 128],
                            start=True, stop=False),
                            reads=[bkT[j], bqT[i]], writes=[S.bpb[sbk]])
                        k.op("pe", lambda: nc.tensor.matmul(
                            out=S.pb[sbk][:].rearrange("p (g t) -> p g t", g=4),
                            lhsT=am[:, j * 128:(j + 1) * 128], rhs=ident4[:, :, :],
                            start=False, stop=True),
                            reads=[bam, bident4], writes=[S.bpb[sbk]])
                        pt, bpt = ptR.next()
                        k.op("act", lambda: nc.scalar.activation(
                            out=pt[:].rearrange("p g t -> p (g t)"), in_=S.pb[sbk][:], func=AF.Exp,
                            scale=0.125), reads=[S.bpb[sbk]], writes=[bpt])
                        for g in range(4):
                            k.op("pe", lambda: nc.tensor.matmul(
                                out=S.pb[ob][:, g * 65:(g + 1) * 65], lhsT=pt[:, g, :],
                                rhs=Vaug[:, j, n, :], start=(j == 0 and g == 0), stop=(j == i),
                                skip_group_check=True),
                                reads=[bpt, bV[j]], writes=[S.bpb[ob]])
                        yield
                    rec, brec = recR.next()
                    ov = S.pb[ob][:, 0:260].rearrange("p (g e) -> p g e", g=4)
                    k.op("dve", lambda: nc.vector.reciprocal(out=rec[:].rearrange("p (g o) -> p g o", o=1),
                                                             in_=ov[:, :, 64:65]),
                         reads=[S.bpb[ob]], writes=[brec])
                    k.op("dve", lambda: nc.vector.tensor_tensor(
                        out=osb[:, n * 256:(n + 1) * 256].rearrange("p (g e) -> p g e", g=4),
                        in0=ov[:, :, 0:64],
                        in1=rec[:].rearrange("p (g o) -> p g o", o=1).to_broadcast([128, 4, 64]),
                        op=ALU.mult), reads=[S.bpb[ob], brec], writes=[bosb])
                for c in range(DC):
                    k.op("pe", lambda: nc.tensor.transpose(out=S.pT[:, c * 128:(c + 1) * 128],
                                                           in_=osb[:, c * 128:(c + 1) * 128],
                                                           identity=S.ident[:]),
                         reads=[bosb, S.bident], writes=[S.bpT])
                k.op("act", lambda: nc.scalar.copy(out=qT[:, :, i * 128:(i + 1) * 128],
                                                   in_=S.pT[:].rearrange("p (c t) -> p c t", c=DC)),
                     reads=[S.bpT], writes=[bqT[i]])
                yield

            def chain(*gens):
                for g in gens:
                    for _ in g:
                        yield

            for _ in chain(gen_index(0), gen_bisect(0)):
                pass
            for i in range(NT):
                a_steps = 4 * (i + 1) + 1
                if i + 1 < NT:
                    nxt = chain(gen_index(i + 1), gen_bisect(i + 1))
                    Wn = (i + 2) * 128
                    n_steps = IDXH * ((Wn + 511) // 512) + 1 + (NIT + 3 if Wn > TOPK else 1)
                else:
                    nxt = iter(())
                    n_steps = 0
                done = 0
                for si, _ in enumerate(gen_attn(i)):
                    target = (n_steps * (si + 1) + a_steps - 1) // a_steps
                    while done < target:
                        try:
                            next(nxt)
                        except StopIteration:
                            break
                        done += 1
                for _ in nxt:
                    pass
        k.barrier()
        with ExitStack() as es2:
            wo = k.sb("dsa_wo", [128, DC, D], BF16, es2)
            bwo = Buf("wo")
            for c in range(DC):
                k.dma("pool", wo[:, c, :], w_o[c * 128:(c + 1) * 128, :], writes=[bwo])
            cc = 0
            for i in range(NT):
                for half in range(2):
                    b = 2 + cc % 4
                    cc += 1
                    for c in range(DC):
                        k.op("pe", lambda: nc.tensor.matmul(
                            out=S.pb[b][:], lhsT=qT[:, c, i * 128:(i + 1) * 128],
                            rhs=wo[:, c, half * 512:(half + 1) * 512], start=(c == 0), stop=(c == DC - 1)),
                            reads=[bqT[i], bwo], writes=[S.bpb[b]])
                    xs = S.X[:, i, half * 512:(half + 1) * 512]
                    k.op("dve", lambda: nc.vector.tensor_tensor(out=xs, in0=S.pb[b][:], in1=xs, op=ALU.add),
                         reads=[S.bpb[b], S.bX[i]], writes=[S.bX[i]])
            k.barrier()


def phase_rwkv(k, nc, S):
    L, NT, P = S.L, S.NT, S.P
    G, CH, NHG, FC = 2, 512, 8, 4
    CDEC = math.exp(-0.5)
    mu = P.rwkv_mu[0]

    def v3(ap, a):
        return ap.rearrange("p (a b) -> p a b", a=a)

    def bc3(ap2, a, b):
        return ap2.unsqueeze(2).to_broadcast([128, a, b])

    with ExitStack() as es:
        triI = k.sb("triI", [128, 128], F32, es)
        triS = k.sb("triS", [128, 128], F32, es)
        negc = k.sb("negc", [128, 1], F32, es)
        selw = k.sb("selw", [2, 128], F32, es)
        sela = k.sb("sela", [2, 128], F32, es)
        rows = k.sb("rows", [2, CH], F32, es)
        maskL = k.sb("maskL", [128, 128], BF16, es)
        maskU = k.sb("maskU", [128, 128], BF16, es)
        maskUi = k.sb("maskUi", [128, 128], BF16, es)
        muT = k.sb("muT", [128, 6, DC], F32, es)
        bcv = k.sb("bcv", [128, 5, CH], BF16, es)
        wl1 = k.sb("wl1", [128, DC, 288], BF16, es)
        yo0 = k.sb("yo0", [128, NT, CH], BF16, es)
        byo0 = [Buf("yo0") for _ in range(NT)]
        bconst = Buf("rconst")
        k.op("pool", lambda: nc.gpsimd.memset(triI[:], -CDEC), writes=[bconst])
        k.op("pool", lambda: nc.gpsimd.affine_select(out=triI[:], in_=triI[:], pattern=[[1, 128]],
                                                     compare_op=ALU.is_ge, fill=0.0, base=0,
                                                     channel_multiplier=-1), reads=[bconst], writes=[bconst])
        k.op("pool", lambda: nc.gpsimd.memset(triS[:], -CDEC), writes=[bconst])
        k.op("pool", lambda: nc.gpsimd.affine_select(out=triS[:], in_=triS[:], pattern=[[1, 128]],
                                                     compare_op=ALU.is_gt, fill=0.0, base=0,
                                                     channel_multiplier=-1), reads=[bconst], writes=[bconst])
        k.op("pool", lambda: nc.gpsimd.memset(negc[:], -CDEC), writes=[bconst])
        for sel_, base_ in ((selw, 0), (sela, -1)):
            k.op("pool", lambda: nc.gpsimd.memset(sel_[:], 1.0), writes=[bconst])
            k.op("pool", lambda: nc.gpsimd.affine_select(out=sel_[:], in_=sel_[:], pattern=[[0, 128]],
                                                         compare_op=ALU.is_equal, fill=0.0, base=base_,
                                                         channel_multiplier=1), reads=[bconst], writes=[bconst])
        for m_, (cmp_, cm, pat) in ((maskL, (ALU.is_gt, 1, -1)), (maskU, (ALU.is_gt, -1, 1)),
                                    (maskUi, (ALU.is_ge, -1, 1))):
            k.op("pool", lambda: nc.gpsimd.memset(m_[:], 1.0), writes=[bconst])
            k.op("pool", lambda: nc.gpsimd.affine_select(out=m_[:], in_=m_[:], pattern=[[pat, 128]],
                                                         compare_op=cmp_, fill=0.0, base=0,
                                                         channel_multiplier=cm), reads=[bconst], writes=[bconst])
        NLEVS = 7
        lmask = k.sb("lmask", [128, NLEVS, 2, 128], BF16, es)
        with ExitStack() as esm:
            iI = k.sb("iI", [128, 128], I32, esm)
            iJ = k.sb("iJ", [128, 128], I32, esm)
            iT = k.sb("iT", [128, 128], I32, esm)
            eqm = k.sb("eqm", [128, 128], BF16, esm)
            bm = Buf("lmtmp")
            k.op("pool", lambda: nc.gpsimd.iota(out=iI[:], pattern=[[0, 128]], base=0, channel_multiplier=1), writes=[bm])
            k.op("pool", lambda: nc.gpsimd.iota(out=iJ[:], pattern=[[1, 128]], base=0, channel_multiplier=0), writes=[bm])
            k.op("dve", lambda: nc.vector.tensor_tensor(out=iI[:], in0=iI[:], in1=iJ[:], op=ALU.bitwise_xor),
                 reads=[bm], writes=[bm])
            for l in range(1, NLEVS + 1):
                k.op("dve", lambda: nc.vector.tensor_single_scalar(out=iT[:], in_=iI[:], scalar=l - 1,
                                                                   op=ALU.logical_shift_right), reads=[bm], writes=[bm])
                k.op("dve", lambda: nc.vector.tensor_single_scalar(out=eqm[:], in_=iT[:], scalar=1, op=ALU.is_equal),
                     reads=[bm], writes=[bm])
                k.op("dve", lambda: nc.vector.tensor_tensor(out=lmask[:, l - 1, 0, :], in0=eqm[:], in1=maskL[:], op=ALU.mult),
                     reads=[bm, bconst], writes=[bconst])
                k.op("dve", lambda: nc.vector.tensor_tensor(out=lmask[:, l - 1, 1, :], in0=eqm[:], in1=maskU[:], op=ALU.mult),
                     reads=[bm, bconst], writes=[bconst])
            k.barrier()
        with nc.allow_non_contiguous_dma(reason="tiny per-channel vectors"):
            for c in range(6):
                k.dma("sp", muT[:, c, :], mu[c].rearrange("(dc p) -> p dc", p=128), writes=[bconst])
        k.dma("pool", wl1[:, :, 0:64], P.rwkv_w1[0].rearrange("(c p) n -> p c n", p=128), writes=[bconst])
        k.dma("pool", wl1[:, :, 64:128], P.rwkv_a1[0].rearrange("(c p) n -> p c n", p=128), writes=[bconst])
        k.dma("pool", wl1[:, :, 128:288], P.rwkv_g1[0].rearrange("(c p) n -> p c n", p=128), writes=[bconst])
        gbc, bgbc = load_gbc(k, nc, S, P.mixer_norm[1])

        mark(S, "const")
        wr_ = k.sb("rw_wr", [128, DC, CH], BF16, es)
        wk_ = k.sb("rw_wk", [128, DC, CH], BF16, es)
        wv_ = k.sb("rw_wv", [128, DC, CH], BF16, es)
        wa2 = k.sb("rw_wa2", [64, CH], BF16, es)
        a2g_ = k.sb("rw_a2g", [64, CH], BF16, es)
        g2a = k.sb("rw_g2a", [128, CH], BF16, es)
        g2b = k.sb("rw_g2b", [32, CH], BF16, es)
        bw = Buf("rw_w")
        wo = k.sb("rw_wo", [128, DC, D], BF16, es)
        bwo = Buf("rw_wo")
        hc1 = k.sb("rw_hc", [128, DC, 129], BF16, es)
        hc = [hc1, hc1]
        bhc1 = Buf("hc")
        bhc = [bhc1, bhc1]
        dx = k.sb("rw_dx", [128, DC, 128], BF16, es)
        bdx = Buf("dx")
        xsR = Rot(k, "rw_xs", 2, [128, DC, 128], BF16, es)
        Fp = [k.sb("rw_F%d" % i, [128, CH], F32, es) for i in range(8)]
        bF = [Buf("F%d" % i) for i in range(8)]
        small = k.sb("rw_small", [128, 64], F32, es)
        bsmall = Buf("small")
        lT = k.sb("rw_lT", [128, 3, 128], BF16, es)
        g1b = k.sb("rw_g1b", [32, 128], BF16, es)
        blT = Buf("lT")
        Vb = k.sb("rw_V", [128, CH], BF16, es)
        bVb = Buf("V")
        tok4 = k.sb("rw_tok4", [128, 4, CH], BF16, es)
        btok = [Buf("tok%d" % i) for i in range(4)]
        KRT = k.sb("rw_KRT", [128, FC, 2, 128], BF16, es)
        KBT = k.sb("rw_KBT", [128, 2, FC, 128], BF16, es)
        bKRT = Buf("KRT")
        bKBT = Buf("KBT")
        Pm = [k.sb("rw_P%d" % i, [128, NHG, 128], BF16, es) for i in range(2)]
        PTm = [k.sb("rw_PT%d" % i, [128, NHG, 128], BF16, es) for i in range(2)]
        bPm = [Buf("P") for _ in range(2)]
        bPTm = [Buf("PT") for _ in range(2)]
        Ym = [k.sb("rw_Y%d" % i, [128, NHG, 128], BF16, es) for i in range(2)]
        bYm = [Buf("Y") for _ in range(2)]
        QT = PTm[1]
        bQT = bPTm[1]
        ArbT = k.sb("rw_ArbT", [128, NHG, 128], BF16, es)
        AkkT = k.sb("rw_AkkT", [128, NHG, 128], BF16, es)
        ArkT = k.sb("rw_ArkT", [128, NHG, 128], BF16, es)
        bArb, bAkk, bArk = Buf("Arb"), Buf("Akk"), Buf("Ark")
        Hf = k.sb("rw_Hf", [128, FC * 64], F32, es)
        Hb = k.sb("rw_Hb", [128, FC * 64], BF16, es)
        bHf, bHb = Buf("Hf"), Buf("Hb")
        pC = k.sb("rw_pC", [128, FC], F32, es)
        bpC = Buf("pC")
        Zs = k.sb("rw_Zs", [128, CH], BF16, es)
        Us = k.sb("rw_Us", [128, CH], BF16, es)
        bZs, bUs = Buf("Zs"), Buf("Us")
        yo = Zs
        byo = bZs
        pb, bpb = S.pb, S.bpb
        prot = {"i": 0}

        def pbank():
            b = prot["i"] % 3
            prot["i"] += 1
            return b

        for hg in range(G):
            c0 = hg * CH
            for wt, ci in ((wr_, 0), (wk_, 1), (wv_, 2)):
                k.dma("pool", wt[:], P.rwkv_w_rkv[0][ci][:, c0:c0 + CH].rearrange("(c p) n -> p c n", p=128),
                      writes=[bw])
            k.dma("pool", wa2[0:64, :], P.rwkv_w2[0][:, c0:c0 + CH], writes=[bw])
            k.dma("pool", a2g_[:], P.rwkv_a2[0][:, c0:c0 + CH], writes=[bw])
            k.dma("pool", g2a[:], P.rwkv_g2[0][0:128, c0:c0 + CH], writes=[bw])
            k.dma("pool", g2b[:], P.rwkv_g2[0][128:160, c0:c0 + CH], writes=[bw])
            k.dma("sp", rows[0:1, :], P.rwkv_w0[0:1, c0:c0 + CH], writes=[bw])
            k.dma("sp", rows[1:2, :], P.rwkv_a0[0:1, c0:c0 + CH], writes=[bw])
            for j, vec in enumerate((P.rwkv_k_k[0], P.rwkv_k_a[0], P.rwkv_r_k[0].rearrange("h n -> (h n)"),
                                     P.rwkv_lnx_w[0], P.rwkv_lnx_b[0])):
                k.dma("pool", bcv[:, j, :], vec[c0:c0 + CH].partition_broadcast(128), writes=[bw])
            if hg == G - 1:
                for c in range(DC):
                    k.dma("pool", wo[:, c, :], P.rwkv_w_o[0][c * 128:(c + 1) * 128, :], writes=[bwo])
            k.op("dve", lambda: nc.vector.memset(Hf[:], 0.0), writes=[bHf])
            k.op("dve", lambda: nc.vector.memset(Hb[:], 0.0), writes=[bHb])
            k.op("dve", lambda: nc.vector.memset(hc[0][:, :, 0:1], 0.0), writes=[bhc[0]])

            for n in range(NT):
                cur, nxt = n % 2, (n + 1) % 2
                h_ = hc[cur]
                norm_to_hT(k, nc, S, n, gbc, bgbc, h_[:, :, 1:129], bhc[cur],
                           evac_eng=("act" if n % 2 == 0 else "dve"))
                mark(S, "P0")
                k.op("dve", lambda: nc.vector.tensor_tensor(out=dx[:], in0=h_[:, :, 0:128], in1=h_[:, :, 1:129],
                                                            op=ALU.subtract), reads=[bhc[cur]], writes=[bdx])
                if n + 1 < NT:
                    k.op("dve", lambda: nc.vector.tensor_copy(out=h_[:, :, 0:1], in_=h_[:, :, 128:129]),
                         reads=[bhc[cur]], writes=[bhc[cur]])

                def mix(c):
                    xs, bxs = xsR.next()
                    k.op("dve", lambda: nc.vector.tensor_tensor(out=xs[:], in0=dx[:],
                                                                in1=bc3(muT[:, c, :], DC, 128), op=ALU.mult),
                         reads=[bdx, bconst], writes=[bxs])
                    k.op("pool", lambda: nc.gpsimd.tensor_tensor(out=xs[:], in0=xs[:], in1=h_[:, :, 1:129],
                                                                 op=ALU.add), reads=[bxs, bhc[cur]], writes=[bxs])
                    return xs, bxs

                def proj_tok(xs, bxs, wt, extra=None):
                    b = pbank()
                    nmm = DC + (1 if extra else 0)
                    for dc in range(DC):
                        k.op("pe", lambda: nc.tensor.matmul(out=pb[b][:], lhsT=xs[:, dc, :], rhs=wt[:, dc, :],
                                                            start=(dc == 0), stop=(dc == nmm - 1)),
                             reads=[bxs, bw], writes=[bpb[b]])
                    return b

                mark(S, "P1")
                R_, K_, SG, A_, PP, PINV, GG, TA = range(8)
                PPREV = PP
                xs, bxs = mix(0)
                b = proj_tok(xs, bxs, wr_)
                k.op("act", lambda: nc.scalar.copy(out=Fp[R_][:], in_=pb[b][:]), reads=[bpb[b]], writes=[bF[R_]])
                xs, bxs = mix(1)
                b = proj_tok(xs, bxs, wk_)
                k.op("act", lambda: nc.scalar.copy(out=Fp[K_][:], in_=pb[b][:]), reads=[bpb[b]], writes=[bF[K_]])
                xs, bxs = mix(2)
                b = proj_tok(xs, bxs, wv_)
                k.op("act", lambda: nc.scalar.copy(out=Vb[:], in_=pb[b][:]), reads=[bpb[b]], writes=[bVb])
                mark(S, "P2a")
                b = pbank()
                xs, bxs = mix(3)
                for dc in range(DC):
                    k.op("pe", lambda: nc.tensor.matmul(out=pb[b][0:64, 0:128], lhsT=wl1[:, dc, 0:64], rhs=xs[:, dc, :],
                                                        start=(dc == 0), stop=(dc == DC - 1), skip_group_check=True),
                         reads=[bxs, bconst], writes=[bpb[b]])
                xs, bxs = mix(4)
                for dc in range(DC):
                    k.op("pe", lambda: nc.tensor.matmul(out=pb[b][0:64, 384:512], lhsT=wl1[:, dc, 64:128], rhs=xs[:, dc, :],
                                                        start=False, stop=(dc == DC - 1), skip_group_check=True),
                         reads=[bxs, bconst], writes=[bpb[b]])
                xs, bxs = mix(5)
                for dc in range(DC):
                    k.op("pe", lambda: nc.tensor.matmul(out=pb[b][:, 128:256], lhsT=wl1[:, dc, 128:256], rhs=xs[:, dc, :],
                                                        start=(dc == 0), stop=(dc == DC - 1), skip_group_check=True),
                         reads=[bxs, bconst], writes=[bpb[b]])
                for dc in range(DC):
                    k.op("pe", lambda: nc.tensor.matmul(out=pb[b][0:32, 256:384], lhsT=wl1[:, dc, 256:288], rhs=xs[:, dc, :],
                                                        start=False, stop=(dc == DC - 1), skip_group_check=True),
                         reads=[bxs, bconst], writes=[bpb[b]])
                k.op("act", lambda: nc.scalar.activation(out=lT[0:64, 0, :], in_=pb[b][0:64, 0:128], func=AF.Tanh),
                     reads=[bpb[b]], writes=[blT])
                k.op("act", lambda: nc.scalar.copy(out=lT[0:64, 2, :], in_=pb[b][0:64, 384:512]),
                     reads=[bpb[b]], writes=[blT])
                k.op("act", lambda: nc.scalar.activation(out=lT[:, 1, :], in_=pb[b][:, 128:256], func=AF.Sigmoid),
                     reads=[bpb[b]], writes=[blT])
                k.op("act", lambda: nc.scalar.activation(out=g1b[:], in_=pb[b][0:32, 256:384], func=AF.Sigmoid),
                     reads=[bpb[b]], writes=[blT])
                mark(S, "P2b")
                b = pbank()
                k.op("pe", lambda: nc.tensor.matmul(out=pb[b][:], lhsT=lT[0:64, 0, :], rhs=wa2[0:64, :], start=True, stop=False),
                     reads=[blT, bw], writes=[bpb[b]])
                k.op("pe", lambda: nc.tensor.matmul(out=pb[b][:], lhsT=selw[:], rhs=rows[:, :],
                                                    start=False, stop=True), reads=[bconst, bw], writes=[bpb[b]])
                k.op("act", lambda: nc.scalar.activation(out=Fp[SG][:], in_=pb[b][:], func=AF.Sigmoid),
                     reads=[bpb[b]], writes=[bF[SG]])
                b = pbank()
                k.op("pe", lambda: nc.tensor.matmul(out=pb[b][:], lhsT=lT[0:64, 2, :], rhs=a2g_[:], start=True, stop=False),
                     reads=[blT, bw], writes=[bpb[b]])
                k.op("pe", lambda: nc.tensor.matmul(out=pb[b][:], lhsT=sela[:], rhs=rows[:, :],
                                                    start=False, stop=True), reads=[bconst, bw], writes=[bpb[b]])
                k.op("act", lambda: nc.scalar.activation(out=Fp[A_][:], in_=pb[b][:], func=AF.Sigmoid),
                     reads=[bpb[b]], writes=[bF[A_]])
                b = pbank()
                k.op("pe", lambda: nc.tensor.matmul(out=pb[b][:], lhsT=lT[:, 1, :], rhs=g2a[:], start=True, stop=False),
                     reads=[blT, bw], writes=[bpb[b]])
                k.op("pe", lambda: nc.tensor.matmul(out=pb[b][:], lhsT=g1b[:], rhs=g2b[:], start=False, stop=True),
                     reads=[blT, bw], writes=[bpb[b]])
                k.op("act", lambda: nc.scalar.copy(out=Fp[GG][:], in_=pb[b][:]), reads=[bpb[b]], writes=[bF[GG]])
                mark(S, "P2c")
                b = pbank()
                k.op("pe", lambda: nc.tensor.matmul(out=pb[b][:], lhsT=triI[:], rhs=Fp[SG][:], start=True, stop=True),
                     reads=[bconst, bF[SG]], writes=[bpb[b]])
                k.op("act", lambda: nc.scalar.activation(out=Fp[PP][:], in_=pb[b][:], func=AF.Exp),
                     reads=[bpb[b]], writes=[bF[PP]])
                k.op("act", lambda: nc.scalar.activation(out=Fp[PINV][:], in_=pb[b][:], func=AF.Exp, scale=-1.0),
                     reads=[bpb[b]], writes=[bF[PINV]])
                k.op("pool", lambda: nc.gpsimd.tensor_tensor(out=tok4[:, 1, :], in0=Fp[R_][:], in1=Fp[PP][:],
                                                             op=ALU.mult), reads=[bF[R_], bF[PP]], writes=[btok[1]])
                b = pbank()
                k.op("pe", lambda: nc.tensor.matmul(out=pb[b][:], lhsT=triS[:], rhs=Fp[SG][:], start=True, stop=True),
                     reads=[bconst, bF[SG]], writes=[bpb[b]])
                k.op("act", lambda: nc.scalar.activation(out=Fp[PPREV][:], in_=pb[b][:], func=AF.Exp),
                     reads=[bpb[b]], writes=[bF[PPREV]])
                b = pbank()
                for fc in range(FC):
                    k.op("pe", lambda: nc.tensor.matmul(out=pb[b][:, fc:fc + 1],
                                                        lhsT=Fp[SG][:, fc * 128:(fc + 1) * 128], rhs=negc[:],
                                                        start=(fc == 0), stop=True, skip_group_check=True),
                         reads=[bconst, bF[SG]], writes=[bpb[b]])
                k.op("act", lambda: nc.scalar.activation(out=pC[:], in_=pb[b][:, 0:FC], func=AF.Exp),
                     reads=[bpb[b]], writes=[bpC])
                mark(S, "P3")
                kkb = bcv[:, 0, :]
                kab = bcv[:, 1, :]
                rkb = bcv[:, 2, :]
                TB, TC = SG, TA + 0
                k.op("dve", lambda: nc.vector.tensor_tensor(out=Fp[TA][:], in0=Fp[K_][:], in1=kkb, op=ALU.mult),
                     reads=[bF[K_], bw], writes=[bF[TA]])
                k.op("pool", lambda: nc.gpsimd.tensor_tensor(out=Fp[TB][:], in0=Fp[TA][:], in1=Fp[TA][:], op=ALU.mult),
                     reads=[bF[TA]], writes=[bF[TB]])
                k.op("dve", lambda: nc.vector.tensor_reduce(out=small[:, 0:8], in_=v3(Fp[TB][:], NHG), axis=AX.X,
                                                            op=ALU.add), reads=[bF[TB]], writes=[bsmall])
                k.op("dve", lambda: nc.vector.tensor_scalar(out=small[:, 0:8], in0=small[:, 0:8], scalar1=1e-24,
                                                            scalar2=None, op0=ALU.max), reads=[bsmall], writes=[bsmall])
                k.op("act", lambda: nc.scalar.activation(out=small[:, 0:8], in_=small[:, 0:8], func=AF.Sqrt),
                     reads=[bsmall], writes=[bsmall])
                k.op("dve", lambda: nc.vector.reciprocal(out=small[:, 8:16], in_=small[:, 0:8]),
                     reads=[bsmall], writes=[bsmall])
                k.op("dve", lambda: nc.vector.tensor_tensor(out=v3(Fp[TA][:], NHG), in0=v3(Fp[TA][:], NHG),
                                                            in1=bc3(small[:, 8:16], NHG, 64), op=ALU.mult),
                     reads=[bF[TA], bsmall], writes=[bF[TA]])
                k.op("pool", lambda: nc.gpsimd.tensor_tensor(out=tok4[:, 0, :], in0=Fp[TA][:], in1=Fp[PPREV][:],
                                                             op=ALU.mult), reads=[bF[TA], bF[PPREV]], writes=[btok[0]])
                k.op("dve", lambda: nc.vector.scalar_tensor_tensor(out=Fp[TB][:], in0=Fp[TA][:], scalar=-1.0,
                                                                   in1=Fp[A_][:], op0=ALU.mult, op1=ALU.mult),
                     reads=[bF[TA], bF[A_]], writes=[bF[TB]])
                k.op("pool", lambda: nc.gpsimd.tensor_tensor(out=tok4[:, 3, :], in0=Fp[TB][:], in1=Fp[PINV][:],
                                                             op=ALU.mult), reads=[bF[TB], bF[PINV]], writes=[btok[3]])
                k.op("dve", lambda: nc.vector.scalar_tensor_tensor(out=Fp[TB][:], in0=Fp[A_][:], scalar=-1.0,
                                                                   in1=kab, op0=ALU.add, op1=ALU.mult),
                     reads=[bF[A_], bw], writes=[bF[TB]])
                k.op("dve", lambda: nc.vector.scalar_tensor_tensor(out=Fp[TB][:], in0=Fp[TB][:], scalar=1.0,
                                                                   in1=Fp[K_][:], op0=ALU.add, op1=ALU.mult),
                     reads=[bF[TB], bF[K_]], writes=[bF[TB]])
                k.op("pool", lambda: nc.gpsimd.tensor_tensor(out=tok4[:, 2, :], in0=Fp[TB][:], in1=Fp[PINV][:],
                                                             op=ALU.mult), reads=[bF[TB], bF[PINV]], writes=[btok[2]])
                k.op("dve", lambda: nc.vector.tensor_tensor(out=Fp[TA][:], in0=Fp[R_][:], in1=rkb, op=ALU.mult),
                     reads=[bF[R_], bw], writes=[bF[TA]])
                k.op("pool", lambda: nc.gpsimd.tensor_tensor(out=Fp[TA][:], in0=Fp[TA][:], in1=Fp[TB][:], op=ALU.mult),
                     reads=[bF[TA], bF[TB]], writes=[bF[TA]])
                k.op("dve", lambda: nc.vector.tensor_reduce(out=small[:, 16:24], in_=v3(Fp[TA][:], NHG), axis=AX.X,
                                                            op=ALU.add), reads=[bF[TA]], writes=[bsmall])
                mark(S, "P4")
                for j, (src_i, dst_ap) in enumerate(((0, 0), (1, 1))):
                    for fc in range(FC):
                        k.op("pe", lambda: nc.tensor.transpose(
                            out=S.pT[:, (fc * 2 + j) * 128:(fc * 2 + j + 1) * 128],
                            in_=tok4[:, src_i, fc * 128:(fc + 1) * 128], identity=S.ident[:]),
                            reads=[btok[src_i], S.bident], writes=[S.bpT])
                mark(S, "P5a")
                k.op("act", lambda: nc.scalar.copy(out=KRT[:].rearrange("p f j t -> p (f j t)"), in_=S.pT[:]),
                     reads=[S.bpT], writes=[bKRT])
                mark(S, "P5b")
                for j, src_i in enumerate((2, 3)):
                    for fc in range(FC):
                        k.op("pe", lambda: nc.tensor.transpose(
                            out=S.pT[:, (j * FC + fc) * 128:(j * FC + fc + 1) * 128],
                            in_=tok4[:, src_i, fc * 128:(fc + 1) * 128], identity=S.ident[:]),
                            reads=[btok[src_i], S.bident], writes=[S.bpT])
                mark(S, "P5c")
                k.op("act", lambda: nc.scalar.copy(out=KBT[:].rearrange("p j f t -> p (j f t)"), in_=S.pT[:]),
                     reads=[S.bpT], writes=[bKBT])
                mark(S, "P5")
                for h4 in range(2):
                    b = pbank()
                    for hh in range(4):
                        plo, fc = 64 * h4, hh
                        k.op("pe", lambda: nc.tensor.matmul(out=pb[b][:, hh * 128:(hh + 1) * 128],
                                                            lhsT=KRT[plo:plo + 64, fc, 0, :], rhs=KBT[plo:plo + 64, 1, fc, :],
                                                            start=True, stop=True, skip_group_check=True),
                             reads=[bKRT, bKBT], writes=[bpb[b]])
                    mark(S, "P6m")
                    k.op("dve", lambda: nc.vector.tensor_tensor(out=Pm[0][:, h4 * 4:(h4 + 1) * 4, :], in0=v3(pb[b][:], 4),
                                                                in1=maskL[:].unsqueeze(1).to_broadcast([128, 4, 128]),
                                                                op=ALU.mult), reads=[bpb[b], bconst], writes=[bPm[0]])
                mark(S, "P6a")
                for h2 in range(4):
                    b = pbank()
                    for hh in range(2):
                        s_ = h2 * 2 + hh
                        plo, fc = 64 * (s_ // 4), s_ % 4
                        k.op("pe", lambda: nc.tensor.matmul(out=pb[b][:, hh * 256:(hh + 1) * 256],
                                                            lhsT=KBT[plo:plo + 64, 1, fc, :], rhs=KRT[plo:plo + 64, fc, :, :],
                                                            start=True, stop=True, skip_group_check=True),
                             reads=[bKRT, bKBT], writes=[bpb[b]])
                    pv = pb[b][:].rearrange("p (h j t) -> p h j t", h=2, j=2)
                    k.op("dve", lambda: nc.vector.tensor_tensor(out=PTm[0][:, h2 * 2:(h2 + 1) * 2, :], in0=pv[:, :, 0, :],
                                                                in1=maskU[:].unsqueeze(1).to_broadcast([128, 2, 128]),
                                                                op=ALU.mult), reads=[bpb[b], bconst], writes=[bPTm[0]])
                    k.op("act", lambda: nc.scalar.copy(out=ArbT[:, h2 * 2:(h2 + 1) * 2, :], in_=pv[:, :, 1, :]),
                         reads=[bpb[b]], writes=[bArb])
                mark(S, "P6b")
                for h2 in range(4):
                    b = pbank()
                    for hh in range(2):
                        s_ = h2 * 2 + hh
                        plo, fc = 64 * (s_ // 4), s_ % 4
                        k.op("pe", lambda: nc.tensor.matmul(out=pb[b][:, hh * 256:(hh + 1) * 256],
                                                            lhsT=KBT[plo:plo + 64, 0, fc, :], rhs=KRT[plo:plo + 64, fc, :, :],
                                                            start=True, stop=True, skip_group_check=True),
                             reads=[bKRT, bKBT], writes=[bpb[b]])
                    pv = pb[b][:].rearrange("p (h j t) -> p h j t", h=2, j=2)
                    k.op("dve", lambda: nc.vector.tensor_tensor(out=AkkT[:, h2 * 2:(h2 + 1) * 2, :], in0=pv[:, :, 0, :],
                                                                in1=maskU[:].unsqueeze(1).to_broadcast([128, 2, 128]),
                                                                op=ALU.mult), reads=[bpb[b], bconst], writes=[bAkk])
                    k.op("act", lambda: nc.scalar.copy(out=ArkT[:, h2 * 2:(h2 + 1) * 2, :], in_=pv[:, :, 1, :]),
                         reads=[bpb[b]], writes=[bArk])
                mark(S, "P6c")
                k.op("pool", lambda: nc.gpsimd.tensor_tensor(out=ArbT[:], in0=ArbT[:],
                                                             in1=maskUi[:].unsqueeze(1).to_broadcast([128, NHG, 128]),
                                                             op=ALU.mult), reads=[bArb, bconst], writes=[bArb])
                k.op("pool", lambda: nc.gpsimd.tensor_tensor(out=ArkT[:], in0=ArkT[:],
                                                             in1=maskUi[:].unsqueeze(1).to_broadcast([128, NHG, 128]),
                                                             op=ALU.mult), reads=[bArk, bconst], writes=[bArk])
                mark(S, "P6")
                Dm, DTm, bD_, bDT_ = Pm[1], PTm[1], bPm[1], bPTm[1]
                k.op("pool", lambda: nc.gpsimd.tensor_tensor(out=Dm[:], in0=Pm[0][:],
                                                             in1=lmask[:, 0, 0, :].unsqueeze(1).to_broadcast([128, NHG, 128]),
                                                             op=ALU.mult), reads=[bPm[0], bconst], writes=[bD_])
                k.op("pool", lambda: nc.gpsimd.tensor_tensor(out=Dm[:], in0=Dm[:],
                                                             in1=S.ident[:].unsqueeze(1).to_broadcast([128, NHG, 128]),
                                                             op=ALU.add), reads=[bD_, S.bident], writes=[bD_])
                k.op("pool", lambda: nc.gpsimd.tensor_tensor(out=DTm[:], in0=PTm[0][:],
                                                             in1=lmask[:, 0, 1, :].unsqueeze(1).to_broadcast([128, NHG, 128]),
                                                             op=ALU.mult), reads=[bPTm[0], bconst], writes=[bDT_])
                k.op("pool", lambda: nc.gpsimd.tensor_tensor(out=DTm[:], in0=DTm[:],
                                                             in1=S.ident[:].unsqueeze(1).to_broadcast([128, NHG, 128]),
                                                             op=ALU.add), reads=[bDT_, S.bident], writes=[bDT_])
                for l in range(2, NLEVS + 1):
                    last = (l == NLEVS)
                    for h4 in range(2):
                        if not last:
                            b1 = pbank()
                            for hh in range(4):
                                h = h4 * 4 + hh
                                k.op("pe", lambda: nc.tensor.matmul(out=pb[b1][:, hh * 128:(hh + 1) * 128],
                                                                    lhsT=PTm[0][:, h, :], rhs=Dm[:, h, :],
                                                                    start=True, stop=True, skip_group_check=True),
                                     reads=[bPTm[0], bD_], writes=[bpb[b1]])
                            k.op("dve", lambda: nc.vector.tensor_tensor(
                                out=Ym[0][:, h4 * 4:(h4 + 1) * 4, :], in0=v3(pb[b1][:], 4),
                                in1=lmask[:, l - 1, 0, :].unsqueeze(1).to_broadcast([128, 4, 128]), op=ALU.mult),
                                reads=[bpb[b1], bconst], writes=[bYm[0]])
                        b2 = pbank()
                        for hh in range(4):
                            h = h4 * 4 + hh
                            k.op("pe", lambda: nc.tensor.matmul(out=pb[b2][:, hh * 128:(hh + 1) * 128],
                                                                lhsT=Pm[0][:, h, :], rhs=DTm[:, h, :],
                                                                start=True, stop=True, skip_group_check=True),
                                 reads=[bPm[0], bDT_], writes=[bpb[b2]])
                        k.op("dve", lambda: nc.vector.tensor_tensor(
                            out=Ym[1][:, h4 * 4:(h4 + 1) * 4, :], in0=v3(pb[b2][:], 4),
                            in1=lmask[:, l - 1, 1, :].unsqueeze(1).to_broadcast([128, 4, 128]), op=ALU.mult),
                            reads=[bpb[b2], bconst], writes=[bYm[1]])
                    zb = []
                    for h4 in range(2):
                        if not last:
                            b1 = pbank()
                            for hh in range(4):
                                h = h4 * 4 + hh
                                k.op("pe", lambda: nc.tensor.matmul(out=pb[b1][:, hh * 128:(hh + 1) * 128],
                                                                    lhsT=DTm[:, h, :], rhs=Ym[0][:, h, :],
                                                                    start=True, stop=True, skip_group_check=True),
                                     reads=[bDT_, bYm[0]], writes=[bpb[b1]])
                            zb.append((0, h4, b1))
                        b2 = pbank()
                        for hh in range(4):
                            h = h4 * 4 + hh
                            k.op("pe", lambda: nc.tensor.matmul(out=pb[b2][:, hh * 128:(hh + 1) * 128],
                                                                lhsT=Dm[:, h, :], rhs=Ym[1][:, h, :],
                                                                start=True, stop=True, skip_group_check=True),
                                 reads=[bD_, bYm[1]], writes=[bpb[b2]])
                        zb.append((1, h4, b2))
                        if len(zb) >= 2 or last:
                            for (which, hq, bb) in zb:
                                tgt, btgt = (Dm, bD_) if which == 0 else (DTm, bDT_)
                                k.op("dve", lambda: nc.vector.tensor_tensor(
                                    out=tgt[:, hq * 4:(hq + 1) * 4, :], in0=v3(pb[bb][:], 4),
                                    in1=tgt[:, hq * 4:(hq + 1) * 4, :], op=ALU.add),
                                    reads=[bpb[bb], btgt], writes=[btgt])
                            zb = []
                mark(S, "P7")
                bZ, bU, bD, bY = 3, 4, 5, 6
                for s_ in range(NHG):
                    plo, fc = 64 * (s_ // 4), s_ % 4
                    h = 2 * fc + s_ // 4
                    k.op("pe", lambda: nc.tensor.matmul(out=pb[bZ][:, h * 64:(h + 1) * 64], lhsT=KRT[plo:plo + 64, fc, 0, :],
                                                        rhs=Hb[plo:plo + 64, fc * 64:(fc + 1) * 64],
                                                        start=(s_ == 0), stop=False, skip_group_check=True),
                         reads=[bKRT, bHb], writes=[bpb[bZ]])
                    k.op("pe", lambda: nc.tensor.matmul(out=pb[bZ][:, h * 64:(h + 1) * 64], lhsT=AkkT[:, s_, :],
                                                        rhs=Vb[:, h * 64:(h + 1) * 64],
                                                        start=False, stop=True, skip_group_check=True),
                         reads=[bAkk, bVb], writes=[bpb[bZ]])
                k.op("act", lambda: nc.scalar.copy(out=Zs[:], in_=pb[bZ][:]), reads=[bpb[bZ]], writes=[bZs])
                for s_ in range(NHG):
                    h = 2 * (s_ % 4) + s_ // 4
                    k.op("pe", lambda: nc.tensor.matmul(out=pb[bU][:, h * 64:(h + 1) * 64], lhsT=QT[:, s_, :],
                                                        rhs=Zs[:, h * 64:(h + 1) * 64],
                                                        start=(s_ == 0), stop=True, skip_group_check=True),
                         reads=[bQT, bZs], writes=[bpb[bU]])
                k.op("act", lambda: nc.scalar.copy(out=Us[:], in_=pb[bU][:]), reads=[bpb[bU]], writes=[bUs])
                for s_ in range(NHG):
                    plo, fc = 64 * (s_ // 4), s_ % 4
                    h = 2 * fc + s_ // 4
                    k.op("pe", lambda: nc.tensor.matmul(out=pb[bY][:, h * 64:(h + 1) * 64], lhsT=KRT[plo:plo + 64, fc, 1, :],
                                                        rhs=Hb[plo:plo + 64, fc * 64:(fc + 1) * 64],
                                                        start=(s_ == 0), stop=False, skip_group_check=True),
                         reads=[bKRT, bHb], writes=[bpb[bY]])
                    k.op("pe", lambda: nc.tensor.matmul(out=pb[bY][:, h * 64:(h + 1) * 64], lhsT=ArkT[:, s_, :],
                                                        rhs=Vb[:, h * 64:(h + 1) * 64],
                                                        start=False, stop=False, skip_group_check=True),
                         reads=[bArk, bVb], writes=[bpb[bY]])
                    k.op("pe", lambda: nc.tensor.matmul(out=pb[bY][:, h * 64:(h + 1) * 64], lhsT=ArbT[:, s_, :],
                                                        rhs=Us[:, h * 64:(h + 1) * 64],
                                                        start=False, stop=True, skip_group_check=True),
                         reads=[bArb, bUs], writes=[bpb[bY]])
                for s_ in range(NHG):
                    plo, fc = 64 * (s_ // 4), s_ % 4
                    h = 2 * fc + s_ // 4
                    bDD = bD if s_ < 4 else bZ
                    k.op("pe", lambda: nc.tensor.matmul(out=pb[bDD][plo:plo + 64, fc * 64:(fc + 1) * 64],
                                                        lhsT=tok4[:, 2, h * 64:(h + 1) * 64], rhs=Vb[:, h * 64:(h + 1) * 64],
                                                        start=(s_ % 4 == 0), stop=False, skip_group_check=True),
                         reads=[btok[2], bVb], writes=[bpb[bDD]])
                    k.op("pe", lambda: nc.tensor.matmul(out=pb[bDD][plo:plo + 64, fc * 64:(fc + 1) * 64],
                                                        lhsT=tok4[:, 3, h * 64:(h + 1) * 64], rhs=Us[:, h * 64:(h + 1) * 64],
                                                        start=False, stop=True, skip_group_check=True),
                         reads=[btok[3], bUs], writes=[bpb[bDD]])
                k.op("dve", lambda: nc.vector.tensor_tensor(out=Hf[0:64, :], in0=pb[bD][0:64, 0:FC * 64], in1=Hf[0:64, :],
                                                            op=ALU.add), reads=[bpb[bD], bHf], writes=[bHf])
                k.op("dve", lambda: nc.vector.tensor_tensor(out=Hf[64:128, :], in0=pb[bZ][64:128, 0:FC * 64],
                                                            in1=Hf[64:128, :], op=ALU.add),
                     reads=[bpb[bZ], bHf], writes=[bHf])
                k.op("dve", lambda: nc.vector.tensor_tensor(out=v3(Hf[:], FC), in0=v3(Hf[:], FC), in1=bc3(pC[:], FC, 64),
                                                            op=ALU.mult), reads=[bHf, bpC], writes=[bHf])
                k.op("act", lambda: nc.scalar.copy(out=Hb[:], in_=Hf[:]), reads=[bHf], writes=[bHb])
                mark(S, "scan")
                YS, YQ = PP, PINV
                k.op("act", lambda: nc.scalar.copy(out=Fp[YS][:], in_=pb[bY][:]), reads=[bpb[bY]], writes=[bF[YS]])
                k.op("dve", lambda: nc.vector.tensor_reduce(out=small[:, 24:32], in_=v3(Fp[YS][:], NHG), axis=AX.X,
                                                            op=ALU.add), reads=[bF[YS]], writes=[bsmall])
                k.op("pool", lambda: nc.gpsimd.tensor_tensor(out=Fp[YQ][:], in0=Fp[YS][:], in1=Fp[YS][:], op=ALU.mult),
                     reads=[bF[YS]], writes=[bF[YQ]])
                k.op("dve", lambda: nc.vector.tensor_reduce(out=small[:, 32:40], in_=v3(Fp[YQ][:], NHG), axis=AX.X,
                                                            op=ALU.add), reads=[bF[YQ]], writes=[bsmall])
                k.op("dve", lambda: nc.vector.tensor_scalar(out=small[:, 24:32], in0=small[:, 24:32], scalar1=1.0 / 64,
                                                            scalar2=None, op0=ALU.mult), reads=[bsmall], writes=[bsmall])
                k.op("dve", lambda: nc.vector.tensor_tensor(out=small[:, 40:48], in0=small[:, 24:32], in1=small[:, 24:32],
                                                            op=ALU.mult), reads=[bsmall], writes=[bsmall])
                k.op("dve", lambda: nc.vector.scalar_tensor_tensor(out=small[:, 32:40], in0=small[:, 32:40], scalar=1.0 / 64,
                                                                   in1=small[:, 40:48], op0=ALU.mult, op1=ALU.subtract),
                     reads=[bsmall], writes=[bsmall])
                k.op("act", lambda: nc.scalar.activation(out=small[:, 32:40], in_=small[:, 32:40], func=AF.Sqrt,
                                                         bias=LNX_EPS, scale=1.0), reads=[bsmall], writes=[bsmall])
                k.op("dve", lambda: nc.vector.reciprocal(out=small[:, 40:48], in_=small[:, 32:40]),
                     reads=[bsmall], writes=[bsmall])
                k.op("dve", lambda: nc.vector.tensor_tensor(out=v3(Fp[YS][:], NHG), in0=v3(Fp[YS][:], NHG),
                                                            in1=bc3(small[:, 24:32], NHG, 64), op=ALU.subtract),
                     reads=[bF[YS], bsmall], writes=[bF[YS]])
                k.op("dve", lambda: nc.vector.tensor_tensor(out=v3(Fp[YS][:], NHG), in0=v3(Fp[YS][:], NHG),
                                                            in1=bc3(small[:, 40:48], NHG, 64), op=ALU.mult),
                     reads=[bF[YS], bsmall], writes=[bF[YS]])
                k.op("pool", lambda: nc.gpsimd.tensor_tensor(out=Fp[YS][:], in0=Fp[YS][:], in1=bcv[:, 3, :],
                                                             op=ALU.mult), reads=[bF[YS], bw], writes=[bF[YS]])
                k.op("pool", lambda: nc.gpsimd.tensor_tensor(out=Fp[YS][:], in0=Fp[YS][:], in1=bcv[:, 4, :],
                                                             op=ALU.add), reads=[bF[YS], bw], writes=[bF[YS]])
                k.op("dve", lambda: nc.vector.tensor_tensor(out=v3(Fp[YQ][:], NHG), in0=v3(Vb[:], NHG),
                                                            in1=bc3(small[:, 16:24], NHG, 64), op=ALU.mult),
                     reads=[bVb, bsmall], writes=[bF[YQ]])
                k.op("pool", lambda: nc.gpsimd.tensor_tensor(out=Fp[YS][:], in0=Fp[YS][:], in1=Fp[YQ][:], op=ALU.add),
                     reads=[bF[YS], bF[YQ]], writes=[bF[YS]])
                if hg < G - 1:
                    k.op("dve", lambda: nc.vector.tensor_tensor(out=yo0[:, n, :], in0=Fp[YS][:], in1=Fp[GG][:], op=ALU.mult),
                         reads=[bF[YS], bF[GG]], writes=[byo0[n]])
                else:
                    k.op("dve", lambda: nc.vector.tensor_tensor(out=yo[:], in0=Fp[YS][:], in1=Fp[GG][:], op=ALU.mult),
                         reads=[bF[YS], bF[GG]], writes=[byo])
                    for fc in range(FC):
                        k.op("pe", lambda: nc.tensor.transpose(out=S.pT[:, fc * 128:(fc + 1) * 128],
                                                               in_=yo0[:, n, fc * 128:(fc + 1) * 128], identity=S.ident[:]),
                             reads=[byo0[n], S.bident], writes=[S.bpT])
                    for fc in range(FC):
                        k.op("pe", lambda: nc.tensor.transpose(out=S.pT[:, (FC + fc) * 128:(FC + fc + 1) * 128],
                                                               in_=yo[:, fc * 128:(fc + 1) * 128], identity=S.ident[:]),
                             reads=[byo, S.bident], writes=[S.bpT])
                    yoT = KBT[:].rearrange("p j f t -> p (j f) t")
                    byoT = bKBT
                    k.op("act", lambda: nc.scalar.copy(out=KBT[:].rearrange("p j f t -> p (j f t)"), in_=S.pT[:]),
                         reads=[S.bpT], writes=[byoT])
                    for half in range(2):
                        b = pbank()
                        for c in range(DC):
                            k.op("pe", lambda: nc.tensor.matmul(out=pb[b][:], lhsT=yoT[:, c, :],
                                                                rhs=wo[:, c, half * 512:(half + 1) * 512],
                                                                start=(c == 0), stop=(c == DC - 1)),
                                 reads=[byoT, bwo], writes=[bpb[b]])
                        xsl = S.X[:, n, half * 512:(half + 1) * 512]
                        k.op("dve", lambda: nc.vector.tensor_tensor(out=xsl, in0=pb[b][:], in1=xsl, op=ALU.add),
                             reads=[bpb[b], S.bX[n]], writes=[S.bX[n]])
        k.barrier()


LAST_K = None
_NC_CACHE = {}


def kernel(**inputs):
    x = np.asarray(inputs["x"], dtype=np.float32)
    B, L, _ = x.shape
    if L not in _NC_CACHE:
        _NC_CACHE[L] = build(L)
    nc = _NC_CACHE[L]
    params = {n: np.ascontiguousarray(np.asarray(inputs[n], dtype=np.float32))
              for n in PARAM_SHAPES}
    in_maps = []
    for b in range(B):
        m = {"x": np.ascontiguousarray(x[b])}
        m.update(params)
        in_maps.append(m)
    res = run_bass_kernel_spmd(nc, in_maps, core_ids=list(range(B)))
    return np.stack([res.results[b]["out"] for b in range(B)], axis=0)
```

```python
import math
import numpy as np
from contextlib import ExitStack
import concourse.bass as bass
import concourse.mybir as mybir
from concourse.bass_utils import run_bass_kernel_spmd

F32 = mybir.dt.float32
BF16 = mybir.dt.bfloat16
I32 = mybir.dt.int32
AF = mybir.ActivationFunctionType
ALU = mybir.AluOpType
AX = mybir.AxisListType

D = 1024
DC = 8
DFF = 4096
NCORES = 8
HEADS = 16
KVH = 4
IDXH = 8
IN_COLS = 2120
NORM_EPS = 1e-6
LNX_EPS = 64e-5
NEG = -30000.0


class Buf:
    __slots__ = ("w", "r", "name", "excl")

    def __init__(self, name="", excl=False):
        self.w = None
        self.r = {}
        self.name = name
        self.excl = excl


class K:
    N_DMA_SEMS = 24

    def __init__(self, nc, es):
        self.nc = nc
        self.es = es
        self.eng = {"pe": nc.tensor, "act": nc.scalar, "dve": nc.vector,
                    "pool": nc.gpsimd, "sp": nc.sync}
        self.sem = {}
        self.cnt = {}
        self.seen = {}
        self.hist = {}
        for e in self.eng:
            self.sem[e] = es.enter_context(nc.semaphore("s_" + e))
            self.cnt[e] = 0
            self.seen[e] = {}
            self.hist[e] = {}
        self.dsem = {"hw": [], "sw": []}
        for kind in ("hw", "sw"):
            for i in range(self.N_DMA_SEMS // 2):
                key = "d%s%d" % (kind, i)
                self.sem[key] = es.enter_context(nc.semaphore("s_" + key))
                self.cnt[key] = 0
                self.hist[key] = {}
                self.dsem[kind].append(key)
        self.dnext = {"hw": 0, "sw": 0}
        self.dead = False
        self.uid = 0
        self.names = []
        self.n_instr = 0
        self.n_wait = 0
        self.per_eng = {e: 0 for e in self.eng}

    def sb(self, name, shape, dtype, es=None):
        self.uid += 1
        self.names.append("%s_%d" % (name, self.uid))
        return (es or self.es).enter_context(self.nc.sbuf_tensor("%s_%d" % (name, self.uid), list(shape), dtype))

    def ps(self, name, shape, dtype, es=None):
        self.uid += 1
        self.names.append("%s_%d" % (name, self.uid))
        return (es or self.es).enter_context(self.nc.psum_tensor("%s_%d" % (name, self.uid), list(shape), dtype))

    def _need(self, e, deps):
        eng = self.eng[e]
        seen = self.seen[e]
        for (sk, v) in deps:
            if seen.get(sk, 0) >= v:
                continue
            eng.wait_ge(self.sem[sk], v)
            self.n_wait += 1
            seen[sk] = v
            h = self.hist[sk].get(v)
            if h:
                for k2, v2 in h.items():
                    if seen.get(k2, 0) < v2:
                        seen[k2] = v2

    def _deps(self, e, reads, writes):
        deps = []
        for b in reads:
            if b.w is not None:
                if b.w[0] == e and e == "pe":
                    continue
                deps.append(b.w)
        for b in writes:
            if b.w is not None and b.w[0] != e:
                deps.append(b.w)
            for sk, v in b.r.items():
                if sk != e or e != "pe":
                    deps.append((sk, v))
        return deps

    def op(self, e, fn, reads=(), writes=()):
        if self.dead:
            return None
        ex = [b for b in reads if b.excl]
        if ex:
            reads = [b for b in reads if not b.excl]
            writes = list(writes) + ex
        self._need(e, self._deps(e, reads, writes))
        ins = fn()
        self.cnt[e] += 1
        c = self.cnt[e]
        ins.then_inc(self.sem[e], 1)
        self.hist[e][c] = dict(self.seen[e])
        for b in reads:
            if b.r.get(e, 0) < c:
                b.r[e] = c
        for b in writes:
            b.w = (e, c)
            b.r = {}
        self.n_instr += 1
        self.per_eng[e] += 1
        return ins

    def dma(self, e, out, in_, reads=(), writes=(), **kw):
        if self.dead:
            return None
        kind = "sw" if e == "pool" else "hw"
        sk = self.dsem[kind][self.dnext[kind]]
        self.dnext[kind] = (self.dnext[kind] + 1) % len(self.dsem[kind])
        deps = self._deps(sk, reads, writes)
        if self.cnt[sk] > 0:
            deps.append((sk, self.cnt[sk]))
        self._need(e, deps)
        ins = self.eng[e].dma_start(out=out, in_=in_, **kw)
        self.cnt[sk] += 16
        c = self.cnt[sk]
        ins.then_inc(self.sem[sk], 16)
        self.hist[sk][c] = dict(self.seen[e])
        for b in reads:
            if b.r.get(sk, 0) < c:
                b.r[sk] = c
        for b in writes:
            b.w = (sk, c)
            b.r = {}
        self.n_instr += 1
        return ins

    def barrier(self):
        if self.dead:
            return
        allk = [(sk, v) for sk, v in self.cnt.items() if v > 0]
        for e in self.eng:
            self._need(e, [d for d in allk if d[0] != e])

    def finish(self, bufs, e="sp"):
        self._need(e, [b.w for b in bufs if b.w is not None])


class Rot:
    def __init__(self, k, name, n, shape, dtype, es=None, psum=False):
        self.items = []
        for i in range(n):
            t = (k.ps if psum else k.sb)("%s%d" % (name, i), shape, dtype, es)
            self.items.append((t, Buf(name)))
        self.i = 0

    def next(self):
        it = self.items[self.i]
        self.i = (self.i + 1) % len(self.items)
        return it


class NS:
    pass


class StopBuild(Exception):
    pass


def mark(S, name):
    if getattr(S, "stop_at", None) == name:
        S.k.dead = True


def setup_common(k, nc, S, L):
    S.L = L
    S.NT = L // 128
    S.X = k.sb("X", [128, S.NT, D], F32)
    S.bX = [Buf("X%d" % i) for i in range(S.NT)]
    S.ident = k.sb("ident", [128, 128], BF16)
    S.bident = Buf("ident")
    k.op("pool", lambda: nc.gpsimd.memset(S.ident[:], 1.0), writes=[S.bident])
    k.op("pool", lambda: nc.gpsimd.affine_select(
        out=S.ident[:], in_=S.ident[:], pattern=[[-1, 128]], compare_op=ALU.is_equal,
        fill=0.0, base=0, channel_multiplier=1), reads=[S.bident], writes=[S.bident])
    S.pb = [k.ps("pb%d" % i, [128, 512], F32) for i in range(7)]
    S.bpb = [Buf("pb%d" % i, excl=True) for i in range(7)]
    S.pT = k.ps("pT", [128, 1024], BF16)
    S.bpT = Buf("pT", excl=True)
    S.ss = Rot(k, "ss", 4, [128, 1], F32)
    S.rs = Rot(k, "rs", 4, [128, 1], F32)
    S.junk = k.sb("junk", [128, 1024], BF16)
    S.bjunk = Buf("junk")
    S.hb = Rot(k, "hb", 1, [128, 1024], BF16)
    S.gbc = Rot(k, "gbc", 1, [128, D], F32)


def load_gbc(k, nc, S, g_ap):
    t, b = S.gbc.next()
    k.dma("sp", t[:], g_ap.partition_broadcast(128), writes=[b])
    return t, b


def rstd_tile(k, nc, S, i):
    ss, bss = S.ss.next()
    rs, brs = S.rs.next()
    k.op("act", lambda: nc.scalar.activation(out=S.junk[:], in_=S.X[:, i, :], func=AF.Square,
                                             accum_out=ss[:]),
         reads=[S.bX[i]], writes=[S.bjunk, bss])
    k.op("act", lambda: nc.scalar.activation(out=ss[:], in_=ss[:], func=AF.Sqrt,
                                             scale=1.0 / D, bias=NORM_EPS),
         reads=[bss], writes=[bss])
    k.op("dve", lambda: nc.vector.reciprocal(out=rs[:], in_=ss[:]), reads=[bss], writes=[brs])
    return rs, brs


def norm_to_hT(k, nc, S, i, gbc, bgbc, dst_ap, bdst, evac_eng="act"):
    rs, brs = rstd_tile(k, nc, S, i)
    hb, bhb = S.hb.next()
    k.op("dve", lambda: nc.vector.scalar_tensor_tensor(
        out=hb[:], in0=S.X[:, i, :], scalar=rs[:], in1=gbc[:], op0=ALU.mult, op1=ALU.mult),
        reads=[S.bX[i], brs, bgbc], writes=[bhb])
    for c in range(DC):
        k.op("pe", lambda: nc.tensor.transpose(out=S.pT[:, c * 128:(c + 1) * 128],
                                               in_=hb[:, c * 128:(c + 1) * 128],
                                               identity=S.ident[:]),
             reads=[bhb, S.bident], writes=[S.bpT])
    src = S.pT[:].rearrange("p (c t) -> p c t", c=DC)
    if evac_eng == "act":
        k.op("act", lambda: nc.scalar.copy(out=dst_ap, in_=src), reads=[S.bpT], writes=[bdst])
    else:
        k.op("dve", lambda: nc.vector.tensor_copy(out=dst_ap, in_=src), reads=[S.bpT],
             writes=[bdst])


def phase_mlp(k, nc, S, g_ap, wup_ap, wdn_ap):
    L, NT = S.L, S.NT
    NTB = L // 512
    FP = 512
    NFP = DFF // FP
    FCP = FP // 128
    with ExitStack() as es:
        hT = k.sb("mlp_hT", [128, DC, L], BF16, es)
        bhT = [Buf("hT%d" % i) for i in range(NT)]
        uT = [k.sb("mlp_uT%d" % s, [128, FCP, L], BF16, es) for s in range(2)]
        buT = [[Buf("uT") for _ in range(NTB)] for s in range(2)]
        wup = [k.sb("mlp_wup%d" % s, [128, DC, FP], BF16, es) for s in range(2)]
        bwup = [Buf("wup") for s in range(2)]
        wdn = [k.sb("mlp_wdn%d" % s, [128, FCP, D], BF16, es) for s in range(2)]
        bwdn = [Buf("wdn") for s in range(2)]
        rtmp = Rot(k, "mlp_rt", 2, [128, 512], F32, es)

        def load_w(fp):
            s = fp % 2
            k.dma("pool", wup[s][:], wup_ap[:, fp * FP:(fp + 1) * FP].rearrange(
                "(c p) n -> p c n", p=128), writes=[bwup[s]])
            k.dma("pool", wdn[s][:], wdn_ap[fp * FP:(fp + 1) * FP, :].rearrange(
                "(c p) n -> p c n", p=128), writes=[bwdn[s]])

        load_w(0)
        load_w(1)
        gbc, bgbc = load_gbc(k, nc, S, g_ap)
        for i in range(NT):
            norm_to_hT(k, nc, S, i, gbc, bgbc, hT[:, :, i * 128:(i + 1) * 128], bhT[i],
                       evac_eng=("act" if i % 2 == 0 else "dve"))

        upb = [0, 1]
        dnb = [2, 3, 4, 5]
        cnt = {"u": 0, "d": 0}

        def up(fp):
            s = fp % 2
            for fc in range(FCP):
                for tb in range(NTB):
                    b = upb[cnt["u"] % 2]
                    cnt["u"] += 1
                    for dc in range(DC):
                        k.op("pe", lambda: nc.tensor.matmul(
                            out=S.pb[b][:], lhsT=wup[s][:, dc, fc * 128:(fc + 1) * 128],
                            rhs=hT[:, dc, tb * 512:(tb + 1) * 512],
                            start=(dc == 0), stop=(dc == DC - 1)),
                            reads=[bwup[s]] + bhT[tb * 4:(tb + 1) * 4], writes=[S.bpb[b]])
                    rt, brt = rtmp.next()
                    k.op("act", lambda: nc.scalar.activation(out=rt[:], in_=S.pb[b][:],
                                                             func=AF.Relu),
                         reads=[S.bpb[b]], writes=[brt])
                    k.op("dve", lambda: nc.vector.tensor_tensor(
                        out=uT[s][:, fc, tb * 512:(tb + 1) * 512], in0=rt[:], in1=rt[:],
                        op=ALU.mult), reads=[brt], writes=[buT[s][tb]])

        def down(fp):
            s = fp % 2
            for i in range(NT):
                for half in range(2):
                    b = dnb[cnt["d"] % 4]
                    cnt["d"] += 1
                    for fc in range(FCP):
                        k.op("pe", lambda: nc.tensor.matmul(
                            out=S.pb[b][:], lhsT=uT[s][:, fc, i * 128:(i + 1) * 128],
                            rhs=wdn[s][:, fc, half * 512:(half + 1) * 512],
                            start=(fc == 0), stop=(fc == FCP - 1)),
                            reads=[bwdn[s], buT[s][i // 4]], writes=[S.bpb[b]])
                    xs = S.X[:, i, half * 512:(half + 1) * 512]
                    k.op("dve", lambda: nc.vector.tensor_tensor(out=xs, in0=S.pb[b][:], in1=xs,
                                                                op=ALU.add),
                         reads=[S.bpb[b], S.bX[i]], writes=[S.bX[i]])
            if fp + 2 < NFP:
                load_w(fp + 2)

        up(0)
        for fp in range(NFP):
            if fp + 1 < NFP:
                up(fp + 1)
            down(fp)
        k.barrier()


def phase_final(k, nc, S, g_ap, out_ap, bout):
    with ExitStack() as es:
        ot = Rot(k, "fin_o", 2, [128, D], F32, es)
        gbc, bgbc = load_gbc(k, nc, S, g_ap)
        for i in range(S.NT):
            rs, brs = rstd_tile(k, nc, S, i)
            o, bo = ot.next()
            k.op("dve", lambda: nc.vector.scalar_tensor_tensor(
                out=o[:], in0=S.X[:, i, :], scalar=rs[:], in1=gbc[:], op0=ALU.mult,
                op1=ALU.mult), reads=[S.bX[i], brs, bgbc], writes=[bo])
            k.dma("sp", out_ap[i * 128:(i + 1) * 128, :], o[:], reads=[bo], writes=[bout])
        k.finish([bout], "sp")
        k.barrier()


PARAM_SHAPES = {
    "mixer_norm": [2, D], "mlp_norm": [2, D], "mlp_w_up": [2, D, DFF], "mlp_w_down": [2, DFF, D],
    "final_norm": [D], "dsa_w_in": [1, D, IN_COLS], "dsa_w_o": [1, D, D],
    "rwkv_mu": [1, 6, D], "rwkv_w_rkv": [1, 3, D, D], "rwkv_w0": [1, D], "rwkv_w1": [1, D, 64],
    "rwkv_w2": [1, 64, D], "rwkv_a0": [1, D], "rwkv_a1": [1, D, 64], "rwkv_a2": [1, 64, D],
    "rwkv_g1": [1, D, 160], "rwkv_g2": [1, 160, D], "rwkv_k_k": [1, D], "rwkv_k_a": [1, D],
    "rwkv_r_k": [1, 16, 64], "rwkv_lnx_w": [1, D], "rwkv_lnx_b": [1, D], "rwkv_w_o": [1, D, D],
}


def dbg(k, S, name, ap, reads):
    if name in S.dbg_t:
        t = S.dbg_t[name]
        b = Buf("dbg")
        k.dma("sp", t, ap, reads=reads, writes=[b])
        S.dbg_b.append(b)


def build(L=2048, phases=("dsa", "mlp0", "rwkv", "mlp1", "final"), debug=None, stop_at=None):
    nc = bass.Bass("TRN2", target_bir_lowering=False)
    P = NS()
    P.x = nc.dram_tensor("x", [L, D], F32, kind="ExternalInput").ap()
    for name, shp in PARAM_SHAPES.items():
        setattr(P, name, nc.dram_tensor(name, shp, F32, kind="ExternalInput").ap())
    P.out = nc.dram_tensor("out", [L, D], F32, kind="ExternalOutput").ap()
    with ExitStack() as es:
        k = K(nc, es)
        global LAST_K
        LAST_K = k
        S = NS()
        S.P = P
        S.dbg_t = {}
        S.stop_at = stop_at
        S.k = k
        S.dbg_b = []
        for name, (shp, dt_) in (debug or {}).items():
            S.dbg_t[name] = nc.dram_tensor("dbg_" + name, list(shp), dt_, kind="ExternalOutput").ap()
        setup_common(k, nc, S, L)
        for i in range(S.NT):
            k.dma("sp", S.X[:, i, :], P.x[i * 128:(i + 1) * 128, :], writes=[S.bX[i]])
        for ph in phases:
            if ph == "dsa":
                phase_dsa(k, nc, S)
            elif ph == "mlp0":
                phase_mlp(k, nc, S, P.mlp_norm[0], P.mlp_w_up[0], P.mlp_w_down[0])
            elif ph == "rwkv":
                phase_rwkv(k, nc, S)
                k.dead = False
                k.barrier()
            elif ph == "mlp1":
                phase_mlp(k, nc, S, P.mlp_norm[1], P.mlp_w_up[1], P.mlp_w_down[1])
            elif ph == "final":
                bout = Buf("out")
                phase_final(k, nc, S, P.final_norm, P.out, bout)
                k.finish([bout], "sp")
            elif ph == "dump":
                bout = Buf("out")
                for i in range(S.NT):
                    k.dma("sp", P.out[i * 128:(i + 1) * 128, :], S.X[:, i, :],
                          reads=[S.bX[i]], writes=[bout])
                k.finish([bout], "sp")
        k.finish(S.dbg_b, "sp")
        print("instr", k.n_instr, "waits", k.n_wait, k.per_eng)
    return nc


def phase_dsa(k, nc, S):
    L, NT, P = S.L, S.NT, S.P
    TOPK = min(256, L // 4)
    NIT = 12
    w_in = P.dsa_w_in[0]
    w_o = P.dsa_w_o[0]
    o0, o1, o2, o3, o4 = 1024, 1280, 1536, 2048, 2112
    NHALF = 2 if L >= 1024 else 1
    LH = L // NHALF
    NTH = LH // 128
    NTBH = LH // 512
    with ExitStack() as es:
        qT = k.sb("qT", [128, 8, L], BF16, es)
        bqT = [Buf("qT%d" % i) for i in range(NT)]
        kT = k.sb("kT", [128, 2, L], BF16, es)
        bkT = [Buf("kT%d" % i) for i in range(NT)]
        qiT = k.sb("qiT", [128, 4, L], BF16, es)
        bqiT = [Buf("qiT%d" % i) for i in range(NT)]
        kiT = k.sb("kiT", [128, 1, L], BF16, es)
        bkiT = [Buf("kiT%d" % i) for i in range(NT)]
        Vaug = k.sb("Vaug", [128, NT, KVH, 65], BF16, es)
        bV = [Buf("V%d" % i) for i in range(NT)]
        wi = k.sb("wi", [128, NT, 8], F32, es)
        bwi = [Buf("wi%d" % i) for i in range(NT)]
        ident4 = k.sb("ident4", [128, 4, 128], BF16, es)
        bident4 = Buf("ident4")
        pw2 = k.sb("pw2", [128, NIT + 2], F32, es)
        bpw2 = Buf("pw2")
        for g in range(4):
            k.op("pool", lambda: nc.gpsimd.tensor_copy(out=ident4[:, g, :], in_=S.ident[:]),
                 reads=[S.bident], writes=[bident4])
        for n in range(NIT + 2):
            k.op("pool", lambda: nc.gpsimd.memset(pw2[:, n:n + 1], 2.0 ** (-(n + 1))),
                 writes=[bpw2])
        k.op("pool", lambda: nc.gpsimd.memset(Vaug[:, :, :, 64:65], 1.0), writes=bV)

        with ExitStack() as es2:
            cosT = k.sb("cosT", [128, LH], F32, es2)
            sinT = k.sb("sinT", [128, LH], F32, es2)
            btab = Buf("tab")
            bhT = [Buf("hT%d" % i) for i in range(NTH)]
            wn = [k.sb("dsa_wn%d" % s, [128, DC, 2, 2, 64], BF16, es2) for s in range(2)]
            wr = [k.sb("dsa_wr%d" % s, [128, DC, 2, 2, 2, 32], BF16, es2) for s in range(2)]
            bwn = [Buf("wn") for s in range(2)]
            bwr = [Buf("wr") for s in range(2)]
            wv = k.sb("dsa_wv", [128, DC, 264], BF16, es2)
            bwv = Buf("wv")
            t1r = Rot(k, "rope_t1", 2, [128, 512], F32, es2)
            t2r = Rot(k, "rope_t2", 2, [128, 512], F32, es2)
            k.dma("pool", wv[:, :, 0:256], w_in[:, o1:o2].rearrange("(c p) n -> p c n", p=128),
                  writes=[bwv])
            k.dma("pool", wv[:, :, 256:264], w_in[:, o4:o4 + 8].rearrange("(c p) n -> p c n", p=128),
                  writes=[bwv])
            gbc, bgbc = load_gbc(k, nc, S, P.mixer_norm[0])

            with ExitStack() as es3:
                pidx = k.sb("pidx", [128, 1], I32, es3)
                pj = k.sb("pj", [128, 1], I32, es3)
                pjf = k.sb("pjf", [128, 1], F32, es3)
                inv = k.sb("rinv", [128, 1], F32, es3)
                sgn = k.sb("rsgn", [128, 1], F32, es3)
                bc = Buf("ropec")
                k.op("pool", lambda: nc.gpsimd.iota(out=pidx[:], pattern=[[0, 1]], base=0,
                                                    channel_multiplier=1), writes=[bc])
                k.op("dve", lambda: nc.vector.tensor_single_scalar(out=pj[:], in_=pidx[:], scalar=31,
                                                                   op=ALU.bitwise_and),
                     reads=[bc], writes=[bc])
                k.op("dve", lambda: nc.vector.tensor_copy(out=pjf[:], in_=pj[:]), reads=[bc], writes=[bc])
                k.op("act", lambda: nc.scalar.activation(out=inv[:], in_=pjf[:], func=AF.Exp,
                                                         scale=-2.0 * math.log(10000.0) / 64.0),
                     reads=[bc], writes=[bc])
                k.op("dve", lambda: nc.vector.tensor_single_scalar(out=pj[:], in_=pidx[:], scalar=32,
                                                                   op=ALU.bitwise_and),
                     reads=[bc], writes=[bc])
                k.op("dve", lambda: nc.vector.tensor_copy(out=pjf[:], in_=pj[:]), reads=[bc], writes=[bc])
                k.op("dve", lambda: nc.vector.tensor_scalar(out=sgn[:], in0=pjf[:], scalar1=2.0 / 32.0,
                                                            scalar2=-1.0, op0=ALU.mult, op1=ALU.add),
                     reads=[bc], writes=[bc])

                def make_tables(hf):
                    with ExitStack() as es4:
                        ti = k.sb("tpos_i", [128, LH], I32, es4)
                        ang = k.sb("ang", [128, LH], F32, es4)
                        rr = k.sb("rr", [128, LH], F32, es4)
                        ki_ = ti
                        mm = k.sb("mm", [128, LH], F32, es4)
                        bt = Buf("tabtmp")
                        TWO_PI = 2.0 * math.pi
                        k.op("pool", lambda: nc.gpsimd.iota(out=ti[:], pattern=[[1, LH]], base=hf * LH,
                                                            channel_multiplier=0), writes=[bt])
                        k.op("dve", lambda: nc.vector.tensor_copy(out=ang[:], in_=ti[:]), reads=[bt], writes=[bt])
                        k.op("dve", lambda: nc.vector.tensor_scalar(out=ang[:], in0=ang[:], scalar1=inv[:],
                                                                    scalar2=None, op0=ALU.mult),
                             reads=[bt, bc], writes=[bt])
                        for which, dst in (("sin", sinT), ("cos", cosT)):
                            off = 0.0 if which == "sin" else math.pi / 2
                            k.op("dve", lambda: nc.vector.tensor_scalar(
                                out=ki_[:], in0=ang[:], scalar1=off, scalar2=1.0 / TWO_PI,
                                op0=ALU.add, op1=ALU.mult), reads=[bt], writes=[bt])
                            k.op("dve", lambda: nc.vector.tensor_copy(out=mm[:], in_=ki_[:]),
                                 reads=[bt], writes=[bt])
                            k.op("dve", lambda: nc.vector.scalar_tensor_tensor(
                                out=rr[:], in0=mm[:], scalar=-TWO_PI, in1=ang[:], op0=ALU.mult,
                                op1=ALU.add), reads=[bt], writes=[bt])
                            if off != 0.0:
                                k.op("dve", lambda: nc.vector.tensor_scalar(
                                    out=rr[:], in0=rr[:], scalar1=off, scalar2=None, op0=ALU.add),
                                    reads=[bt], writes=[bt])
                            k.op("dve", lambda: nc.vector.tensor_scalar(
                                out=mm[:], in0=rr[:], scalar1=math.pi, scalar2=-TWO_PI,
                                op0=ALU.is_gt, op1=ALU.mult), reads=[bt], writes=[bt])
                            k.op("dve", lambda: nc.vector.tensor_tensor(out=rr[:], in0=rr[:], in1=mm[:],
                                                                        op=ALU.add), reads=[bt], writes=[bt])
                            k.op("dve", lambda: nc.vector.tensor_scalar(
                                out=mm[:], in0=rr[:], scalar1=-math.pi, scalar2=TWO_PI,
                                op0=ALU.is_lt, op1=ALU.mult), reads=[bt], writes=[bt])
                            k.op("dve", lambda: nc.vector.tensor_tensor(out=rr[:], in0=rr[:], in1=mm[:],
                                                                        op=ALU.add), reads=[bt], writes=[bt])
                            k.op("dve", lambda: nc.vector.tensor_scalar(
                                out=rr[:], in0=rr[:], scalar1=-3.14159, scalar2=3.14159,
                                op0=ALU.max, op1=ALU.min), reads=[bt], writes=[bt])
                            k.op("act", lambda: nc.scalar.activation(out=dst[:], in_=rr[:], func=AF.Sin),
                                 reads=[bt], writes=[btab, bt])
                        k.op("dve", lambda: nc.vector.tensor_scalar(out=sinT[:], in0=sinT[:], scalar1=sgn[:],
                                                                    scalar2=None, op0=ALU.mult),
                             reads=[btab, bc], writes=[btab])
                        k.barrier()

                specs = []
                for c in range(8):
                    specs.append((qT, c, bqT, c * 64, (c + 8) * 64))
                for c in range(2):
                    specs.append((kT, c, bkT, o0 + c * 64, o0 + (c + 2) * 64))
                for c in range(4):
                    specs.append((qiT, c, bqiT, o2 + c * 64, o2 + (c + 4) * 64))
                specs.append((kiT, 0, bkiT, o3, o3))
                blocks = [specs[i:i + 2] for i in range(0, len(specs), 2)]

                def load_block(bi):
                    s = bi % 2
                    for cc, (_, _, _, lo, up) in enumerate(blocks[bi]):
                        for h, col in enumerate((lo, up)):
                            k.dma("pool", wn[s][:, :, cc, h, :],
                                  w_in[:, col:col + 64].rearrange("(c p) n -> p c n", p=128),
                                  writes=[bwn[s]])
                            for r in range(2):
                                k.dma("pool", wr[s][:, :, cc, h, r, :],
                                      w_in[:, col + (1 - r) * 32:col + (1 - r) * 32 + 32].rearrange(
                                          "(c p) n -> p c n", p=128), writes=[bwr[s]])

                cntp = {"a": 0}
                for hf in range(NHALF):
                    make_tables(hf)
                    es5 = ExitStack()
                    hT = k.sb("dsa_hT", [128, DC, LH], BF16, es5)
                    load_block(0)
                    load_block(1)
                    for it in range(NTH):
                        gi = hf * NTH + it
                        norm_to_hT(k, nc, S, gi, gbc, bgbc, hT[:, :, it * 128:(it + 1) * 128], bhT[it],
                                   evac_eng=("act" if it % 2 == 0 else "dve"))
                    for it in range(NTH):
                        gi = hf * NTH + it
                        for dc in range(DC):
                            k.op("pe", lambda: nc.tensor.matmul(
                                out=S.pb[4][:, 0:264], lhsT=hT[:, dc, it * 128:(it + 1) * 128],
                                rhs=wv[:, dc, :], start=(dc == 0), stop=(dc == DC - 1)),
                                reads=[bhT[it], bwv], writes=[S.bpb[4]])
                        k.op("act", lambda: nc.scalar.copy(
                            out=Vaug[:, gi, :, 0:64],
                            in_=S.pb[4][:, 0:256].rearrange("p (n e) -> p n e", n=KVH)),
                            reads=[S.bpb[4]], writes=[bV[gi]])
                        k.op("act", lambda: nc.scalar.mul(out=wi[:, gi, :], in_=S.pb[4][:, 256:264],
                                                          mul=(8.0 ** -0.5) * (64.0 ** -0.5)),
                             reads=[S.bpb[4]], writes=[bwi[gi]])
                    for bi, blk in enumerate(blocks):
                        s = bi % 2
                        for cc, (dst, dci, bdst, lo, up) in enumerate(blk):
                            for tb in range(NTBH):
                                ba = cntp["a"] % 2
                                bb = 2 + cntp["a"] % 2
                                cntp["a"] += 1
                                for dc in range(DC):
                                    k.op("pe", lambda: nc.tensor.matmul(
                                        out=S.pb[ba][:], lhsT=wn[s][:, dc, cc, :, :],
                                        rhs=hT[:, dc, tb * 512:(tb + 1) * 512],
                                        start=(dc == 0), stop=(dc == DC - 1)),
                                        reads=[bwn[s]] + bhT[tb * 4:(tb + 1) * 4], writes=[S.bpb[ba]])
                                for dc in range(DC):
                                    k.op("pe", lambda: nc.tensor.matmul(
                                        out=S.pb[bb][:], lhsT=wr[s][:, dc, cc, :, :, :],
                                        rhs=hT[:, dc, tb * 512:(tb + 1) * 512],
                                        start=(dc == 0), stop=(dc == DC - 1)),
                                        reads=[bwr[s]] + bhT[tb * 4:(tb + 1) * 4], writes=[S.bpb[bb]])
                                t1, bt1 = t1r.next()
                                t2, bt2 = t2r.next()
                                tsl = slice(tb * 512, (tb + 1) * 512)
                                k.op("dve", lambda: nc.vector.tensor_tensor(
                                    out=t1[:], in0=S.pb[ba][:], in1=cosT[:, tsl], op=ALU.mult),
                                    reads=[S.bpb[ba], btab], writes=[bt1])
                                k.op("dve", lambda: nc.vector.tensor_tensor(
                                    out=t2[:], in0=S.pb[bb][:], in1=sinT[:, tsl], op=ALU.mult),
                                    reads=[S.bpb[bb], btab], writes=[bt2])
                                g0 = hf * LH + tb * 512
                                gt = g0 // 128
                                k.op("pool", lambda: nc.gpsimd.tensor_tensor(
                                    out=dst[:, dci, g0:g0 + 512], in0=t1[:], in1=t2[:], op=ALU.add),
                                    reads=[bt1, bt2], writes=bdst[gt:gt + 4])
                        if bi + 2 < len(blocks):
                            load_block(bi + 2)
                    k.barrier()
                    es5.close()

        k.barrier()
        dbg(k, S, "qT", qT[:], bqT)
        dbg(k, S, "kT", kT[:], bkT)
        dbg(k, S, "qiT", qiT[:], bqiT)
        dbg(k, S, "kiT", kiT[:], bkiT)
        dbg(k, S, "Vaug", Vaug[:], bV)
        dbg(k, S, "wi", wi[:], bwi)
        with ExitStack() as es2:
            accR = Rot(k, "acc", 2, [128, L], F32, es2)
            amR = Rot(k, "amask", 2, [128, L], BF16, es2)
            junk2 = k.sb("junk2", [128, L], BF16, es2)
            bjunk2 = Buf("junk2")
            rlR = Rot(k, "rl", 2, [128, 512], F32, es2)
            ptR = Rot(k, "PT", 3, [128, 4, 128], BF16, es2)
            osR = Rot(k, "osb", 2, [128, D], BF16, es2)
            smR = Rot(k, "bis", 2, [128, 16], F32, es2)
            stR = Rot(k, "bstp", 2, [128, 2 * (NIT + 2)], F32, es2)
            recR = Rot(k, "rec", 2, [128, 4], F32, es2)
            cz = {"z": 0, "s": 0, "o": 0}
            tile_state = {}

            def gen_index(i):
                W = (i + 1) * 128
                acc, bacc = accR.next()
                am, bam = amR.next()
                sm, bsm = smR.next()
                tile_state[i] = (acc, bacc, am, bam, sm, bsm)
                for h in range(IDXH):
                    c = h % 4
                    plo = 64 * (h // 4)
                    for w0 in range(0, W, 512):
                        w1 = min(W, w0 + 512)
                        zb = cz["z"] % 2
                        cz["z"] += 1
                        k.op("pe", lambda: nc.tensor.matmul(
                            out=S.pb[zb][:, 0:w1 - w0], lhsT=qiT[plo:plo + 64, c, i * 128:(i + 1) * 128],
                            rhs=kiT[plo:plo + 64, 0, w0:w1], start=True, stop=True),
                            reads=[bqiT[i]] + bkiT[w0 // 128:w1 // 128], writes=[S.bpb[zb]])
                        rl, brl = rlR.next()
                        k.op("act", lambda: nc.scalar.activation(out=rl[:, 0:w1 - w0],
                                                                 in_=S.pb[zb][:, 0:w1 - w0], func=AF.Relu),
                             reads=[S.bpb[zb]], writes=[brl])
                        if h == 0:
                            k.op("dve", lambda: nc.vector.tensor_scalar(
                                out=acc[:, w0:w1], in0=rl[:, 0:w1 - w0], scalar1=wi[:, i, 0:1],
                                scalar2=None, op0=ALU.mult), reads=[brl, bwi[i]], writes=[bacc])
                        else:
                            k.op("dve", lambda: nc.vector.scalar_tensor_tensor(
                                out=acc[:, w0:w1], in0=rl[:, 0:w1 - w0], scalar=wi[:, i, h:h + 1],
                                in1=acc[:, w0:w1], op0=ALU.mult, op1=ALU.add),
                                reads=[brl, bwi[i], bacc], writes=[bacc])
                        yield
                k.op("pool", lambda: nc.gpsimd.affine_select(
                    out=acc[:, i * 128:W], in_=acc[:, i * 128:W], pattern=[[-1, 128]],
                    compare_op=ALU.is_ge, fill=-1e30, base=0, channel_multiplier=1),
                    reads=[bacc], writes=[bacc])
                yield

            def gen_bisect(i):
                W = (i + 1) * 128
                acc, bacc, am, bam, sm, bsm = tile_state[i]
                if W <= TOPK:
                    k.op("dve", lambda: nc.vector.tensor_scalar(
                        out=am[:, 0:W], in0=acc[:, 0:W], scalar1=-1e29, scalar2=NEG,
                        op0=ALU.is_lt, op1=ALU.mult), reads=[bacc], writes=[bam])
                    yield
                    return
                st, bst = stR.next()
                hi_, lo_, rng_, mid, cntv, tt, thr = [sm[:, j:j + 1] for j in range(7)]
                k.op("dve", lambda: nc.vector.tensor_reduce(out=hi_, in_=acc[:, 0:W], axis=AX.X,
                                                            op=ALU.max), reads=[bacc], writes=[bsm])
                k.op("dve", lambda: nc.vector.tensor_reduce(out=lo_, in_=acc[:, 0:i * 128], axis=AX.X,
                                                            op=ALU.min), reads=[bacc], writes=[bsm])
                k.op("dve", lambda: nc.vector.tensor_tensor(out=rng_, in0=hi_, in1=lo_,
                                                            op=ALU.subtract), reads=[bsm], writes=[bsm])
                k.op("dve", lambda: nc.vector.tensor_scalar(
                    out=st[:, 0:NIT + 2], in0=pw2[:, :], scalar1=rng_, scalar2=None, op0=ALU.mult),
                    reads=[bsm, bpw2], writes=[bst])
                k.op("dve", lambda: nc.vector.tensor_scalar(
                    out=st[:, NIT + 2:2 * (NIT + 2)], in0=pw2[:, :], scalar1=rng_, scalar2=2.0,
                    op0=ALU.mult, op1=ALU.mult), reads=[bsm, bpw2], writes=[bst])
                k.op("dve", lambda: nc.vector.tensor_tensor(out=mid, in0=lo_, in1=st[:, 0:1],
                                                            op=ALU.add), reads=[bsm, bst], writes=[bsm])
                yield
                for n in range(NIT + 1):
                    k.op("dve", lambda: nc.vector.tensor_scalar(
                        out=junk2[:, 0:W], in0=acc[:, 0:W], scalar1=mid, scalar2=0.0,
                        op0=ALU.is_ge, op1=ALU.add, accum_out=cntv),
                        reads=[bacc, bsm], writes=[bjunk2, bsm])
                    if n < NIT:
                        k.op("dve", lambda: nc.vector.tensor_scalar(
                            out=tt, in0=cntv, scalar1=TOPK - 0.5, scalar2=st[:, NIT + 2 + n + 1:NIT + 2 + n + 2],
                            op0=ALU.is_ge, op1=ALU.mult), reads=[bsm, bst], writes=[bsm])
                        k.op("dve", lambda: nc.vector.scalar_tensor_tensor(
                            out=mid, in0=tt, scalar=st[:, n + 1:n + 2], in1=mid, op0=ALU.subtract,
                            op1=ALU.add), reads=[bsm, bst], writes=[bsm])
                    else:
                        k.op("dve", lambda: nc.vector.tensor_scalar(
                            out=tt, in0=cntv, scalar1=TOPK - 0.5, scalar2=st[:, n:n + 1],
                            op0=ALU.is_lt, op1=ALU.mult), reads=[bsm, bst], writes=[bsm])
                        k.op("dve", lambda: nc.vector.tensor_tensor(out=thr, in0=mid, in1=tt,
                                                                    op=ALU.subtract),
                             reads=[bsm], writes=[bsm])
                    yield
                k.op("dve", lambda: nc.vector.tensor_scalar(
                    out=am[:, 0:W], in0=acc[:, 0:W], scalar1=thr, scalar2=NEG,
                    op0=ALU.is_lt, op1=ALU.mult), reads=[bacc, bsm], writes=[bam])
                yield

            def gen_attn(i):
                acc, bacc, am, bam, sm, bsm = tile_state[i]
                osb, bosb = osR.next()
                for n in range(KVH):
                    plo = 64 * (n // 2)
                    qc0 = 4 * (n % 2)
                    ob = 4 + cz["o"] % 2
                    cz["o"] += 1
                    for j in range(i + 1):
                        sbk = 2 + cz["s"] % 2
                        cz["s"] += 1
                        k.op("pe", lambda: nc.tensor.matmul(
                            out=S.pb[sbk][:].rearrange("p (g t) -> p g t", g=4),
                            lhsT=kT[plo:plo + 64, n % 2, j * 128:(j + 1) * 128],
                            rhs=qT[plo:plo + 64, qc0:qc0 + 4, i * 128:(i + 1) * 128],
                            start=True, stop=False),
                            reads=[bkT[j], bqT[i]], writes=[S.bpb[sbk]])
                        k.op("pe", lambda: nc.tensor.matmul(
                            out=S.pb[sbk][:].rearrange("p (g t) -> p g t", g=4),
                            lhsT=am[:, j * 128:(j + 1) * 128], rhs=ident4[:, :, :],
                            start=False, stop=True),
                            reads=[bam, bident4], writes=[S.bpb[sbk]])
                        pt, bpt = ptR.next()
                        k.op("act", lambda: nc.scalar.activation(
                            out=pt[:].rearrange("p g t -> p (g t)"), in_=S.pb[sbk][:], func=AF.Exp,
                            scale=0.125), reads=[S.bpb[sbk]], writes=[bpt])
                        for g in range(4):
                            k.op("pe", lambda: nc.tensor.matmul(
                                out=S.pb[ob][:, g * 65:(g + 1) * 65], lhsT=pt[:, g, :],
                                rhs=Vaug[:, j, n, :], start=(j == 0 and g == 0), stop=(j == i),
                                skip_group_check=True),
                                reads=[bpt, bV[j]], writes=[S.bpb[ob]])
                        yield
                    rec, brec = recR.next()
                    ov = S.pb[ob][:, 0:260].rearrange("p (g e) -> p g e", g=4)
                    k.op("dve", lambda: nc.vector.reciprocal(out=rec[:].rearrange("p (g o) -> p g o", o=1),
                                                             in_=ov[:, :, 64:65]),
                         reads=[S.bpb[ob]], writes=[brec])
                    k.op("dve", lambda: nc.vector.tensor_tensor(
                        out=osb[:, n * 256:(n + 1) * 256].rearrange("p (g e) -> p g e", g=4),
                        in0=ov[:, :, 0:64],
                        in1=rec[:].rearrange("p (g o) -> p g o", o=1).to_broadcast([128, 4, 64]),
                        op=ALU.mult), reads=[S.bpb[ob], brec], writes=[bosb])
                for c in range(DC):
                    k.op("pe", lambda: nc.tensor.transpose(out=S.pT[:, c * 128:(c + 1) * 128],
                                                           in_=osb[:, c * 128:(c + 1) * 128],
                                                           identity=S.ident[:]),
                         reads=[bosb, S.bident], writes=[S.bpT])
                k.op("act", lambda: nc.scalar.copy(out=qT[:, :, i * 128:(i + 1) * 128],
                                                   in_=S.pT[:].rearrange("p (c t) -> p c t", c=DC)),
                     reads=[S.bpT], writes=[bqT[i]])
                yield

            def chain(*gens):
                for g in gens:
                    for _ in g:
                        yield

            for _ in chain(gen_index(0), gen_bisect(0)):
                pass
            for i in range(NT):
                a_steps = 4 * (i + 1) + 1
                if i + 1 < NT:
                    nxt = chain(gen_index(i + 1), gen_bisect(i + 1))
                    Wn = (i + 2) * 128
                    n_steps = IDXH * ((Wn + 511) // 512) + 1 + (NIT + 3 if Wn > TOPK else 1)
                else:
                    nxt = iter(())
                    n_steps = 0
                done = 0
                for si, _ in enumerate(gen_attn(i)):
                    target = (n_steps * (si + 1) + a_steps - 1) // a_steps
                    while done < target:
                        try:
                            next(nxt)
                        except StopIteration:
                            break
                        done += 1
                for _ in nxt:
                    pass
        k.barrier()
        with ExitStack() as es2:
            wo = k.sb("dsa_wo", [128, DC, D], BF16, es2)
            bwo = Buf("wo")
            for c in range(DC):
                k.dma("pool", wo[:, c, :], w_o[c * 128:(c + 1) * 128, :], writes=[bwo])
            cc = 0
            for i in range(NT):
                for half in range(2):
                    b = 2 + cc % 4
                    cc += 1
                    for c in range(DC):
                        k.op("pe", lambda: nc.tensor.matmul(
                            out=S.pb[b][:], lhsT=qT[:, c, i * 128:(i + 1) * 128],
                            rhs=wo[:, c, half * 512:(half + 1) * 512], start=(c == 0), stop=(c == DC - 1)),
                            reads=[bqT[i], bwo], writes=[S.bpb[b]])
                    xs = S.X[:, i, half * 512:(half + 1) * 512]
                    k.op("dve", lambda: nc.vector.tensor_tensor(out=xs, in0=S.pb[b][:], in1=xs, op=ALU.add),
                         reads=[S.bpb[b], S.bX[i]], writes=[S.bX[i]])
            k.barrier()


def phase_rwkv(k, nc, S):
    L, NT, P = S.L, S.NT, S.P
    G, CH, NHG, FC = 2, 512, 8, 4
    CDEC = math.exp(-0.5)
    mu = P.rwkv_mu[0]

    def v3(ap, a):
        return ap.rearrange("p (a b) -> p a b", a=a)

    def bc3(ap2, a, b):
        return ap2.unsqueeze(2).to_broadcast([128, a, b])

    with ExitStack() as es:
        triI = k.sb("triI", [128, 128], F32, es)
        triS = k.sb("triS", [128, 128], F32, es)
        negc = k.sb("negc", [128, 1], F32, es)
        selw = k.sb("selw", [2, 128], F32, es)
        sela = k.sb("sela", [2, 128], F32, es)
        rows = k.sb("rows", [2, CH], F32, es)
        maskL = k.sb("maskL", [128, 128], BF16, es)
        maskU = k.sb("maskU", [128, 128], BF16, es)
        maskUi = k.sb("maskUi", [128, 128], BF16, es)
        muT = k.sb("muT", [128, 6, DC], F32, es)
        bcv = k.sb("bcv", [128, 5, CH], BF16, es)
        wl1 = k.sb("wl1", [128, DC, 288], BF16, es)
        yo0 = k.sb("yo0", [128, NT, CH], BF16, es)
        byo0 = [Buf("yo0") for _ in range(NT)]
        bconst = Buf("rconst")
        k.op("pool", lambda: nc.gpsimd.memset(triI[:], -CDEC), writes=[bconst])
        k.op("pool", lambda: nc.gpsimd.affine_select(out=triI[:], in_=triI[:], pattern=[[1, 128]],
                                                     compare_op=ALU.is_ge, fill=0.0, base=0,
                                                     channel_multiplier=-1), reads=[bconst], writes=[bconst])
        k.op("pool", lambda: nc.gpsimd.memset(triS[:], -CDEC), writes=[bconst])
        k.op("pool", lambda: nc.gpsimd.affine_select(out=triS[:], in_=triS[:], pattern=[[1, 128]],
                                                     compare_op=ALU.is_gt, fill=0.0, base=0,
                                                     channel_multiplier=-1), reads=[bconst], writes=[bconst])
        k.op("pool", lambda: nc.gpsimd.memset(negc[:], -CDEC), writes=[bconst])
        for sel_, base_ in ((selw, 0), (sela, -1)):
            k.op("pool", lambda: nc.gpsimd.memset(sel_[:], 1.0), writes=[bconst])
            k.op("pool", lambda: nc.gpsimd.affine_select(out=sel_[:], in_=sel_[:], pattern=[[0, 128]],
                                                         compare_op=ALU.is_equal, fill=0.0, base=base_,
                                                         channel_multiplier=1), reads=[bconst], writes=[bconst])
        for m_, (cmp_, cm, pat) in ((maskL, (ALU.is_gt, 1, -1)), (maskU, (ALU.is_gt, -1, 1)),
                                    (maskUi, (ALU.is_ge, -1, 1))):
            k.op("pool", lambda: nc.gpsimd.memset(m_[:], 1.0), writes=[bconst])
            k.op("pool", lambda: nc.gpsimd.affine_select(out=m_[:], in_=m_[:], pattern=[[pat, 128]],
                                                         compare_op=cmp_, fill=0.0, base=0,
                                                         channel_multiplier=cm), reads=[bconst], writes=[bconst])
        NLEVS = 7
        lmask = k.sb("lmask", [128, NLEVS, 2, 128], BF16, es)
        with ExitStack() as esm:
            iI = k.sb("iI", [128, 128], I32, esm)
            iJ = k.sb("iJ", [128, 128], I32, esm)
            iT = k.sb("iT", [128, 128], I32, esm)
            eqm = k.sb("eqm", [128, 128], BF16, esm)
            bm = Buf("lmtmp")
            k.op("pool", lambda: nc.gpsimd.iota(out=iI[:], pattern=[[0, 128]], base=0, channel_multiplier=1), writes=[bm])
            k.op("pool", lambda: nc.gpsimd.iota(out=iJ[:], pattern=[[1, 128]], base=0, channel_multiplier=0), writes=[bm])
            k.op("dve", lambda: nc.vector.tensor_tensor(out=iI[:], in0=iI[:], in1=iJ[:], op=ALU.bitwise_xor),
                 reads=[bm], writes=[bm])
            for l in range(1, NLEVS + 1):
                k.op("dve", lambda: nc.vector.tensor_single_scalar(out=iT[:], in_=iI[:], scalar=l - 1,
                                                                   op=ALU.logical_shift_right), reads=[bm], writes=[bm])
                k.op("dve", lambda: nc.vector.tensor_single_scalar(out=eqm[:], in_=iT[:], scalar=1, op=ALU.is_equal),
                     reads=[bm], writes=[bm])
                k.op("dve", lambda: nc.vector.tensor_tensor(out=lmask[:, l - 1, 0, :], in0=eqm[:], in1=maskL[:], op=ALU.mult),
                     reads=[bm, bconst], writes=[bconst])
                k.op("dve", lambda: nc.vector.tensor_tensor(out=lmask[:, l - 1, 1, :], in0=eqm[:], in1=maskU[:], op=ALU.mult),
                     reads=[bm, bconst], writes=[bconst])
            k.barrier()
        with nc.allow_non_contiguous_dma(reason="tiny per-channel vectors"):
            for c in range(6):
                k.dma("sp", muT[:, c, :], mu[c].rearrange("(dc p) -> p dc", p=128), writes=[bconst])
        k.dma("pool", wl1[:, :, 0:64], P.rwkv_w1[0].rearrange("(c p) n -> p c n", p=128), writes=[bconst])
        k.dma("pool", wl1[:, :, 64:128], P.rwkv_a1[0].rearrange("(c p) n -> p c n", p=128), writes=[bconst])
        k.dma("pool", wl1[:, :, 128:288], P.rwkv_g1[0].rearrange("(c p) n -> p c n", p=128), writes=[bconst])
        gbc, bgbc = load_gbc(k, nc, S, P.mixer_norm[1])

        mark(S, "const")
        wr_ = k.sb("rw_wr", [128, DC, CH], BF16, es)
        wk_ = k.sb("rw_wk", [128, DC, CH], BF16, es)
        wv_ = k.sb("rw_wv", [128, DC, CH], BF16, es)
        wa2 = k.sb("rw_wa2", [64, CH], BF16, es)
        a2g_ = k.sb("rw_a2g", [64, CH], BF16, es)
        g2a = k.sb("rw_g2a", [128, CH], BF16, es)
        g2b = k.sb("rw_g2b", [32, CH], BF16, es)
        bw = Buf("rw_w")
        wo = k.sb("rw_wo", [128, DC, D], BF16, es)
        bwo = Buf("rw_wo")
        hc1 = k.sb("rw_hc", [128, DC, 129], BF16, es)
        hc = [hc1, hc1]
        bhc1 = Buf("hc")
        bhc = [bhc1, bhc1]
        dx = k.sb("rw_dx", [128, DC, 128], BF16, es)
        bdx = Buf("dx")
        xsR = Rot(k, "rw_xs", 2, [128, DC, 128], BF16, es)
        Fp = [k.sb("rw_F%d" % i, [128, CH], F32, es) for i in range(8)]
        bF = [Buf("F%d" % i) for i in range(8)]
        small = k.sb("rw_small", [128, 64], F32, es)
        bsmall = Buf("small")
        lT = k.sb("rw_lT", [128, 3, 128], BF16, es)
        g1b = k.sb("rw_g1b", [32, 128], BF16, es)
        blT = Buf("lT")
        Vb = k.sb("rw_V", [128, CH], BF16, es)
        bVb = Buf("V")
        tok4 = k.sb("rw_tok4", [128, 4, CH], BF16, es)
        btok = [Buf("tok%d" % i) for i in range(4)]
        KRT = k.sb("rw_KRT", [128, FC, 2, 128], BF16, es)
        KBT = k.sb("rw_KBT", [128, 2, FC, 128], BF16, es)
        bKRT = Buf("KRT")
        bKBT = Buf("KBT")
        Pm = [k.sb("rw_P%d" % i, [128, NHG, 128], BF16, es) for i in range(2)]
        PTm = [k.sb("rw_PT%d" % i, [128, NHG, 128], BF16, es) for i in range(2)]
        bPm = [[Buf("P"), Buf("P")] for _ in range(2)]
        bPTm = [[Buf("PT"), Buf("PT")] for _ in range(2)]
        Ym = [k.sb("rw_Y%d" % i, [128, NHG, 128], BF16, es) for i in range(2)]
        bYm = [[Buf("Y"), Buf("Y")] for _ in range(2)]
        QT = PTm[1]
        ArbT = k.sb("rw_ArbT", [128, NHG, 128], BF16, es)
        AkkT = k.sb("rw_AkkT", [128, NHG, 128], BF16, es)
        ArkT = k.sb("rw_ArkT", [128, NHG, 128], BF16, es)
        bArb, bAkk, bArk = Buf("Arb"), Buf("Akk"), Buf("Ark")
        Hf = k.sb("rw_Hf", [128, FC * 64], F32, es)
        Hb = k.sb("rw_Hb", [128, FC * 64], BF16, es)
        bHf, bHb = Buf("Hf"), Buf("Hb")
        pC = k.sb("rw_pC", [128, FC], F32, es)
        bpC = Buf("pC")
        Zs = k.sb("rw_Zs", [128, CH], BF16, es)
        Us = k.sb("rw_Us", [128, CH], BF16, es)
        bZs, bUs = Buf("Zs"), Buf("Us")
        yo = Zs
        byo = bZs
        pb, bpb = S.pb, S.bpb
        prot = {"i": 0}

        def pbank():
            b = prot["i"] % 7
            prot["i"] += 1
            return b

        for hg in range(G):
            c0 = hg * CH
            for wt, ci in ((wr_, 0), (wk_, 1), (wv_, 2)):
                k.dma("pool", wt[:], P.rwkv_w_rkv[0][ci][:, c0:c0 + CH].rearrange("(c p) n -> p c n", p=128),
                      writes=[bw])
            k.dma("pool", wa2[0:64, :], P.rwkv_w2[0][:, c0:c0 + CH], writes=[bw])
            k.dma("pool", a2g_[:], P.rwkv_a2[0][:, c0:c0 + CH], writes=[bw])
            k.dma("pool", g2a[:], P.rwkv_g2[0][0:128, c0:c0 + CH], writes=[bw])
            k.dma("pool", g2b[:], P.rwkv_g2[0][128:160, c0:c0 + CH], writes=[bw])
            k.dma("sp", rows[0:1, :], P.rwkv_w0[0:1, c0:c0 + CH], writes=[bw])
            k.dma("sp", rows[1:2, :], P.rwkv_a0[0:1, c0:c0 + CH], writes=[bw])
            for j, vec in enumerate((P.rwkv_k_k[0], P.rwkv_k_a[0], P.rwkv_r_k[0].rearrange("h n -> (h n)"),
                                     P.rwkv_lnx_w[0], P.rwkv_lnx_b[0])):
                k.dma("pool", bcv[:, j, :], vec[c0:c0 + CH].partition_broadcast(128), writes=[bw])
            if hg == G - 1:
                for c in range(DC):
                    k.dma("pool", wo[:, c, :], P.rwkv_w_o[0][c * 128:(c + 1) * 128, :], writes=[bwo])
            k.op("dve", lambda: nc.vector.memset(Hf[:], 0.0), writes=[bHf])
            k.op("dve", lambda: nc.vector.memset(Hb[:], 0.0), writes=[bHb])
            k.op("dve", lambda: nc.vector.memset(hc[0][:, :, 0:1], 0.0), writes=[bhc[0]])

            for n in range(NT):
                cur, nxt = n % 2, (n + 1) % 2
                h_ = hc[cur]
                norm_to_hT(k, nc, S, n, gbc, bgbc, h_[:, :, 1:129], bhc[cur],
                           evac_eng=("act" if n % 2 == 0 else "dve"))
                mark(S, "P0")
                k.op("dve", lambda: nc.vector.tensor_tensor(out=dx[:], in0=h_[:, :, 0:128], in1=h_[:, :, 1:129],
                                                            op=ALU.subtract), reads=[bhc[cur]], writes=[bdx])
                if n + 1 < NT:
                    k.op("dve", lambda: nc.vector.tensor_copy(out=h_[:, :, 0:1], in_=h_[:, :, 128:129]),
                         reads=[bhc[cur]], writes=[bhc[cur]])

                def mix(c):
                    xs, bxs = xsR.next()
                    k.op("dve", lambda: nc.vector.tensor_tensor(out=xs[:], in0=dx[:],
                                                                in1=bc3(muT[:, c, :], DC, 128), op=ALU.mult),
                         reads=[bdx, bconst], writes=[bxs])
                    k.op("pool", lambda: nc.gpsimd.tensor_tensor(out=xs[:], in0=xs[:], in1=h_[:, :, 1:129],
                                                                 op=ALU.add), reads=[bxs, bhc[cur]], writes=[bxs])
                    return xs, bxs

                def proj_tok(xs, bxs, wt, extra=None):
                    b = pbank()
                    nmm = DC + (1 if extra else 0)
                    for dc in range(DC):
                        k.op("pe", lambda: nc.tensor.matmul(out=pb[b][:], lhsT=xs[:, dc, :], rhs=wt[:, dc, :],
                                                            start=(dc == 0), stop=(dc == nmm - 1)),
                             reads=[bxs, bw], writes=[bpb[b]])
                    return b

                mark(S, "P1")
                R_, K_, SG, A_, PP, PINV, GG, TA = range(8)
                PPREV = PP
                xs, bxs = mix(0)
                b = proj_tok(xs, bxs, wr_)
                k.op("act", lambda: nc.scalar.copy(out=Fp[R_][:], in_=pb[b][:]), reads=[bpb[b]], writes=[bF[R_]])
                xs, bxs = mix(1)
                b = proj_tok(xs, bxs, wk_)
                k.op("act", lambda: nc.scalar.copy(out=Fp[K_][:], in_=pb[b][:]), reads=[bpb[b]], writes=[bF[K_]])
                xs, bxs = mix(2)
                b = proj_tok(xs, bxs, wv_)
                k.op("act", lambda: nc.scalar.copy(out=Vb[:], in_=pb[b][:]), reads=[bpb[b]], writes=[bVb])
                mark(S, "P2a")
                b = pbank()
                xs, bxs = mix(3)
                for dc in range(DC):
                    k.op("pe", lambda: nc.tensor.matmul(out=pb[b][0:64, 0:128], lhsT=wl1[:, dc, 0:64], rhs=xs[:, dc, :],
                                                        start=(dc == 0), stop=(dc == DC - 1), skip_group_check=True),
                         reads=[bxs, bconst], writes=[bpb[b]])
                xs, bxs = mix(4)
                for dc in range(DC):
                    k.op("pe", lambda: nc.tensor.matmul(out=pb[b][0:64, 384:512], lhsT=wl1[:, dc, 64:128], rhs=xs[:, dc, :],
                                                        start=False, stop=(dc == DC - 1), skip_group_check=True),
                         reads=[bxs, bconst], writes=[bpb[b]])
                xs, bxs = mix(5)
                for dc in range(DC):
                    k.op("pe", lambda: nc.tensor.matmul(out=pb[b][:, 128:256], lhsT=wl1[:, dc, 128:256], rhs=xs[:, dc, :],
                                                        start=(dc == 0), stop=(dc == DC - 1), skip_group_check=True),
                         reads=[bxs, bconst], writes=[bpb[b]])
                for dc in range(DC):
                    k.op("pe", lambda: nc.tensor.matmul(out=pb[b][0:32, 256:384], lhsT=wl1[:, dc, 256:288], rhs=xs[:, dc, :],
                                                        start=False, stop=(dc == DC - 1), skip_group_check=True),
                         reads=[bxs, bconst], writes=[bpb[b]])
                k.op("act", lambda: nc.scalar.activation(out=lT[0:64, 0, :], in_=pb[b][0:64, 0:128], func=AF.Tanh),
                     reads=[bpb[b]], writes=[blT])
                k.op("act", lambda: nc.scalar.copy(out=lT[0:64, 2, :], in_=pb[b][0:64, 384:512]),
                     reads=[bpb[b]], writes=[blT])
                k.op("act", lambda: nc.scalar.activation(out=lT[:, 1, :], in_=pb[b][:, 128:256], func=AF.Sigmoid),
                     reads=[bpb[b]], writes=[blT])
                k.op("act", lambda: nc.scalar.activation(out=g1b[:], in_=pb[b][0:32, 256:384], func=AF.Sigmoid),
                     reads=[bpb[b]], writes=[blT])
                mark(S, "P2b")
                b = pbank()
                k.op("pe", lambda: nc.tensor.matmul(out=pb[b][:], lhsT=lT[0:64, 0, :], rhs=wa2[0:64, :], start=True, stop=False),
                     reads=[blT, bw], writes=[bpb[b]])
                k.op("pe", lambda: nc.tensor.matmul(out=pb[b][:], lhsT=selw[:], rhs=rows[:, :],
                                                    start=False, stop=True), reads=[bconst, bw], writes=[bpb[b]])
                k.op("act", lambda: nc.scalar.activation(out=Fp[SG][:], in_=pb[b][:], func=AF.Sigmoid),
                     reads=[bpb[b]], writes=[bF[SG]])
                b = pbank()
                k.op("pe", lambda: nc.tensor.matmul(out=pb[b][:], lhsT=lT[0:64, 2, :], rhs=a2g_[:], start=True, stop=False),
                     reads=[blT, bw], writes=[bpb[b]])
                k.op("pe", lambda: nc.tensor.matmul(out=pb[b][:], lhsT=sela[:], rhs=rows[:, :],
                                                    start=False, stop=True), reads=[bconst, bw], writes=[bpb[b]])
                k.op("act", lambda: nc.scalar.activation(out=Fp[A_][:], in_=pb[b][:], func=AF.Sigmoid),
                     reads=[bpb[b]], writes=[bF[A_]])
                b = pbank()
                k.op("pe", lambda: nc.tensor.matmul(out=pb[b][:], lhsT=lT[:, 1, :], rhs=g2a[:], start=True, stop=False),
                     reads=[blT, bw], writes=[bpb[b]])
                k.op("pe", lambda: nc.tensor.matmul(out=pb[b][:], lhsT=g1b[:], rhs=g2b[:], start=False, stop=True),
                     reads=[blT, bw], writes=[bpb[b]])
                k.op("act", lambda: nc.scalar.copy(out=Fp[GG][:], in_=pb[b][:]), reads=[bpb[b]], writes=[bF[GG]])
                mark(S, "P2c")
                b = pbank()
                k.op("pe", lambda: nc.tensor.matmul(out=pb[b][:], lhsT=triI[:], rhs=Fp[SG][:], start=True, stop=True),
                     reads=[bconst, bF[SG]], writes=[bpb[b]])
                k.op("act", lambda: nc.scalar.activation(out=Fp[PP][:], in_=pb[b][:], func=AF.Exp),
                     reads=[bpb[b]], writes=[bF[PP]])
                k.op("act", lambda: nc.scalar.activation(out=Fp[PINV][:], in_=pb[b][:], func=AF.Exp, scale=-1.0),
                     reads=[bpb[b]], writes=[bF[PINV]])
                k.op("pool", lambda: nc.gpsimd.tensor_tensor(out=tok4[:, 1, :], in0=Fp[R_][:], in1=Fp[PP][:],
                                                             op=ALU.mult), reads=[bF[R_], bF[PP]], writes=[btok[1]])
                b = pbank()
                k.op("pe", lambda: nc.tensor.matmul(out=pb[b][:], lhsT=triS[:], rhs=Fp[SG][:], start=True, stop=True),
                     reads=[bconst, bF[SG]], writes=[bpb[b]])
                k.op("act", lambda: nc.scalar.activation(out=Fp[PPREV][:], in_=pb[b][:], func=AF.Exp),
                     reads=[bpb[b]], writes=[bF[PPREV]])
                b = pbank()
                for fc in range(FC):
                    k.op("pe", lambda: nc.tensor.matmul(out=pb[b][:, fc:fc + 1],
                                                        lhsT=Fp[SG][:, fc * 128:(fc + 1) * 128], rhs=negc[:],
                                                        start=(fc == 0), stop=True, skip_group_check=True),
                         reads=[bconst, bF[SG]], writes=[bpb[b]])
                k.op("act", lambda: nc.scalar.activation(out=pC[:], in_=pb[b][:, 0:FC], func=AF.Exp),
                     reads=[bpb[b]], writes=[bpC])
                mark(S, "P3")
                kkb = bcv[:, 0, :]
                kab = bcv[:, 1, :]
                rkb = bcv[:, 2, :]
                TB, TC = SG, TA + 0
                k.op("dve", lambda: nc.vector.tensor_tensor(out=Fp[TA][:], in0=Fp[K_][:], in1=kkb, op=ALU.mult),
                     reads=[bF[K_], bw], writes=[bF[TA]])
                k.op("pool", lambda: nc.gpsimd.tensor_tensor(out=Fp[TB][:], in0=Fp[TA][:], in1=Fp[TA][:], op=ALU.mult),
                     reads=[bF[TA]], writes=[bF[TB]])
                k.op("dve", lambda: nc.vector.tensor_reduce(out=small[:, 0:8], in_=v3(Fp[TB][:], NHG), axis=AX.X,
                                                            op=ALU.add), reads=[bF[TB]], writes=[bsmall])
                k.op("dve", lambda: nc.vector.tensor_scalar(out=small[:, 0:8], in0=small[:, 0:8], scalar1=1e-24,
                                                            scalar2=None, op0=ALU.max), reads=[bsmall], writes=[bsmall])
                k.op("act", lambda: nc.scalar.activation(out=small[:, 0:8], in_=small[:, 0:8], func=AF.Sqrt),
                     reads=[bsmall], writes=[bsmall])
                k.op("dve", lambda: nc.vector.reciprocal(out=small[:, 8:16], in_=small[:, 0:8]),
                     reads=[bsmall], writes=[bsmall])
                k.op("dve", lambda: nc.vector.tensor_tensor(out=v3(Fp[TA][:], NHG), in0=v3(Fp[TA][:], NHG),
                                                            in1=bc3(small[:, 8:16], NHG, 64), op=ALU.mult),
                     reads=[bF[TA], bsmall], writes=[bF[TA]])
                k.op("pool", lambda: nc.gpsimd.tensor_tensor(out=tok4[:, 0, :], in0=Fp[TA][:], in1=Fp[PPREV][:],
                                                             op=ALU.mult), reads=[bF[TA], bF[PPREV]], writes=[btok[0]])
                k.op("dve", lambda: nc.vector.scalar_tensor_tensor(out=Fp[TB][:], in0=Fp[TA][:], scalar=-1.0,
                                                                   in1=Fp[A_][:], op0=ALU.mult, op1=ALU.mult),
                     reads=[bF[TA], bF[A_]], writes=[bF[TB]])
                k.op("pool", lambda: nc.gpsimd.tensor_tensor(out=tok4[:, 3, :], in0=Fp[TB][:], in1=Fp[PINV][:],
                                                             op=ALU.mult), reads=[bF[TB], bF[PINV]], writes=[btok[3]])
                k.op("dve", lambda: nc.vector.scalar_tensor_tensor(out=Fp[TB][:], in0=Fp[A_][:], scalar=-1.0,
                                                                   in1=kab, op0=ALU.add, op1=ALU.mult),
                     reads=[bF[A_], bw], writes=[bF[TB]])
                k.op("dve", lambda: nc.vector.scalar_tensor_tensor(out=Fp[TB][:], in0=Fp[TB][:], scalar=1.0,
                                                                   in1=Fp[K_][:], op0=ALU.add, op1=ALU.mult),
                     reads=[bF[TB], bF[K_]], writes=[bF[TB]])
                k.op("pool", lambda: nc.gpsimd.tensor_tensor(out=tok4[:, 2, :], in0=Fp[TB][:], in1=Fp[PINV][:],
                                                             op=ALU.mult), reads=[bF[TB], bF[PINV]], writes=[btok[2]])
                k.op("dve", lambda: nc.vector.tensor_tensor(out=Fp[TA][:], in0=Fp[R_][:], in1=rkb, op=ALU.mult),
                     reads=[bF[R_], bw], writes=[bF[TA]])
                k.op("pool", lambda: nc.gpsimd.tensor_tensor(out=Fp[TA][:], in0=Fp[TA][:], in1=Fp[TB][:], op=ALU.mult),
                     reads=[bF[TA], bF[TB]], writes=[bF[TA]])
                k.op("dve", lambda: nc.vector.tensor_reduce(out=small[:, 16:24], in_=v3(Fp[TA][:], NHG), axis=AX.X,
                                                            op=ALU.add), reads=[bF[TA]], writes=[bsmall])
                mark(S, "P4")
                for j, (src_i, dst_ap) in enumerate(((0, 0), (1, 1))):
                    for fc in range(FC):
                        k.op("pe", lambda: nc.tensor.transpose(
                            out=S.pT[:, (fc * 2 + j) * 128:(fc * 2 + j + 1) * 128],
                            in_=tok4[:, src_i, fc * 128:(fc + 1) * 128], identity=S.ident[:]),
                            reads=[btok[src_i], S.bident], writes=[S.bpT])
                mark(S, "P5a")
                k.op("act", lambda: nc.scalar.copy(out=KRT[:].rearrange("p f j t -> p (f j t)"), in_=S.pT[:]),
                     reads=[S.bpT], writes=[bKRT])
                mark(S, "P5b")
                for j, src_i in enumerate((2, 3)):
                    for fc in range(FC):
                        k.op("pe", lambda: nc.tensor.transpose(
                            out=S.pT[:, (j * FC + fc) * 128:(j * FC + fc + 1) * 128],
                            in_=tok4[:, src_i, fc * 128:(fc + 1) * 128], identity=S.ident[:]),
                            reads=[btok[src_i], S.bident], writes=[S.bpT])
                mark(S, "P5c")
                k.op("act", lambda: nc.scalar.copy(out=KBT[:].rearrange("p j f t -> p (j f t)"), in_=S.pT[:]),
                     reads=[S.bpT], writes=[bKBT])
                mark(S, "P5")
                for h4 in range(2):
                    b = pbank()
                    for hh in range(4):
                        plo, fc = 64 * h4, hh
                        k.op("pe", lambda: nc.tensor.matmul(out=pb[b][:, hh * 128:(hh + 1) * 128],
                                                            lhsT=KRT[plo:plo + 64, fc, 0, :], rhs=KBT[plo:plo + 64, 1, fc, :],
                                                            start=True, stop=True, skip_group_check=True),
                             reads=[bKRT, bKBT], writes=[bpb[b]])
                    mark(S, "P6m")
                    k.op("dve", lambda: nc.vector.tensor_tensor(out=Pm[0][:, h4 * 4:(h4 + 1) * 4, :], in0=v3(pb[b][:], 4),
                                                                in1=maskL[:].unsqueeze(1).to_broadcast([128, 4, 128]),
                                                                op=ALU.mult), reads=[bpb[b], bconst], writes=[bPm[0][h4]])
                mark(S, "P6a")
                for h2 in range(4):
                    b = pbank()
                    for hh in range(2):
                        s_ = h2 * 2 + hh
                        plo, fc = 64 * (s_ // 4), s_ % 4
                        k.op("pe", lambda: nc.tensor.matmul(out=pb[b][:, hh * 256:(hh + 1) * 256],
                                                            lhsT=KBT[plo:plo + 64, 1, fc, :], rhs=KRT[plo:plo + 64, fc, :, :],
                                                            start=True, stop=True, skip_group_check=True),
                             reads=[bKRT, bKBT], writes=[bpb[b]])
                    pv = pb[b][:].rearrange("p (h j t) -> p h j t", h=2, j=2)
                    k.op("dve", lambda: nc.vector.tensor_tensor(out=PTm[0][:, h2 * 2:(h2 + 1) * 2, :], in0=pv[:, :, 0, :],
                                                                in1=maskU[:].unsqueeze(1).to_broadcast([128, 2, 128]),
                                                                op=ALU.mult), reads=[bpb[b], bconst], writes=[bPTm[0][h2 // 2]])
                    k.op("act", lambda: nc.scalar.copy(out=ArbT[:, h2 * 2:(h2 + 1) * 2, :], in_=pv[:, :, 1, :]),
                         reads=[bpb[b]], writes=[bArb])
                mark(S, "P6b")
                for h2 in range(4):
                    b = pbank()
                    for hh in range(2):
                        s_ = h2 * 2 + hh
                        plo, fc = 64 * (s_ // 4), s_ % 4
                        k.op("pe", lambda: nc.tensor.matmul(out=pb[b][:, hh * 256:(hh + 1) * 256],
                                                            lhsT=KBT[plo:plo + 64, 0, fc, :], rhs=KRT[plo:plo + 64, fc, :, :],
                                                            start=True, stop=True, skip_group_check=True),
                             reads=[bKRT, bKBT], writes=[bpb[b]])
                    pv = pb[b][:].rearrange("p (h j t) -> p h j t", h=2, j=2)
                    k.op("dve", lambda: nc.vector.tensor_tensor(out=AkkT[:, h2 * 2:(h2 + 1) * 2, :], in0=pv[:, :, 0, :],
                                                                in1=maskU[:].unsqueeze(1).to_broadcast([128, 2, 128]),
                                                                op=ALU.mult), reads=[bpb[b], bconst], writes=[bAkk])
                    k.op("act", lambda: nc.scalar.copy(out=ArkT[:, h2 * 2:(h2 + 1) * 2, :], in_=pv[:, :, 1, :]),
                         reads=[bpb[b]], writes=[bArk])
                mark(S, "P6c")
                k.op("pool", lambda: nc.gpsimd.tensor_tensor(out=ArbT[:], in0=ArbT[:],
                                                             in1=maskUi[:].unsqueeze(1).to_broadcast([128, NHG, 128]),
                                                             op=ALU.mult), reads=[bArb, bconst], writes=[bArb])
                k.op("pool", lambda: nc.gpsimd.tensor_tensor(out=ArkT[:], in0=ArkT[:],
                                                             in1=maskUi[:].unsqueeze(1).to_broadcast([128, NHG, 128]),
                                                             op=ALU.mult), reads=[bArk, bconst], writes=[bArk])
                mark(S, "P6")
                Dm, DTm, bD_, bDT_ = Pm[1], PTm[1], bPm[1], bPTm[1]
                for h4 in range(2):
                    hs = slice(h4 * 4, (h4 + 1) * 4)
                    k.op("pool", lambda: nc.gpsimd.tensor_tensor(out=Dm[:, hs, :], in0=Pm[0][:, hs, :],
                                                                 in1=lmask[:, 0, 0, :].unsqueeze(1).to_broadcast([128, 4, 128]),
                                                                 op=ALU.mult), reads=[bPm[0][h4], bconst], writes=[bD_[h4]])
                    k.op("pool", lambda: nc.gpsimd.tensor_tensor(out=Dm[:, hs, :], in0=Dm[:, hs, :],
                                                                 in1=S.ident[:].unsqueeze(1).to_broadcast([128, 4, 128]),
                                                                 op=ALU.add), reads=[bD_[h4], S.bident], writes=[bD_[h4]])
                    k.op("pool", lambda: nc.gpsimd.tensor_tensor(out=DTm[:, hs, :], in0=PTm[0][:, hs, :],
                                                                 in1=lmask[:, 0, 1, :].unsqueeze(1).to_broadcast([128, 4, 128]),
                                                                 op=ALU.mult), reads=[bPTm[0][h4], bconst], writes=[bDT_[h4]])
                    k.op("pool", lambda: nc.gpsimd.tensor_tensor(out=DTm[:, hs, :], in0=DTm[:, hs, :],
                                                                 in1=S.ident[:].unsqueeze(1).to_broadcast([128, 4, 128]),
                                                                 op=ALU.add), reads=[bDT_[h4], S.bident], writes=[bDT_[h4]])
                for l in range(2, NLEVS + 1):
                    last = (l == NLEVS)
                    for h4 in range(2):
                        if not last:
                            b1 = pbank()
                            for hh in range(4):
                                h = h4 * 4 + hh
                                k.op("pe", lambda: nc.tensor.matmul(out=pb[b1][:, hh * 128:(hh + 1) * 128],
                                                                    lhsT=PTm[0][:, h, :], rhs=Dm[:, h, :],
                                                                    start=True, stop=True, skip_group_check=True),
                                     reads=[bPTm[0][h4], bD_[h4]], writes=[bpb[b1]])
                            k.op("dve", lambda: nc.vector.tensor_tensor(
                                out=Ym[0][:, h4 * 4:(h4 + 1) * 4, :], in0=v3(pb[b1][:], 4),
                                in1=lmask[:, l - 1, 0, :].unsqueeze(1).to_broadcast([128, 4, 128]), op=ALU.mult),
                                reads=[bpb[b1], bconst], writes=[bYm[0][h4]])
                        b2 = pbank()
                        for hh in range(4):
                            h = h4 * 4 + hh
                            k.op("pe", lambda: nc.tensor.matmul(out=pb[b2][:, hh * 128:(hh + 1) * 128],
                                                                lhsT=Pm[0][:, h, :], rhs=DTm[:, h, :],
                                                                start=True, stop=True, skip_group_check=True),
                                 reads=[bPm[0][h4], bDT_[h4]], writes=[bpb[b2]])
                        k.op("dve", lambda: nc.vector.tensor_tensor(
                            out=Ym[1][:, h4 * 4:(h4 + 1) * 4, :], in0=v3(pb[b2][:], 4),
                            in1=lmask[:, l - 1, 1, :].unsqueeze(1).to_broadcast([128, 4, 128]), op=ALU.mult),
                            reads=[bpb[b2], bconst], writes=[bYm[1][h4]])
                    zb = []
                    for h4 in range(2):
                        if not last:
                            b1 = pbank()
                            for hh in range(4):
                                h = h4 * 4 + hh
                                k.op("pe", lambda: nc.tensor.matmul(out=pb[b1][:, hh * 128:(hh + 1) * 128],
                                                                    lhsT=DTm[:, h, :], rhs=Ym[0][:, h, :],
                                                                    start=True, stop=True, skip_group_check=True),
                                     reads=[bDT_[h4], bYm[0][h4]], writes=[bpb[b1]])
                            zb.append((0, h4, b1))
                        b2 = pbank()
                        for hh in range(4):
                            h = h4 * 4 + hh
                            k.op("pe", lambda: nc.tensor.matmul(out=pb[b2][:, hh * 128:(hh + 1) * 128],
                                                                lhsT=Dm[:, h, :], rhs=Ym[1][:, h, :],
                                                                start=True, stop=True, skip_group_check=True),
                                 reads=[bD_[h4], bYm[1][h4]], writes=[bpb[b2]])
                        zb.append((1, h4, b2))
                        if len(zb) >= 2 or last:
                            for (which, hq, bb) in zb:
                                tgt, btgt = (Dm, bD_[hq]) if which == 0 else (DTm, bDT_[hq])
                                k.op("dve", lambda: nc.vector.tensor_tensor(
                                    out=tgt[:, hq * 4:(hq + 1) * 4, :], in0=v3(pb[bb][:], 4),
                                    in1=tgt[:, hq * 4:(hq + 1) * 4, :], op=ALU.add),
                                    reads=[bpb[bb], btgt], writes=[btgt])
                            zb = []
                mark(S, "P7")
                bZ, bU, bD, bY = 3, 4, 5, 6
                for s_ in range(NHG):
                    plo, fc = 64 * (s_ // 4), s_ % 4
                    h = 2 * fc + s_ // 4
                    k.op("pe", lambda: nc.tensor.matmul(out=pb[bZ][:, h * 64:(h + 1) * 64], lhsT=KRT[plo:plo + 64, fc, 0, :],
                                                        rhs=Hb[plo:plo + 64, fc * 64:(fc + 1) * 64],
                                                        start=(s_ == 0), stop=False, skip_group_check=True),
                         reads=[bKRT, bHb], writes=[bpb[bZ]])
                    k.op("pe", lambda: nc.tensor.matmul(out=pb[bZ][:, h * 64:(h + 1) * 64], lhsT=AkkT[:, s_, :],
                                                        rhs=Vb[:, h * 64:(h + 1) * 64],
                                                        start=False, stop=True, skip_group_check=True),
                         reads=[bAkk, bVb], writes=[bpb[bZ]])
                k.op("act", lambda: nc.scalar.copy(out=Zs[:], in_=pb[bZ][:]), reads=[bpb[bZ]], writes=[bZs])
                for s_ in range(NHG):
                    h = 2 * (s_ % 4) + s_ // 4
                    k.op("pe", lambda: nc.tensor.matmul(out=pb[bU][:, h * 64:(h + 1) * 64], lhsT=QT[:, s_, :],
                                                        rhs=Zs[:, h * 64:(h + 1) * 64],
                                                        start=(s_ == 0), stop=True, skip_group_check=True),
                         reads=[bPTm[1][0], bPTm[1][1], bZs], writes=[bpb[bU]])
                k.op("act", lambda: nc.scalar.copy(out=Us[:], in_=pb[bU][:]), reads=[bpb[bU]], writes=[bUs])
                for s_ in range(NHG):
                    plo, fc = 64 * (s_ // 4), s_ % 4
                    h = 2 * fc + s_ // 4
                    k.op("pe", lambda: nc.tensor.matmul(out=pb[bY][:, h * 64:(h + 1) * 64], lhsT=KRT[plo:plo + 64, fc, 1, :],
                                                        rhs=Hb[plo:plo + 64, fc * 64:(fc + 1) * 64],
                                                        start=(s_ == 0), stop=False, skip_group_check=True),
                         reads=[bKRT, bHb], writes=[bpb[bY]])
                    k.op("pe", lambda: nc.tensor.matmul(out=pb[bY][:, h * 64:(h + 1) * 64], lhsT=ArkT[:, s_, :],
                                                        rhs=Vb[:, h * 64:(h + 1) * 64],
                                                        start=False, stop=False, skip_group_check=True),
                         reads=[bArk, bVb], writes=[bpb[bY]])
                    k.op("pe", lambda: nc.tensor.matmul(out=pb[bY][:, h * 64:(h + 1) * 64], lhsT=ArbT[:, s_, :],
                                                        rhs=Us[:, h * 64:(h + 1) * 64],
                                                        start=False, stop=True, skip_group_check=True),
                         reads=[bArb, bUs], writes=[bpb[bY]])
                for s_ in range(NHG):
                    plo, fc = 64 * (s_ // 4), s_ % 4
                    h = 2 * fc + s_ // 4
                    bDD = bD if s_ < 4 else bZ
                    k.op("pe", lambda: nc.tensor.matmul(out=pb[bDD][plo:plo + 64, fc * 64:(fc + 1) * 64],
                                                        lhsT=tok4[:, 2, h * 64:(h + 1) * 64], rhs=Vb[:, h * 64:(h + 1) * 64],
                                                        start=(s_ % 4 == 0), stop=False, skip_group_check=True),
                         reads=[btok[2], bVb], writes=[bpb[bDD]])
                    k.op("pe", lambda: nc.tensor.matmul(out=pb[bDD][plo:plo + 64, fc * 64:(fc + 1) * 64],
                                                        lhsT=tok4[:, 3, h * 64:(h + 1) * 64], rhs=Us[:, h * 64:(h + 1) * 64],
                                                        start=False, stop=True, skip_group_check=True),
                         reads=[btok[3], bUs], writes=[bpb[bDD]])
                k.op("dve", lambda: nc.vector.tensor_tensor(out=Hf[0:64, :], in0=pb[bD][0:64, 0:FC * 64], in1=Hf[0:64, :],
                                                            op=ALU.add), reads=[bpb[bD], bHf], writes=[bHf])
                k.op("dve", lambda: nc.vector.tensor_tensor(out=Hf[64:128, :], in0=pb[bZ][64:128, 0:FC * 64],
                                                            in1=Hf[64:128, :], op=ALU.add),
                     reads=[bpb[bZ], bHf], writes=[bHf])
                k.op("dve", lambda: nc.vector.tensor_tensor(out=v3(Hf[:], FC), in0=v3(Hf[:], FC), in1=bc3(pC[:], FC, 64),
                                                            op=ALU.mult), reads=[bHf, bpC], writes=[bHf])
                k.op("act", lambda: nc.scalar.copy(out=Hb[:], in_=Hf[:]), reads=[bHf], writes=[bHb])
                mark(S, "scan")
                YS, YQ = PP, PINV
                k.op("act", lambda: nc.scalar.copy(out=Fp[YS][:], in_=pb[bY][:]), reads=[bpb[bY]], writes=[bF[YS]])
                k.op("dve", lambda: nc.vector.tensor_reduce(out=small[:, 24:32], in_=v3(Fp[YS][:], NHG), axis=AX.X,
                                                            op=ALU.add), reads=[bF[YS]], writes=[bsmall])
                k.op("pool", lambda: nc.gpsimd.tensor_tensor(out=Fp[YQ][:], in0=Fp[YS][:], in1=Fp[YS][:], op=ALU.mult),
                     reads=[bF[YS]], writes=[bF[YQ]])
                k.op("dve", lambda: nc.vector.tensor_reduce(out=small[:, 32:40], in_=v3(Fp[YQ][:], NHG), axis=AX.X,
                                                            op=ALU.add), reads=[bF[YQ]], writes=[bsmall])
                k.op("dve", lambda: nc.vector.tensor_scalar(out=small[:, 24:32], in0=small[:, 24:32], scalar1=1.0 / 64,
                                                            scalar2=None, op0=ALU.mult), reads=[bsmall], writes=[bsmall])
                k.op("dve", lambda: nc.vector.tensor_tensor(out=small[:, 40:48], in0=small[:, 24:32], in1=small[:, 24:32],
                                                            op=ALU.mult), reads=[bsmall], writes=[bsmall])
                k.op("dve", lambda: nc.vector.scalar_tensor_tensor(out=small[:, 32:40], in0=small[:, 32:40], scalar=1.0 / 64,
                                                                   in1=small[:, 40:48], op0=ALU.mult, op1=ALU.subtract),
                     reads=[bsmall], writes=[bsmall])
                k.op("act", lambda: nc.scalar.activation(out=small[:, 32:40], in_=small[:, 32:40], func=AF.Sqrt,
                                                         bias=LNX_EPS, scale=1.0), reads=[bsmall], writes=[bsmall])
                k.op("dve", lambda: nc.vector.reciprocal(out=small[:, 40:48], in_=small[:, 32:40]),
                     reads=[bsmall], writes=[bsmall])
                k.op("dve", lambda: nc.vector.tensor_tensor(out=v3(Fp[YS][:], NHG), in0=v3(Fp[YS][:], NHG),
                                                            in1=bc3(small[:, 24:32], NHG, 64), op=ALU.subtract),
                     reads=[bF[YS], bsmall], writes=[bF[YS]])
                k.op("dve", lambda: nc.vector.tensor_tensor(out=v3(Fp[YS][:], NHG), in0=v3(Fp[YS][:], NHG),
                                                            in1=bc3(small[:, 40:48], NHG, 64), op=ALU.mult),
                     reads=[bF[YS], bsmall], writes=[bF[YS]])
                k.op("pool", lambda: nc.gpsimd.tensor_tensor(out=Fp[YS][:], in0=Fp[YS][:], in1=bcv[:, 3, :],
                                                             op=ALU.mult), reads=[bF[YS], bw], writes=[bF[YS]])
                k.op("pool", lambda: nc.gpsimd.tensor_tensor(out=Fp[YS][:], in0=Fp[YS][:], in1=bcv[:, 4, :],
                                                             op=ALU.add), reads=[bF[YS], bw], writes=[bF[YS]])
                k.op("dve", lambda: nc.vector.tensor_tensor(out=v3(Fp[YQ][:], NHG), in0=v3(Vb[:], NHG),
                                                            in1=bc3(small[:, 16:24], NHG, 64), op=ALU.mult),
                     reads=[bVb, bsmall], writes=[bF[YQ]])
                k.op("pool", lambda: nc.gpsimd.tensor_tensor(out=Fp[YS][:], in0=Fp[YS][:], in1=Fp[YQ][:], op=ALU.add),
                     reads=[bF[YS], bF[YQ]], writes=[bF[YS]])
                if hg < G - 1:
                    k.op("dve", lambda: nc.vector.tensor_tensor(out=yo0[:, n, :], in0=Fp[YS][:], in1=Fp[GG][:], op=ALU.mult),
                         reads=[bF[YS], bF[GG]], writes=[byo0[n]])
                else:
                    k.op("dve", lambda: nc.vector.tensor_tensor(out=yo[:], in0=Fp[YS][:], in1=Fp[GG][:], op=ALU.mult),
                         reads=[bF[YS], bF[GG]], writes=[byo])
                    for fc in range(FC):
                        k.op("pe", lambda: nc.tensor.transpose(out=S.pT[:, fc * 128:(fc + 1) * 128],
                                                               in_=yo0[:, n, fc * 128:(fc + 1) * 128], identity=S.ident[:]),
                             reads=[byo0[n], S.bident], writes=[S.bpT])
                    for fc in range(FC):
                        k.op("pe", lambda: nc.tensor.transpose(out=S.pT[:, (FC + fc) * 128:(FC + fc + 1) * 128],
                                                               in_=yo[:, fc * 128:(fc + 1) * 128], identity=S.ident[:]),
                             reads=[byo, S.bident], writes=[S.bpT])
                    yoT = KBT[:].rearrange("p j f t -> p (j f) t")
                    byoT = bKBT
                    k.op("act", lambda: nc.scalar.copy(out=KBT[:].rearrange("p j f t -> p (j f t)"), in_=S.pT[:]),
                         reads=[S.bpT], writes=[byoT])
                    for half in range(2):
                        b = pbank()
                        for c in range(DC):
                            k.op("pe", lambda: nc.tensor.matmul(out=pb[b][:], lhsT=yoT[:, c, :],
                                                                rhs=wo[:, c, half * 512:(half + 1) * 512],
                                                                start=(c == 0), stop=(c == DC - 1)),
                                 reads=[byoT, bwo], writes=[bpb[b]])
                        xsl = S.X[:, n, half * 512:(half + 1) * 512]
                        k.op("dve", lambda: nc.vector.tensor_tensor(out=xsl, in0=pb[b][:], in1=xsl, op=ALU.add),
                             reads=[bpb[b], S.bX[n]], writes=[S.bX[n]])
        k.barrier()


LAST_K = None
_NC_CACHE = {}


def kernel(**inputs):
    x = np.asarray(inputs["x"], dtype=np.float32)
    B, L, _ = x.shape
    if L not in _NC_CACHE:
        _NC_CACHE[L] = build(L)
    nc = _NC_CACHE[L]
    params = {n: np.ascontiguousarray(np.asarray(inputs[n], dtype=np.float32))
              for n in PARAM_SHAPES}
    in_maps = []
    for b in range(B):
        m = {"x": np.ascontiguousarray(x[b])}
        m.update(params)
        in_maps.append(m)
    res = run_bass_kernel_spmd(nc, in_maps, core_ids=list(range(B)))
    return np.stack([res.results[b]["out"] for b in range(B)], axis=0)
```

```python
import math
import numpy as np
from contextlib import ExitStack
import concourse.bass as bass
import concourse.mybir as mybir
from concourse.bass_utils import run_bass_kernel_spmd

F32 = mybir.dt.float32
BF16 = mybir.dt.bfloat16
I32 = mybir.dt.int32
AF = mybir.ActivationFunctionType
ALU = mybir.AluOpType
AX = mybir.AxisListType

D = 1024
DC = 8
DFF = 4096
NCORES = 8
HEADS = 16
KVH = 4
IDXH = 8
IN_COLS = 2120
NORM_EPS = 1e-6
LNX_EPS = 64e-5
NEG = -30000.0


class Buf:
    __slots__ = ("w", "r", "name", "excl")

    def __init__(self, name="", excl=False):
        self.w = None
        self.r = {}
        self.name = name
        self.excl = excl


class K:
    N_DMA_SEMS = 24

    def __init__(self, nc, es):
        self.nc = nc
        self.es = es
        self.eng = {"pe": nc.tensor, "act": nc.scalar, "dve": nc.vector,
                    "pool": nc.gpsimd, "sp": nc.sync}
        self.sem = {}
        self.cnt = {}
        self.seen = {}
        self.hist = {}
        for e in self.eng:
            self.sem[e] = es.enter_context(nc.semaphore("s_" + e))
            self.cnt[e] = 0
            self.seen[e] = {}
            self.hist[e] = {}
        self.dsem = {"hw": [], "sw": []}
        for kind in ("hw", "sw"):
            for i in range(self.N_DMA_SEMS // 2):
                key = "d%s%d" % (kind, i)
                self.sem[key] = es.enter_context(nc.semaphore("s_" + key))
                self.cnt[key] = 0
                self.hist[key] = {}
                self.dsem[kind].append(key)
        self.dnext = {"hw": 0, "sw": 0}
        self.dead = False
        self.uid = 0
        self.names = []
        self.n_instr = 0
        self.n_wait = 0
        self.per_eng = {e: 0 for e in self.eng}

    def sb(self, name, shape, dtype, es=None):
        self.uid += 1
        self.names.append("%s_%d" % (name, self.uid))
        return (es or self.es).enter_context(self.nc.sbuf_tensor("%s_%d" % (name, self.uid), list(shape), dtype))

    def ps(self, name, shape, dtype, es=None):
        self.uid += 1
        self.names.append("%s_%d" % (name, self.uid))
        return (es or self.es).enter_context(self.nc.psum_tensor("%s_%d" % (name, self.uid), list(shape), dtype))

    def _need(self, e, deps):
        eng = self.eng[e]
        seen = self.seen[e]
        for (sk, v) in deps:
            if seen.get(sk, 0) >= v:
                continue
            eng.wait_ge(self.sem[sk], v)
            self.n_wait += 1
            seen[sk] = v
            h = self.hist[sk].get(v)
            if h:
                for k2, v2 in h.items():
                    if seen.get(k2, 0) < v2:
                        seen[k2] = v2

    def _deps(self, e, reads, writes):
        deps = []
        for b in reads:
            if b.w is not None:
                if b.w[0] == e and e == "pe":
                    continue
                deps.append(b.w)
        for b in writes:
            if b.w is not None and b.w[0] != e:
                deps.append(b.w)
            for sk, v in b.r.items():
                if sk != e or e != "pe":
                    deps.append((sk, v))
        return deps

    def op(self, e, fn, reads=(), writes=()):
        if self.dead:
            return None
        ex = [b for b in reads if b.excl]
        if ex:
            reads = [b for b in reads if not b.excl]
            writes = list(writes) + ex
        self._need(e, self._deps(e, reads, writes))
        ins = fn()
        self.cnt[e] += 1
        c = self.cnt[e]
        ins.then_inc(self.sem[e], 1)
        self.hist[e][c] = dict(self.seen[e])
        for b in reads:
            if b.r.get(e, 0) < c:
                b.r[e] = c
        for b in writes:
            b.w = (e, c)
            b.r = {}
        self.n_instr += 1
        self.per_eng[e] += 1
        return ins

    def dma(self, e, out, in_, reads=(), writes=(), **kw):
        if self.dead:
            return None
        kind = "sw" if e == "pool" else "hw"
        sk = self.dsem[kind][self.dnext[kind]]
        self.dnext[kind] = (self.dnext[kind] + 1) % len(self.dsem[kind])
        deps = self._deps(sk, reads, writes)
        if self.cnt[sk] > 0:
            deps.append((sk, self.cnt[sk]))
        self._need(e, deps)
        ins = self.eng[e].dma_start(out=out, in_=in_, **kw)
        self.cnt[sk] += 16
        c = self.cnt[sk]
        ins.then_inc(self.sem[sk], 16)
        self.hist[sk][c] = dict(self.seen[e])
        for b in reads:
            if b.r.get(sk, 0) < c:
                b.r[sk] = c
        for b in writes:
            b.w = (sk, c)
            b.r = {}
        self.n_instr += 1
        return ins

    def barrier(self):
        if self.dead:
            return
        allk = [(sk, v) for sk, v in self.cnt.items() if v > 0]
        for e in self.eng:
            self._need(e, [d for d in allk if d[0] != e])

    def finish(self, bufs, e="sp"):
        self._need(e, [b.w for b in bufs if b.w is not None])


class Rot:
    def __init__(self, k, name, n, shape, dtype, es=None, psum=False):
        self.items = []
        for i in range(n):
            t = (k.ps if psum else k.sb)("%s%d" % (name, i), shape, dtype, es)
            self.items.append((t, Buf(name)))
        self.i = 0

    def next(self):
        it = self.items[self.i]
        self.i = (self.i + 1) % len(self.items)
        return it


class NS:
    pass


class StopBuild(Exception):
    pass


def mark(S, name):
    if getattr(S, "stop_at", None) == name:
        S.k.dead = True


def setup_common(k, nc, S, L):
    S.L = L
    S.NT = L // 128
    S.X = k.sb("X", [128, S.NT, D], F32)
    S.bX = [Buf("X%d" % i) for i in range(S.NT)]
    S.ident = k.sb("ident", [128, 128], BF16)
    S.bident = Buf("ident")
    k.op("pool", lambda: nc.gpsimd.memset(S.ident[:], 1.0), writes=[S.bident])
    k.op("pool", lambda: nc.gpsimd.affine_select(
        out=S.ident[:], in_=S.ident[:], pattern=[[-1, 128]], compare_op=ALU.is_equal,
        fill=0.0, base=0, channel_multiplier=1), reads=[S.bident], writes=[S.bident])
    S.pb = [k.ps("pb%d" % i, [128, 512], F32) for i in range(7)]
    S.bpb = [Buf("pb%d" % i, excl=True) for i in range(7)]
    S.pT = k.ps("pT", [128, 1024], BF16)
    S.bpT = Buf("pT", excl=True)
    S.ss = Rot(k, "ss", 4, [128, 1], F32)
    S.rs = Rot(k, "rs", 4, [128, 1], F32)
    S.junk = k.sb("junk", [128, 1024], BF16)
    S.bjunk = Buf("junk")
    S.hb = Rot(k, "hb", 1, [128, 1024], BF16)
    S.gbc = Rot(k, "gbc", 1, [128, D], F32)


def load_gbc(k, nc, S, g_ap):
    t, b = S.gbc.next()
    k.dma("sp", t[:], g_ap.partition_broadcast(128), writes=[b])
    return t, b


def rstd_tile(k, nc, S, i):
    ss, bss = S.ss.next()
    rs, brs = S.rs.next()
    k.op("act", lambda: nc.scalar.activation(out=S.junk[:], in_=S.X[:, i, :], func=AF.Square,
                                             accum_out=ss[:]),
         reads=[S.bX[i]], writes=[S.bjunk, bss])
    k.op("act", lambda: nc.scalar.activation(out=ss[:], in_=ss[:], func=AF.Sqrt,
                                             scale=1.0 / D, bias=NORM_EPS),
         reads=[bss], writes=[bss])
    k.op("dve", lambda: nc.vector.reciprocal(out=rs[:], in_=ss[:]), reads=[bss], writes=[brs])
    return rs, brs


def norm_to_hT(k, nc, S, i, gbc, bgbc, dst_ap, bdst, evac_eng="act"):
    rs, brs = rstd_tile(k, nc, S, i)
    hb, bhb = S.hb.next()
    k.op("dve", lambda: nc.vector.scalar_tensor_tensor(
        out=hb[:], in0=S.X[:, i, :], scalar=rs[:], in1=gbc[:], op0=ALU.mult, op1=ALU.mult),
        reads=[S.bX[i], brs, bgbc], writes=[bhb])
    for c in range(DC):
        k.op("pe", lambda: nc.tensor.transpose(out=S.pT[:, c * 128:(c + 1) * 128],
                                               in_=hb[:, c * 128:(c + 1) * 128],
                                               identity=S.ident[:]),
             reads=[bhb, S.bident], writes=[S.bpT])
    src = S.pT[:].rearrange("p (c t) -> p c t", c=DC)
    if evac_eng == "act":
        k.op("act", lambda: nc.scalar.copy(out=dst_ap, in_=src), reads=[S.bpT], writes=[bdst])
    else:
        k.op("dve", lambda: nc.vector.tensor_copy(out=dst_ap, in_=src), reads=[S.bpT],
             writes=[bdst])


def phase_mlp(k, nc, S, g_ap, wup_ap, wdn_ap):
    L, NT = S.L, S.NT
    NTB = L // 512
    FP = 512
    NFP = DFF // FP
    FCP = FP // 128
    with ExitStack() as es:
        hT = k.sb("mlp_hT", [128, DC, L], BF16, es)
        bhT = [Buf("hT%d" % i) for i in range(NT)]
        uT = [k.sb("mlp_uT%d" % s, [128, FCP, L], BF16, es) for s in range(2)]
        buT = [[Buf("uT") for _ in range(NTB)] for s in range(2)]
        wup = [k.sb("mlp_wup%d" % s, [128, DC, FP], BF16, es) for s in range(2)]
        bwup = [Buf("wup") for s in range(2)]
        wdn = [k.sb("mlp_wdn%d" % s, [128, FCP, D], BF16, es) for s in range(2)]
        bwdn = [Buf("wdn") for s in range(2)]
        rtmp = Rot(k, "mlp_rt", 2, [128, 512], F32, es)

        def load_w(fp):
            s = fp % 2
            k.dma("pool", wup[s][:], wup_ap[:, fp * FP:(fp + 1) * FP].rearrange(
                "(c p) n -> p c n", p=128), writes=[bwup[s]])
            k.dma("pool", wdn[s][:], wdn_ap[fp * FP:(fp + 1) * FP, :].rearrange(
                "(c p) n -> p c n", p=128), writes=[bwdn[s]])

        load_w(0)
        load_w(1)
        gbc, bgbc = load_gbc(k, nc, S, g_ap)
        for i in range(NT):
            norm_to_hT(k, nc, S, i, gbc, bgbc, hT[:, :, i * 128:(i + 1) * 128], bhT[i],
                       evac_eng=("act" if i % 2 == 0 else "dve"))

        upb = [0, 1]
        dnb = [2, 3, 4, 5]
        cnt = {"u": 0, "d": 0}

        def up(fp):
            s = fp % 2
            for fc in range(FCP):
                for tb in range(NTB):
                    b = upb[cnt["u"] % 2]
                    cnt["u"] += 1
                    for dc in range(DC):
                        k.op("pe", lambda: nc.tensor.matmul(
                            out=S.pb[b][:], lhsT=wup[s][:, dc, fc * 128:(fc + 1) * 128],
                            rhs=hT[:, dc, tb * 512:(tb + 1) * 512],
                            start=(dc == 0), stop=(dc == DC - 1)),
                            reads=[bwup[s]] + bhT[tb * 4:(tb + 1) * 4], writes=[S.bpb[b]])
                    rt, brt = rtmp.next()
                    k.op("act", lambda: nc.scalar.activation(out=rt[:], in_=S.pb[b][:],
                                                             func=AF.Relu),
                         reads=[S.bpb[b]], writes=[brt])
                    k.op("dve", lambda: nc.vector.tensor_tensor(
                        out=uT[s][:, fc, tb * 512:(tb + 1) * 512], in0=rt[:], in1=rt[:],
                        op=ALU.mult), reads=[brt], writes=[buT[s][tb]])

        def down(fp):
            s = fp % 2
            for i in range(NT):
                for half in range(2):
                    b = dnb[cnt["d"] % 4]
                    cnt["d"] += 1
                    for fc in range(FCP):
                        k.op("pe", lambda: nc.tensor.matmul(
                            out=S.pb[b][:], lhsT=uT[s][:, fc, i * 128:(i + 1) * 128],
                            rhs=wdn[s][:, fc, half * 512:(half + 1) * 512],
                            start=(fc == 0), stop=(fc == FCP - 1)),
                            reads=[bwdn[s], buT[s][i // 4]], writes=[S.bpb[b]])
                    xs = S.X[:, i, half * 512:(half + 1) * 512]
                    k.op("dve", lambda: nc.vector.tensor_tensor(out=xs, in0=S.pb[b][:], in1=xs,
                                                                op=ALU.add),
                         reads=[S.bpb[b], S.bX[i]], writes=[S.bX[i]])
            if fp + 2 < NFP:
                load_w(fp + 2)

        up(0)
        for fp in range(NFP):
            if fp + 1 < NFP:
                up(fp + 1)
            down(fp)
        k.barrier()


def phase_final(k, nc, S, g_ap, out_ap, bout):
    with ExitStack() as es:
        ot = Rot(k, "fin_o", 2, [128, D], F32, es)
        gbc, bgbc = load_gbc(k, nc, S, g_ap)
        for i in range(S.NT):
            rs, brs = rstd_tile(k, nc, S, i)
            o, bo = ot.next()
            k.op("dve", lambda: nc.vector.scalar_tensor_tensor(
                out=o[:], in0=S.X[:, i, :], scalar=rs[:], in1=gbc[:], op0=ALU.mult,
                op1=ALU.mult), reads=[S.bX[i], brs, bgbc], writes=[bo])
            k.dma("sp", out_ap[i * 128:(i + 1) * 128, :], o[:], reads=[bo], writes=[bout])
        k.finish([bout], "sp")
        k.barrier()


PARAM_SHAPES = {
    "mixer_norm": [2, D], "mlp_norm": [2, D], "mlp_w_up": [2, D, DFF], "mlp_w_down": [2, DFF, D],
    "final_norm": [D], "dsa_w_in": [1, D, IN_COLS], "dsa_w_o": [1, D, D],
    "rwkv_mu": [1, 6, D], "rwkv_w_rkv": [1, 3, D, D], "rwkv_w0": [1, D], "rwkv_w1": [1, D, 64],
    "rwkv_w2": [1, 64, D], "rwkv_a0": [1, D], "rwkv_a1": [1, D, 64], "rwkv_a2": [1, 64, D],
    "rwkv_g1": [1, D, 160], "rwkv_g2": [1, 160, D], "rwkv_k_k": [1, D], "rwkv_k_a": [1, D],
    "rwkv_r_k": [1, 16, 64], "rwkv_lnx_w": [1, D], "rwkv_lnx_b": [1, D], "rwkv_w_o": [1, D, D],
}


def dbg(k, S, name, ap, reads):
    if name in S.dbg_t:
        t = S.dbg_t[name]
        b = Buf("dbg")
        k.dma("sp", t, ap, reads=reads, writes=[b])
        S.dbg_b.append(b)


def build(L=2048, phases=("dsa", "mlp0", "rwkv", "mlp1", "final"), debug=None, stop_at=None):
    nc = bass.Bass("TRN2", target_bir_lowering=False)
    P = NS()
    P.x = nc.dram_tensor("x", [L, D], F32, kind="ExternalInput").ap()
    for name, shp in PARAM_SHAPES.items():
        setattr(P, name, nc.dram_tensor(name, shp, F32, kind="ExternalInput").ap())
    P.out = nc.dram_tensor("out", [L, D], F32, kind="ExternalOutput").ap()
    with ExitStack() as es:
        k = K(nc, es)
        global LAST_K
        LAST_K = k
        S = NS()
        S.P = P
        S.dbg_t = {}
        S.stop_at = stop_at
        S.k = k
        S.dbg_b = []
        for name, (shp, dt_) in (debug or {}).items():
            S.dbg_t[name] = nc.dram_tensor("dbg_" + name, list(shp), dt_, kind="ExternalOutput").ap()
        setup_common(k, nc, S, L)
        for i in range(S.NT):
            k.dma("sp", S.X[:, i, :], P.x[i * 128:(i + 1) * 128, :], writes=[S.bX[i]])
        for ph in phases:
            if ph == "dsa":
                phase_dsa(k, nc, S)
            elif ph == "mlp0":
                phase_mlp(k, nc, S, P.mlp_norm[0], P.mlp_w_up[0], P.mlp_w_down[0])
            elif ph == "rwkv":
                phase_rwkv(k, nc, S)
                k.dead = False
                k.barrier()
            elif ph == "mlp1":
                phase_mlp(k, nc, S, P.mlp_norm[1], P.mlp_w_up[1], P.mlp_w_down[1])
            elif ph == "final":
                bout = Buf("out")
                phase_final(k, nc, S, P.final_norm, P.out, bout)
                k.finish([bout], "sp")
            elif ph == "dump":
                bout = Buf("out")
                for i in range(S.NT):
                    k.dma("sp", P.out[i * 128:(i + 1) * 128, :], S.X[:, i, :],
                          reads=[S.bX[i]], writes=[bout])
                k.finish([bout], "sp")
        k.finish(S.dbg_b, "sp")
        print("instr", k.n_instr, "waits", k.n_wait, k.per_eng)
    return nc


def phase_dsa(k, nc, S):
    L, NT, P = S.L, S.NT, S.P
    TOPK = min(256, L // 4)
    NIT = 12
    w_in = P.dsa_w_in[0]
    w_o = P.dsa_w_o[0]
    o0, o1, o2, o3, o4 = 1024, 1280, 1536, 2048, 2112
    NHALF = 2 if L >= 1024 else 1
    LH = L // NHALF
    NTH = LH // 128
    NTBH = LH // 512
    with ExitStack() as es:
        qT = k.sb("qT", [128, 8, L], BF16, es)
        bqT = [Buf("qT%d" % i) for i in range(NT)]
        kT = k.sb("kT", [128, 2, L], BF16, es)
        bkT = [Buf("kT%d" % i) for i in range(NT)]
        qiT = k.sb("qiT", [128, 4, L], BF16, es)
        bqiT = [Buf("qiT%d" % i) for i in range(NT)]
        kiT = k.sb("kiT", [128, 1, L], BF16, es)
        bkiT = [Buf("kiT%d" % i) for i in range(NT)]
        Vaug = k.sb("Vaug", [128, NT, KVH, 65], BF16, es)
        bV = [Buf("V%d" % i) for i in range(NT)]
        wi = k.sb("wi", [128, NT, 8], F32, es)
        bwi = [Buf("wi%d" % i) for i in range(NT)]
        ident4 = k.sb("ident4", [128, 4, 128], BF16, es)
        bident4 = Buf("ident4")
        pw2 = k.sb("pw2", [128, NIT + 2], F32, es)
        bpw2 = Buf("pw2")
        for g in range(4):
            k.op("pool", lambda: nc.gpsimd.tensor_copy(out=ident4[:, g, :], in_=S.ident[:]),
                 reads=[S.bident], writes=[bident4])
        for n in range(NIT + 2):
            k.op("pool", lambda: nc.gpsimd.memset(pw2[:, n:n + 1], 2.0 ** (-(n + 1))),
                 writes=[bpw2])
        k.op("pool", lambda: nc.gpsimd.memset(Vaug[:, :, :, 64:65], 1.0), writes=bV)

        with ExitStack() as es2:
            cosT = k.sb("cosT", [128, LH], F32, es2)
            sinT = k.sb("sinT", [128, LH], F32, es2)
            btab = Buf("tab")
            bhT = [Buf("hT%d" % i) for i in range(NTH)]
            wn = [k.sb("dsa_wn%d" % s, [128, DC, 2, 2, 64], BF16, es2) for s in range(2)]
            wr = [k.sb("dsa_wr%d" % s, [128, DC, 2, 2, 2, 32], BF16, es2) for s in range(2)]
            bwn = [Buf("wn") for s in range(2)]
            bwr = [Buf("wr") for s in range(2)]
            wv = k.sb("dsa_wv", [128, DC, 264], BF16, es2)
            bwv = Buf("wv")
            t1r = Rot(k, "rope_t1", 2, [128, 512], F32, es2)
            t2r = Rot(k, "rope_t2", 2, [128, 512], F32, es2)
            k.dma("pool", wv[:, :, 0:256], w_in[:, o1:o2].rearrange("(c p) n -> p c n", p=128),
                  writes=[bwv])
            k.dma("pool", wv[:, :, 256:264], w_in[:, o4:o4 + 8].rearrange("(c p) n -> p c n", p=128),
                  writes=[bwv])
            gbc, bgbc = load_gbc(k, nc, S, P.mixer_norm[0])

            with ExitStack() as es3:
                pidx = k.sb("pidx", [128, 1], I32, es3)
                pj = k.sb("pj", [128, 1], I32, es3)
                pjf = k.sb("pjf", [128, 1], F32, es3)
                inv = k.sb("rinv", [128, 1], F32, es3)
                sgn = k.sb("rsgn", [128, 1], F32, es3)
                bc = Buf("ropec")
                k.op("pool", lambda: nc.gpsimd.iota(out=pidx[:], pattern=[[0, 1]], base=0,
                                                    channel_multiplier=1), writes=[bc])
                k.op("dve", lambda: nc.vector.tensor_single_scalar(out=pj[:], in_=pidx[:], scalar=31,
                                                                   op=ALU.bitwise_and),
                     reads=[bc], writes=[bc])
                k.op("dve", lambda: nc.vector.tensor_copy(out=pjf[:], in_=pj[:]), reads=[bc], writes=[bc])
                k.op("act", lambda: nc.scalar.activation(out=inv[:], in_=pjf[:], func=AF.Exp,
                                                         scale=-2.0 * math.log(10000.0) / 64.0),
                     reads=[bc], writes=[bc])
                k.op("dve", lambda: nc.vector.tensor_single_scalar(out=pj[:], in_=pidx[:], scalar=32,
                                                                   op=ALU.bitwise_and),
                     reads=[bc], writes=[bc])
                k.op("dve", lambda: nc.vector.tensor_copy(out=pjf[:], in_=pj[:]), reads=[bc], writes=[bc])
                k.op("dve", lambda: nc.vector.tensor_scalar(out=sgn[:], in0=pjf[:], scalar1=2.0 / 32.0,
                                                            scalar2=-1.0, op0=ALU.mult, op1=ALU.add),
                     reads=[bc], writes=[bc])

                def make_tables(hf):
                    with ExitStack() as es4:
                        ti = k.sb("tpos_i", [128, LH], I32, es4)
                        ang = k.sb("ang", [128, LH], F32, es4)
                        rr = k.sb("rr", [128, LH], F32, es4)
                        ki_ = ti
                        mm = k.sb("mm", [128, LH], F32, es4)
                        bt = Buf("tabtmp")
                        TWO_PI = 2.0 * math.pi
                        k.op("pool", lambda: nc.gpsimd.iota(out=ti[:], pattern=[[1, LH]], base=hf * LH,
                                                            channel_multiplier=0), writes=[bt])
                        k.op("dve", lambda: nc.vector.tensor_copy(out=ang[:], in_=ti[:]), reads=[bt], writes=[bt])
                        k.op("dve", lambda: nc.vector.tensor_scalar(out=ang[:], in0=ang[:], scalar1=inv[:],
                                                                    scalar2=None, op0=ALU.mult),
                             reads=[bt, bc], writes=[bt])
                        for which, dst in (("sin", sinT), ("cos", cosT)):
                            off = 0.0 if which == "sin" else math.pi / 2
                            k.op("dve", lambda: nc.vector.tensor_scalar(
                                out=ki_[:], in0=ang[:], scalar1=off, scalar2=1.0 / TWO_PI,
                                op0=ALU.add, op1=ALU.mult), reads=[bt], writes=[bt])
                            k.op("dve", lambda: nc.vector.tensor_copy(out=mm[:], in_=ki_[:]),
                                 reads=[bt], writes=[bt])
                            k.op("dve", lambda: nc.vector.scalar_tensor_tensor(
                                out=rr[:], in0=mm[:], scalar=-TWO_PI, in1=ang[:], op0=ALU.mult,
                                op1=ALU.add), reads=[bt], writes=[bt])
                            if off != 0.0:
                                k.op("dve", lambda: nc.vector.tensor_scalar(
                                    out=rr[:], in0=rr[:], scalar1=off, scalar2=None, op0=ALU.add),
                                    reads=[bt], writes=[bt])
                            k.op("dve", lambda: nc.vector.tensor_scalar(
                                out=mm[:], in0=rr[:], scalar1=math.pi, scalar2=-TWO_PI,
                                op0=ALU.is_gt, op1=ALU.mult), reads=[bt], writes=[bt])
                            k.op("dve", lambda: nc.vector.tensor_tensor(out=rr[:], in0=rr[:], in1=mm[:],
                                                                        op=ALU.add), reads=[bt], writes=[bt])
                            k.op("dve", lambda: nc.vector.tensor_scalar(
                                out=mm[:], in0=rr[:], scalar1=-math.pi, scalar2=TWO_PI,
                                op0=ALU.is_lt, op1=ALU.mult), reads=[bt], writes=[bt])
                            k.op("dve", lambda: nc.vector.tensor_tensor(out=rr[:], in0=rr[:], in1=mm[:],
                                                                        op=ALU.add), reads=[bt], writes=[bt])
                            k.op("dve", lambda: nc.vector.tensor_scalar(
                                out=rr[:], in0=rr[:], scalar1=-3.14159, scalar2=3.14159,
                                op0=ALU.max, op1=ALU.min), reads=[bt], writes=[bt])
                            k.op("act", lambda: nc.scalar.activation(out=dst[:], in_=rr[:], func=AF.Sin),
                                 reads=[bt], writes=[btab, bt])
                        k.op("dve", lambda: nc.vector.tensor_scalar(out=sinT[:], in0=sinT[:], scalar1=sgn[:],
                                                                    scalar2=None, op0=ALU.mult),
                             reads=[btab, bc], writes=[btab])
                        k.barrier()

                specs = []
                for c in range(8):
                    specs.append((qT, c, bqT, c * 64, (c + 8) * 64))
                for c in range(2):
                    specs.append((kT, c, bkT, o0 + c * 64, o0 + (c + 2) * 64))
                for c in range(4):
                    specs.append((qiT, c, bqiT, o2 + c * 64, o2 + (c + 4) * 64))
                specs.append((kiT, 0, bkiT, o3, o3))
                blocks = [specs[i:i + 2] for i in range(0, len(specs), 2)]

                def load_block(bi):
                    s = bi % 2
                    for cc, (_, _, _, lo, up) in enumerate(blocks[bi]):
                        for h, col in enumerate((lo, up)):
                            k.dma("pool", wn[s][:, :, cc, h, :],
                                  w_in[:, col:col + 64].rearrange("(c p) n -> p c n", p=128),
                                  writes=[bwn[s]])
                    wn5 = wn[s][:].rearrange("p c a h (r e) -> p (c a h) r e", r=2)
                    wr5 = wr[s][:].rearrange("p c a h r e -> p (c a h) r e")
                    for r in range(2):
                        k.op("act", lambda: nc.scalar.copy(out=wr5[:, :, r, :], in_=wn5[:, :, 1 - r, :]),
                             reads=[bwn[s]], writes=[bwr[s]])

                cntp = {"a": 0}
                for hf in range(NHALF):
                    make_tables(hf)
                    es5 = ExitStack()
                    hT = k.sb("dsa_hT", [128, DC, LH], BF16, es5)
                    load_block(0)
                    load_block(1)
                    for it in range(NTH):
                        gi = hf * NTH + it
                        norm_to_hT(k, nc, S, gi, gbc, bgbc, hT[:, :, it * 128:(it + 1) * 128], bhT[it],
                                   evac_eng=("act" if it % 2 == 0 else "dve"))
                    for it in range(NTH):
                        gi = hf * NTH + it
                        for dc in range(DC):
                            k.op("pe", lambda: nc.tensor.matmul(
                                out=S.pb[4][:, 0:264], lhsT=hT[:, dc, it * 128:(it + 1) * 128],
                                rhs=wv[:, dc, :], start=(dc == 0), stop=(dc == DC - 1)),
                                reads=[bhT[it], bwv], writes=[S.bpb[4]])
                        k.op("act", lambda: nc.scalar.copy(
                            out=Vaug[:, gi, :, 0:64],
                            in_=S.pb[4][:, 0:256].rearrange("p (n e) -> p n e", n=KVH)),
                            reads=[S.bpb[4]], writes=[bV[gi]])
                        k.op("act", lambda: nc.scalar.mul(out=wi[:, gi, :], in_=S.pb[4][:, 256:264],
                                                          mul=(8.0 ** -0.5) * (64.0 ** -0.5)),
                             reads=[S.bpb[4]], writes=[bwi[gi]])
                    for bi, blk in enumerate(blocks):
                        s = bi % 2
                        for cc, (dst, dci, bdst, lo, up) in enumerate(blk):
                            for tb in range(NTBH):
                                ba = cntp["a"] % 2
                                bb = 2 + cntp["a"] % 2
                                cntp["a"] += 1
                                for dc in range(DC):
                                    k.op("pe", lambda: nc.tensor.matmul(
                                        out=S.pb[ba][:], lhsT=wn[s][:, dc, cc, :, :],
                                        rhs=hT[:, dc, tb * 512:(tb + 1) * 512],
                                        start=(dc == 0), stop=(dc == DC - 1)),
                                        reads=[bwn[s]] + bhT[tb * 4:(tb + 1) * 4], writes=[S.bpb[ba]])
                                for dc in range(DC):
                                    k.op("pe", lambda: nc.tensor.matmul(
                                        out=S.pb[bb][:], lhsT=wr[s][:, dc, cc, :, :, :],
                                        rhs=hT[:, dc, tb * 512:(tb + 1) * 512],
                                        start=(dc == 0), stop=(dc == DC - 1)),
                                        reads=[bwr[s]] + bhT[tb * 4:(tb + 1) * 4], writes=[S.bpb[bb]])
                                t1, bt1 = t1r.next()
                                t2, bt2 = t2r.next()
                                tsl = slice(tb * 512, (tb + 1) * 512)
                                k.op("dve", lambda: nc.vector.tensor_tensor(
                                    out=t1[:], in0=S.pb[ba][:], in1=cosT[:, tsl], op=ALU.mult),
                                    reads=[S.bpb[ba], btab], writes=[bt1])
                                k.op("dve", lambda: nc.vector.tensor_tensor(
                                    out=t2[:], in0=S.pb[bb][:], in1=sinT[:, tsl], op=ALU.mult),
                                    reads=[S.bpb[bb], btab], writes=[bt2])
                                g0 = hf * LH + tb * 512
                                gt = g0 // 128
                                k.op("pool", lambda: nc.gpsimd.tensor_tensor(
                                    out=dst[:, dci, g0:g0 + 512], in0=t1[:], in1=t2[:], op=ALU.add),
                                    reads=[bt1, bt2], writes=bdst[gt:gt + 4])
                        if bi + 2 < len(blocks):
                            load_block(bi + 2)
                    k.barrier()
                    es5.close()

        k.barrier()
        dbg(k, S, "qT", qT[:], bqT)
        dbg(k, S, "kT", kT[:], bkT)
        dbg(k, S, "qiT", qiT[:], bqiT)
        dbg(k, S, "kiT", kiT[:], bkiT)
        dbg(k, S, "Vaug", Vaug[:], bV)
        dbg(k, S, "wi", wi[:], bwi)
        with ExitStack() as es2:
            accR = Rot(k, "acc", 2, [128, L], F32, es2)
            amR = Rot(k, "amask", 2, [128, L], BF16, es2)
            junk2 = k.sb("junk2", [128, L], BF16, es2)
            bjunk2 = Buf("junk2")
            rlR = Rot(k, "rl", 2, [128, 512], F32, es2)
            ptR = Rot(k, "PT", 3, [128, 4, 128], BF16, es2)
            osR = Rot(k, "osb", 2, [128, D], BF16, es2)
            smR = Rot(k, "bis", 2, [128, 16], F32, es2)
            stR = Rot(k, "bstp", 2, [128, 2 * (NIT + 2)], F32, es2)
            recR = Rot(k, "rec", 2, [128, 4], F32, es2)
            cz = {"z": 0, "s": 0, "o": 0}
            tile_state = {}

            def gen_index(i):
                W = (i + 1) * 128
                acc, bacc = accR.next()
                am, bam = amR.next()
                sm, bsm = smR.next()
                tile_state[i] = (acc, bacc, am, bam, sm, bsm)
                for h in range(IDXH):
                    c = h % 4
                    plo = 64 * (h // 4)
                    for w0 in range(0, W, 512):
                        w1 = min(W, w0 + 512)
                        zb = cz["z"] % 2
                        cz["z"] += 1
                        k.op("pe", lambda: nc.tensor.matmul(
                            out=S.pb[zb][:, 0:w1 - w0], lhsT=qiT[plo:plo + 64, c, i * 128:(i + 1) * 128],
                            rhs=kiT[plo:plo + 64, 0, w0:w1], start=True, stop=True),
                            reads=[bqiT[i]] + bkiT[w0 // 128:w1 // 128], writes=[S.bpb[zb]])
                        rl, brl = rlR.next()
                        k.op("act", lambda: nc.scalar.activation(out=rl[:, 0:w1 - w0],
                                                                 in_=S.pb[zb][:, 0:w1 - w0], func=AF.Relu),
                             reads=[S.bpb[zb]], writes=[brl])
                        if h == 0:
                            k.op("dve", lambda: nc.vector.tensor_scalar(
                                out=acc[:, w0:w1], in0=rl[:, 0:w1 - w0], scalar1=wi[:, i, 0:1],
                                scalar2=None, op0=ALU.mult), reads=[brl, bwi[i]], writes=[bacc])
                        else:
                            k.op("dve", lambda: nc.vector.scalar_tensor_tensor(
                                out=acc[:, w0:w1], in0=rl[:, 0:w1 - w0], scalar=wi[:, i, h:h + 1],
                                in1=acc[:, w0:w1], op0=ALU.mult, op1=ALU.add),
                                reads=[brl, bwi[i], bacc], writes=[bacc])
                        yield
                k.op("pool", lambda: nc.gpsimd.affine_select(
                    out=acc[:, i * 128:W], in_=acc[:, i * 128:W], pattern=[[-1, 128]],
                    compare_op=ALU.is_ge, fill=-1e30, base=0, channel_multiplier=1),
                    reads=[bacc], writes=[bacc])
                yield

            def gen_bisect(i):
                W = (i + 1) * 128
                acc, bacc, am, bam, sm, bsm = tile_state[i]
                if W <= TOPK:
                    k.op("dve", lambda: nc.vector.tensor_scalar(
                        out=am[:, 0:W], in0=acc[:, 0:W], scalar1=-1e29, scalar2=NEG,
                        op0=ALU.is_lt, op1=ALU.mult), reads=[bacc], writes=[bam])
                    yield
                    return
                st, bst = stR.next()
                hi_, lo_, rng_, mid, cntv, tt, thr = [sm[:, j:j + 1] for j in range(7)]
                k.op("dve", lambda: nc.vector.tensor_reduce(out=hi_, in_=acc[:, 0:W], axis=AX.X,
                                                            op=ALU.max), reads=[bacc], writes=[bsm])
                k.op("dve", lambda: nc.vector.tensor_reduce(out=lo_, in_=acc[:, 0:i * 128], axis=AX.X,
                                                            op=ALU.min), reads=[bacc], writes=[bsm])
                k.op("dve", lambda: nc.vector.tensor_tensor(out=rng_, in0=hi_, in1=lo_,
                                                            op=ALU.subtract), reads=[bsm], writes=[bsm])
                k.op("dve", lambda: nc.vector.tensor_scalar(
                    out=st[:, 0:NIT + 2], in0=pw2[:, :], scalar1=rng_, scalar2=None, op0=ALU.mult),
                    reads=[bsm, bpw2], writes=[bst])
                k.op("dve", lambda: nc.vector.tensor_scalar(
                    out=st[:, NIT + 2:2 * (NIT + 2)], in0=pw2[:, :], scalar1=rng_, scalar2=2.0,
                    op0=ALU.mult, op1=ALU.mult), reads=[bsm, bpw2], writes=[bst])
                k.op("dve", lambda: nc.vector.tensor_tensor(out=mid, in0=lo_, in1=st[:, 0:1],
                                                            op=ALU.add), reads=[bsm, bst], writes=[bsm])
                yield
                for n in range(NIT + 1):
                    k.op("dve", lambda: nc.vector.tensor_scalar(
                        out=junk2[:, 0:W], in0=acc[:, 0:W], scalar1=mid, scalar2=0.0,
                        op0=ALU.is_ge, op1=ALU.add, accum_out=cntv),
                        reads=[bacc, bsm], writes=[bjunk2, bsm])
                    if n < NIT:
                        k.op("dve", lambda: nc.vector.tensor_scalar(
                            out=tt, in0=cntv, scalar1=TOPK - 0.5, scalar2=st[:, NIT + 2 + n + 1:NIT + 2 + n + 2],
                            op0=ALU.is_ge, op1=ALU.mult), reads=[bsm, bst], writes=[bsm])
                        k.op("dve", lambda: nc.vector.scalar_tensor_tensor(
                            out=mid, in0=tt, scalar=st[:, n + 1:n + 2], in1=mid, op0=ALU.subtract,
                            op1=ALU.add), reads=[bsm, bst], writes=[bsm])
                    else:
                        k.op("dve", lambda: nc.vector.tensor_scalar(
                            out=tt, in0=cntv, scalar1=TOPK - 0.5, scalar2=st[:, n:n + 1],
                            op0=ALU.is_lt, op1=ALU.mult), reads=[bsm, bst], writes=[bsm])
                        k.op("dve", lambda: nc.vector.tensor_tensor(out=thr, in0=mid, in1=tt,
                                                                    op=ALU.subtract),
                             reads=[bsm], writes=[bsm])
                    yield
                k.op("dve", lambda: nc.vector.tensor_scalar(
                    out=am[:, 0:W], in0=acc[:, 0:W], scalar1=thr, scalar2=NEG,
                    op0=ALU.is_lt, op1=ALU.mult), reads=[bacc, bsm], writes=[bam])
                yield

            def gen_attn(i):
                acc, bacc, am, bam, sm, bsm = tile_state[i]
                osb, bosb = osR.next()
                for n in range(KVH):
                    plo = 64 * (n // 2)
                    qc0 = 4 * (n % 2)
                    ob = 4 + cz["o"] % 2
                    cz["o"] += 1
                    for j in range(i + 1):
                        sbk = 2 + cz["s"] % 2
                        cz["s"] += 1
                        k.op("pe", lambda: nc.tensor.matmul(
                            out=S.pb[sbk][:].rearrange("p (g t) -> p g t", g=4),
                            lhsT=kT[plo:plo + 64, n % 2, j * 128:(j + 1) * 128],
                            rhs=qT[plo:plo + 64, qc0:qc0 + 4, i * 128:(i + 1) * 128],
                            start=True, stop=False),
                            reads=[bkT[j], bqT[i]], writes=[S.bpb[sbk]])
                        k.op("pe", lambda: nc.tensor.matmul(
                            out=S.pb[sbk][:].rearrange("p (g t) -> p g t", g=4),
                            lhsT=am[:, j * 128:(j + 1) * 128], rhs=ident4[:, :, :],
                            start=False, stop=True),
                            reads=[bam, bident4], writes=[S.bpb[sbk]])
                        pt, bpt = ptR.next()
                        k.op("act", lambda: nc.scalar.activation(
                            out=pt[:].rearrange("p g t -> p (g t)"), in_=S.pb[sbk][:], func=AF.Exp,
                            scale=0.125), reads=[S.bpb[sbk]], writes=[bpt])
                        for g in range(4):
                            k.op("pe", lambda: nc.tensor.matmul(
                                out=S.pb[ob][:, g * 65:(g + 1) * 65], lhsT=pt[:, g, :],
                                rhs=Vaug[:, j, n, :], start=(j == 0 and g == 0), stop=(j == i),
                                skip_group_check=True),
                                reads=[bpt, bV[j]], writes=[S.bpb[ob]])
                        yield
                    rec, brec = recR.next()
                    ov = S.pb[ob][:, 0:260].rearrange("p (g e) -> p g e", g=4)
                    k.op("dve", lambda: nc.vector.reciprocal(out=rec[:].rearrange("p (g o) -> p g o", o=1),
                                                             in_=ov[:, :, 64:65]),
                         reads=[S.bpb[ob]], writes=[brec])
                    k.op("dve", lambda: nc.vector.tensor_tensor(
                        out=osb[:, n * 256:(n + 1) * 256].rearrange("p (g e) -> p g e", g=4),
                        in0=ov[:, :, 0:64],
                        in1=rec[:].rearrange("p (g o) -> p g o", o=1).to_broadcast([128, 4, 64]),
                        op=ALU.mult), reads=[S.bpb[ob], brec], writes=[bosb])
                for c in range(DC):
                    k.op("pe", lambda: nc.tensor.transpose(out=S.pT[:, c * 128:(c + 1) * 128],
                                                           in_=osb[:, c * 128:(c + 1) * 128],
                                                           identity=S.ident[:]),
                         reads=[bosb, S.bident], writes=[S.bpT])
                k.op("act", lambda: nc.scalar.copy(out=qT[:, :, i * 128:(i + 1) * 128],
                                                   in_=S.pT[:].rearrange("p (c t) -> p c t", c=DC)),
                     reads=[S.bpT], writes=[bqT[i]])
                yield

            def chain(*gens):
                for g in gens:
                    for _ in g:
                        yield

            for _ in chain(gen_index(0), gen_bisect(0)):
                pass
            for i in range(NT):
                a_steps = 4 * (i + 1) + 1
                if i + 1 < NT:
                    nxt = chain(gen_index(i + 1), gen_bisect(i + 1))
                    Wn = (i + 2) * 128
                    n_steps = IDXH * ((Wn + 511) // 512) + 1 + (NIT + 3 if Wn > TOPK else 1)
                else:
                    nxt = iter(())
                    n_steps = 0
                done = 0
                for si, _ in enumerate(gen_attn(i)):
                    target = (n_steps * (si + 1) + a_steps - 1) // a_steps
                    while done < target:
                        try:
                            next(nxt)
                        except StopIteration:
                            break
                        done += 1
                for _ in nxt:
                    pass
        k.barrier()
        with ExitStack() as es2:
            wo = k.sb("dsa_wo", [128, DC, D], BF16, es2)
            bwo = Buf("wo")
            for c in range(DC):
                k.dma("pool", wo[:, c, :], w_o[c * 128:(c + 1) * 128, :], writes=[bwo])
            cc = 0
            for i in range(NT):
                for half in range(2):
                    b = 2 + cc % 4
                    cc += 1
                    for c in range(DC):
                        k.op("pe", lambda: nc.tensor.matmul(
                            out=S.pb[b][:], lhsT=qT[:, c, i * 128:(i + 1) * 128],
                            rhs=wo[:, c, half * 512:(half + 1) * 512], start=(c == 0), stop=(c == DC - 1)),
                            reads=[bqT[i], bwo], writes=[S.bpb[b]])
                    xs = S.X[:, i, half * 512:(half + 1) * 512]
                    k.op("dve", lambda: nc.vector.tensor_tensor(out=xs, in0=S.pb[b][:], in1=xs, op=ALU.add),
                         reads=[S.bpb[b], S.bX[i]], writes=[S.bX[i]])
            k.barrier()


def phase_rwkv(k, nc, S):
    L, NT, P = S.L, S.NT, S.P
    G, CH, NHG, FC = 2, 512, 8, 4
    CDEC = math.exp(-0.5)
    mu = P.rwkv_mu[0]

    def v3(ap, a):
        return ap.rearrange("p (a b) -> p a b", a=a)

    def bc3(ap2, a, b):
        return ap2.unsqueeze(2).to_broadcast([128, a, b])

    with ExitStack() as es:
        triI = k.sb("triI", [128, 128], F32, es)
        triS = k.sb("triS", [128, 128], F32, es)
        negc = k.sb("negc", [128, 1], F32, es)
        selw = k.sb("selw", [2, 128], F32, es)
        sela = k.sb("sela", [2, 128], F32, es)
        rows = k.sb("rows", [2, CH], F32, es)
        maskL = k.sb("maskL", [128, 128], BF16, es)
        maskU = k.sb("maskU", [128, 128], BF16, es)
        maskUi = k.sb("maskUi", [128, 128], BF16, es)
        muT = k.sb("muT", [128, 6, DC], F32, es)
        bcv = k.sb("bcv", [128, 5, CH], BF16, es)
        wl1 = k.sb("wl1", [128, DC, 288], BF16, es)
        yo0 = k.sb("yo0", [128, NT, CH], BF16, es)
        byo0 = [Buf("yo0") for _ in range(NT)]
        bconst = Buf("rconst")
        k.op("pool", lambda: nc.gpsimd.memset(triI[:], -CDEC), writes=[bconst])
        k.op("pool", lambda: nc.gpsimd.affine_select(out=triI[:], in_=triI[:], pattern=[[1, 128]],
                                                     compare_op=ALU.is_ge, fill=0.0, base=0,
                                                     channel_multiplier=-1), reads=[bconst], writes=[bconst])
        k.op("pool", lambda: nc.gpsimd.memset(triS[:], -CDEC), writes=[bconst])
        k.op("pool", lambda: nc.gpsimd.affine_select(out=triS[:], in_=triS[:], pattern=[[1, 128]],
                                                     compare_op=ALU.is_gt, fill=0.0, base=0,
                                                     channel_multiplier=-1), reads=[bconst], writes=[bconst])
        k.op("pool", lambda: nc.gpsimd.memset(negc[:], -CDEC), writes=[bconst])
        for sel_, base_ in ((selw, 0), (sela, -1)):
            k.op("pool", lambda: nc.gpsimd.memset(sel_[:], 1.0), writes=[bconst])
            k.op("pool", lambda: nc.gpsimd.affine_select(out=sel_[:], in_=sel_[:], pattern=[[0, 128]],
                                                         compare_op=ALU.is_equal, fill=0.0, base=base_,
                                                         channel_multiplier=1), reads=[bconst], writes=[bconst])
        for m_, (cmp_, cm, pat) in ((maskL, (ALU.is_gt, 1, -1)), (maskU, (ALU.is_gt, -1, 1)),
                                    (maskUi, (ALU.is_ge, -1, 1))):
            k.op("pool", lambda: nc.gpsimd.memset(m_[:], 1.0), writes=[bconst])
            k.op("pool", lambda: nc.gpsimd.affine_select(out=m_[:], in_=m_[:], pattern=[[pat, 128]],
                                                         compare_op=cmp_, fill=0.0, base=0,
                                                         channel_multiplier=cm), reads=[bconst], writes=[bconst])
        NLEVS = 7
        lmask = k.sb("lmask", [128, NLEVS, 2, 128], BF16, es)
        with ExitStack() as esm:
            iI = k.sb("iI", [128, 128], I32, esm)
            iJ = k.sb("iJ", [128, 128], I32, esm)
            iT = k.sb("iT", [128, 128], I32, esm)
            eqm = k.sb("eqm", [128, 128], BF16, esm)
            bm = Buf("lmtmp")
            k.op("pool", lambda: nc.gpsimd.iota(out=iI[:], pattern=[[0, 128]], base=0, channel_multiplier=1), writes=[bm])
            k.op("pool", lambda: nc.gpsimd.iota(out=iJ[:], pattern=[[1, 128]], base=0, channel_multiplier=0), writes=[bm])
            k.op("dve", lambda: nc.vector.tensor_tensor(out=iI[:], in0=iI[:], in1=iJ[:], op=ALU.bitwise_xor),
                 reads=[bm], writes=[bm])
            for l in range(1, NLEVS + 1):
                k.op("dve", lambda: nc.vector.tensor_single_scalar(out=iT[:], in_=iI[:], scalar=l - 1,
                                                                   op=ALU.logical_shift_right), reads=[bm], writes=[bm])
                k.op("dve", lambda: nc.vector.tensor_single_scalar(out=eqm[:], in_=iT[:], scalar=1, op=ALU.is_equal),
                     reads=[bm], writes=[bm])
                k.op("dve", lambda: nc.vector.tensor_tensor(out=lmask[:, l - 1, 0, :], in0=eqm[:], in1=maskL[:], op=ALU.mult),
                     reads=[bm, bconst], writes=[bconst])
                k.op("dve", lambda: nc.vector.tensor_tensor(out=lmask[:, l - 1, 1, :], in0=eqm[:], in1=maskU[:], op=ALU.mult),
                     reads=[bm, bconst], writes=[bconst])
            k.barrier()
        with nc.allow_non_contiguous_dma(reason="tiny per-channel vectors"):
            for c in range(6):
                k.dma("sp", muT[:, c, :], mu[c].rearrange("(dc p) -> p dc", p=128), writes=[bconst])
        k.dma("pool", wl1[:, :, 0:64], P.rwkv_w1[0].rearrange("(c p) n -> p c n", p=128), writes=[bconst])
        k.dma("pool", wl1[:, :, 64:128], P.rwkv_a1[0].rearrange("(c p) n -> p c n", p=128), writes=[bconst])
        k.dma("pool", wl1[:, :, 128:288], P.rwkv_g1[0].rearrange("(c p) n -> p c n", p=128), writes=[bconst])
        gbc, bgbc = load_gbc(k, nc, S, P.mixer_norm[1])

        mark(S, "const")
        wr_ = k.sb("rw_wr", [128, DC, CH], BF16, es)
        wk_ = k.sb("rw_wk", [128, DC, CH], BF16, es)
        wv_ = k.sb("rw_wv", [128, DC, CH], BF16, es)
        wa2 = k.sb("rw_wa2", [64, CH], BF16, es)
        a2g_ = k.sb("rw_a2g", [64, CH], BF16, es)
        g2a = k.sb("rw_g2a", [128, CH], BF16, es)
        g2b = k.sb("rw_g2b", [32, CH], BF16, es)
        bw = Buf("rw_w")
        wo = k.sb("rw_wo", [128, DC, D], BF16, es)
        bwo = Buf("rw_wo")
        hc1 = k.sb("rw_hc", [128, DC, 129], BF16, es)
        hc = [hc1, hc1]
        bhc1 = Buf("hc")
        bhc = [bhc1, bhc1]
        dx = k.sb("rw_dx", [128, DC, 128], BF16, es)
        bdx = Buf("dx")
        xsR = Rot(k, "rw_xs", 2, [128, DC, 128], BF16, es)
        Fp = [k.sb("rw_F%d" % i, [128, CH], F32, es) for i in range(8)]
        bF = [Buf("F%d" % i) for i in range(8)]
        small = k.sb("rw_small", [128, 64], F32, es)
        bsmall = Buf("small")
        lT = k.sb("rw_lT", [128, 3, 128], BF16, es)
        g1b = k.sb("rw_g1b", [32, 128], BF16, es)
        blT = Buf("lT")
        Vb = k.sb("rw_V", [128, CH], BF16, es)
        bVb = Buf("V")
        tok4 = k.sb("rw_tok4", [128, 4, CH], BF16, es)
        btok = [Buf("tok%d" % i) for i in range(4)]
        KRT = k.sb("rw_KRT", [128, FC, 2, 128], BF16, es)
        KBT = k.sb("rw_KBT", [128, 2, FC, 128], BF16, es)
        bKRT = Buf("KRT")
        bKBT = Buf("KBT")
        Pm = [k.sb("rw_P%d" % i, [128, NHG, 128], BF16, es) for i in range(2)]
        PTm = [k.sb("rw_PT%d" % i, [128, NHG, 128], BF16, es) for i in range(2)]
        bPm = [[Buf("P"), Buf("P")] for _ in range(2)]
        bPTm = [[Buf("PT"), Buf("PT")] for _ in range(2)]
        Ym = [k.sb("rw_Y%d" % i, [128, NHG, 128], BF16, es) for i in range(2)]
        bYm = [[Buf("Y"), Buf("Y")] for _ in range(2)]
        QT = PTm[1]
        ArbT = k.sb("rw_ArbT", [128, NHG, 128], BF16, es)
        AkkT = k.sb("rw_AkkT", [128, NHG, 128], BF16, es)
        ArkT = k.sb("rw_ArkT", [128, NHG, 128], BF16, es)
        bArb, bAkk, bArk = Buf("Arb"), Buf("Akk"), Buf("Ark")
        Hf = k.sb("rw_Hf", [128, FC * 64], F32, es)
        Hb = k.sb("rw_Hb", [128, FC * 64], BF16, es)
        bHf, bHb = Buf("Hf"), Buf("Hb")
        pC = k.sb("rw_pC", [128, FC], F32, es)
        bpC = Buf("pC")
        Zs = k.sb("rw_Zs", [128, CH], BF16, es)
        Us = k.sb("rw_Us", [128, CH], BF16, es)
        bZs, bUs = Buf("Zs"), Buf("Us")
        yo = Zs
        byo = bZs
        pb, bpb = S.pb, S.bpb
        prot = {"i": 0}

        def pbank():
            b = prot["i"] % 7
            prot["i"] += 1
            return b

        for hg in range(G):
            c0 = hg * CH
            for wt, ci in ((wr_, 0), (wk_, 1), (wv_, 2)):
                k.dma("pool", wt[:], P.rwkv_w_rkv[0][ci][:, c0:c0 + CH].rearrange("(c p) n -> p c n", p=128),
                      writes=[bw])
            k.dma("pool", wa2[0:64, :], P.rwkv_w2[0][:, c0:c0 + CH], writes=[bw])
            k.dma("pool", a2g_[:], P.rwkv_a2[0][:, c0:c0 + CH], writes=[bw])
            k.dma("pool", g2a[:], P.rwkv_g2[0][0:128, c0:c0 + CH], writes=[bw])
            k.dma("pool", g2b[:], P.rwkv_g2[0][128:160, c0:c0 + CH], writes=[bw])
            k.dma("sp", rows[0:1, :], P.rwkv_w0[0:1, c0:c0 + CH], writes=[bw])
            k.dma("sp", rows[1:2, :], P.rwkv_a0[0:1, c0:c0 + CH], writes=[bw])
            for j, vec in enumerate((P.rwkv_k_k[0], P.rwkv_k_a[0], P.rwkv_r_k[0].rearrange("h n -> (h n)"),
                                     P.rwkv_lnx_w[0], P.rwkv_lnx_b[0])):
                k.dma("pool", bcv[:, j, :], vec[c0:c0 + CH].partition_broadcast(128), writes=[bw])
            if hg == G - 1:
                for c in range(DC):
                    k.dma("pool", wo[:, c, :], P.rwkv_w_o[0][c * 128:(c + 1) * 128, :], writes=[bwo])
            k.op("dve", lambda: nc.vector.memset(Hf[:], 0.0), writes=[bHf])
            k.op("dve", lambda: nc.vector.memset(Hb[:], 0.0), writes=[bHb])
            k.op("dve", lambda: nc.vector.memset(hc[0][:, :, 0:1], 0.0), writes=[bhc[0]])

            for n in range(NT):
                cur, nxt = n % 2, (n + 1) % 2
                h_ = hc[cur]
                norm_to_hT(k, nc, S, n, gbc, bgbc, h_[:, :, 1:129], bhc[cur],
                           evac_eng=("act" if n % 2 == 0 else "dve"))
                mark(S, "P0")
                k.op("dve", lambda: nc.vector.tensor_tensor(out=dx[:], in0=h_[:, :, 0:128], in1=h_[:, :, 1:129],
                                                            op=ALU.subtract), reads=[bhc[cur]], writes=[bdx])
                if n + 1 < NT:
                    k.op("dve", lambda: nc.vector.tensor_copy(out=h_[:, :, 0:1], in_=h_[:, :, 128:129]),
                         reads=[bhc[cur]], writes=[bhc[cur]])

                def mix(c):
                    xs, bxs = xsR.next()
                    k.op("dve", lambda: nc.vector.tensor_tensor(out=xs[:], in0=dx[:],
                                                                in1=bc3(muT[:, c, :], DC, 128), op=ALU.mult),
                         reads=[bdx, bconst], writes=[bxs])
                    k.op("pool", lambda: nc.gpsimd.tensor_tensor(out=xs[:], in0=xs[:], in1=h_[:, :, 1:129],
                                                                 op=ALU.add), reads=[bxs, bhc[cur]], writes=[bxs])
                    return xs, bxs

                def proj_tok(xs, bxs, wt, extra=None):
                    b = pbank()
                    nmm = DC + (1 if extra else 0)
                    for dc in range(DC):
                        k.op("pe", lambda: nc.tensor.matmul(out=pb[b][:], lhsT=xs[:, dc, :], rhs=wt[:, dc, :],
                                                            start=(dc == 0), stop=(dc == nmm - 1)),
                             reads=[bxs, bw], writes=[bpb[b]])
                    return b

                mark(S, "P1")
                R_, K_, SG, A_, PP, PINV, GG, TA = range(8)
                PPREV = PP
                xs, bxs = mix(0)
                b = proj_tok(xs, bxs, wr_)
                k.op("act", lambda: nc.scalar.copy(out=Fp[R_][:], in_=pb[b][:]), reads=[bpb[b]], writes=[bF[R_]])
                xs, bxs = mix(1)
                b = proj_tok(xs, bxs, wk_)
                k.op("act", lambda: nc.scalar.copy(out=Fp[K_][:], in_=pb[b][:]), reads=[bpb[b]], writes=[bF[K_]])
                xs, bxs = mix(2)
                b = proj_tok(xs, bxs, wv_)
                k.op("act", lambda: nc.scalar.copy(out=Vb[:], in_=pb[b][:]), reads=[bpb[b]], writes=[bVb])
                mark(S, "P2a")
                b = pbank()
                xs, bxs = mix(3)
                for dc in range(DC):
                    k.op("pe", lambda: nc.tensor.matmul(out=pb[b][0:64, 0:128], lhsT=wl1[:, dc, 0:64], rhs=xs[:, dc, :],
                                                        start=(dc == 0), stop=(dc == DC - 1), skip_group_check=True),
                         reads=[bxs, bconst], writes=[bpb[b]])
                xs, bxs = mix(4)
                for dc in range(DC):
                    k.op("pe", lambda: nc.tensor.matmul(out=pb[b][0:64, 384:512], lhsT=wl1[:, dc, 64:128], rhs=xs[:, dc, :],
                                                        start=False, stop=(dc == DC - 1), skip_group_check=True),
                         reads=[bxs, bconst], writes=[bpb[b]])
                xs, bxs = mix(5)
                for dc in range(DC):
                    k.op("pe", lambda: nc.tensor.matmul(out=pb[b][:, 128:256], lhsT=wl1[:, dc, 128:256], rhs=xs[:, dc, :],
                                                        start=(dc == 0), stop=(dc == DC - 1), skip_group_check=True),
                         reads=[bxs, bconst], writes=[bpb[b]])
                for dc in range(DC):
                    k.op("pe", lambda: nc.tensor.matmul(out=pb[b][0:32, 256:384], lhsT=wl1[:, dc, 256:288], rhs=xs[:, dc, :],
                                                        start=False, stop=(dc == DC - 1), skip_group_check=True),
                         reads=[bxs, bconst], writes=[bpb[b]])
                k.op("act", lambda: nc.scalar.activation(out=lT[0:64, 0, :], in_=pb[b][0:64, 0:128], func=AF.Tanh),
                     reads=[bpb[b]], writes=[blT])
                k.op("act", lambda: nc.scalar.copy(out=lT[0:64, 2, :], in_=pb[b][0:64, 384:512]),
                     reads=[bpb[b]], writes=[blT])
                k.op("act", lambda: nc.scalar.activation(out=lT[:, 1, :], in_=pb[b][:, 128:256], func=AF.Sigmoid),
                     reads=[bpb[b]], writes=[blT])
                k.op("act", lambda: nc.scalar.activation(out=g1b[:], in_=pb[b][0:32, 256:384], func=AF.Sigmoid),
                     reads=[bpb[b]], writes=[blT])
                mark(S, "P2b")
                b = pbank()
                k.op("pe", lambda: nc.tensor.matmul(out=pb[b][:], lhsT=lT[0:64, 0, :], rhs=wa2[0:64, :], start=True, stop=False),
                     reads=[blT, bw], writes=[bpb[b]])
                k.op("pe", lambda: nc.tensor.matmul(out=pb[b][:], lhsT=selw[:], rhs=rows[:, :],
                                                    start=False, stop=True), reads=[bconst, bw], writes=[bpb[b]])
                k.op("act", lambda: nc.scalar.activation(out=Fp[SG][:], in_=pb[b][:], func=AF.Sigmoid),
                     reads=[bpb[b]], writes=[bF[SG]])
                b = pbank()
                k.op("pe", lambda: nc.tensor.matmul(out=pb[b][:], lhsT=lT[0:64, 2, :], rhs=a2g_[:], start=True, stop=False),
                     reads=[blT, bw], writes=[bpb[b]])
                k.op("pe", lambda: nc.tensor.matmul(out=pb[b][:], lhsT=sela[:], rhs=rows[:, :],
                                                    start=False, stop=True), reads=[bconst, bw], writes=[bpb[b]])
                k.op("act", lambda: nc.scalar.activation(out=Fp[A_][:], in_=pb[b][:], func=AF.Sigmoid),
                     reads=[bpb[b]], writes=[bF[A_]])
                b = pbank()
                k.op("pe", lambda: nc.tensor.matmul(out=pb[b][:], lhsT=lT[:, 1, :], rhs=g2a[:], start=True, stop=False),
                     reads=[blT, bw], writes=[bpb[b]])
                k.op("pe", lambda: nc.tensor.matmul(out=pb[b][:], lhsT=g1b[:], rhs=g2b[:], start=False, stop=True),
                     reads=[blT, bw], writes=[bpb[b]])
                k.op("act", lambda: nc.scalar.copy(out=Fp[GG][:], in_=pb[b][:]), reads=[bpb[b]], writes=[bF[GG]])
                mark(S, "P2c")
                b = pbank()
                k.op("pe", lambda: nc.tensor.matmul(out=pb[b][:], lhsT=triI[:], rhs=Fp[SG][:], start=True, stop=True),
                     reads=[bconst, bF[SG]], writes=[bpb[b]])
                k.op("act", lambda: nc.scalar.activation(out=Fp[PP][:], in_=pb[b][:], func=AF.Exp),
                     reads=[bpb[b]], writes=[bF[PP]])
                k.op("act", lambda: nc.scalar.activation(out=Fp[PINV][:], in_=pb[b][:], func=AF.Exp, scale=-1.0),
                     reads=[bpb[b]], writes=[bF[PINV]])
                k.op("pool", lambda: nc.gpsimd.tensor_tensor(out=tok4[:, 1, :], in0=Fp[R_][:], in1=Fp[PP][:],
                                                             op=ALU.mult), reads=[bF[R_], bF[PP]], writes=[btok[1]])
                b = pbank()
                k.op("pe", lambda: nc.tensor.matmul(out=pb[b][:], lhsT=triS[:], rhs=Fp[SG][:], start=True, stop=True),
                     reads=[bconst, bF[SG]], writes=[bpb[b]])
                k.op("act", lambda: nc.scalar.activation(out=Fp[PPREV][:], in_=pb[b][:], func=AF.Exp),
                     reads=[bpb[b]], writes=[bF[PPREV]])
                b = pbank()
                for fc in range(FC):
                    k.op("pe", lambda: nc.tensor.matmul(out=pb[b][:, fc:fc + 1],
                                                        lhsT=Fp[SG][:, fc * 128:(fc + 1) * 128], rhs=negc[:],
                                                        start=(fc == 0), stop=True, skip_group_check=True),
                         reads=[bconst, bF[SG]], writes=[bpb[b]])
                k.op("act", lambda: nc.scalar.activation(out=pC[:], in_=pb[b][:, 0:FC], func=AF.Exp),
                     reads=[bpb[b]], writes=[bpC])
                mark(S, "P3")
                kkb = bcv[:, 0, :]
                kab = bcv[:, 1, :]
                rkb = bcv[:, 2, :]
                TB, TC = SG, TA + 0
                k.op("dve", lambda: nc.vector.tensor_tensor(out=Fp[TA][:], in0=Fp[K_][:], in1=kkb, op=ALU.mult),
                     reads=[bF[K_], bw], writes=[bF[TA]])
                k.op("pool", lambda: nc.gpsimd.tensor_tensor(out=Fp[TB][:], in0=Fp[TA][:], in1=Fp[TA][:], op=ALU.mult),
                     reads=[bF[TA]], writes=[bF[TB]])
                k.op("dve", lambda: nc.vector.tensor_reduce(out=small[:, 0:8], in_=v3(Fp[TB][:], NHG), axis=AX.X,
                                                            op=ALU.add), reads=[bF[TB]], writes=[bsmall])
                k.op("dve", lambda: nc.vector.tensor_scalar(out=small[:, 0:8], in0=small[:, 0:8], scalar1=1e-24,
                                                            scalar2=None, op0=ALU.max), reads=[bsmall], writes=[bsmall])
                k.op("act", lambda: nc.scalar.activation(out=small[:, 0:8], in_=small[:, 0:8], func=AF.Sqrt),
                     reads=[bsmall], writes=[bsmall])
                k.op("dve", lambda: nc.vector.reciprocal(out=small[:, 8:16], in_=small[:, 0:8]),
                     reads=[bsmall], writes=[bsmall])
                k.op("dve", lambda: nc.vector.tensor_tensor(out=v3(Fp[TA][:], NHG), in0=v3(Fp[TA][:], NHG),
                                                            in1=bc3(small[:, 8:16], NHG, 64), op=ALU.mult),
                     reads=[bF[TA], bsmall], writes=[bF[TA]])
                k.op("pool", lambda: nc.gpsimd.tensor_tensor(out=tok4[:, 0, :], in0=Fp[TA][:], in1=Fp[PPREV][:],
                                                             op=ALU.mult), reads=[bF[TA], bF[PPREV]], writes=[btok[0]])
                k.op("dve", lambda: nc.vector.scalar_tensor_tensor(out=Fp[TB][:], in0=Fp[TA][:], scalar=-1.0,
                                                                   in1=Fp[A_][:], op0=ALU.mult, op1=ALU.mult),
                     reads=[bF[TA], bF[A_]], writes=[bF[TB]])
                k.op("pool", lambda: nc.gpsimd.tensor_tensor(out=tok4[:, 3, :], in0=Fp[TB][:], in1=Fp[PINV][:],
                                                             op=ALU.mult), reads=[bF[TB], bF[PINV]], writes=[btok[3]])
                k.op("dve", lambda: nc.vector.scalar_tensor_tensor(out=Fp[TB][:], in0=Fp[A_][:], scalar=-1.0,
                                                                   in1=kab, op0=ALU.add, op1=ALU.mult),
                     reads=[bF[A_], bw], writes=[bF[TB]])
                k.op("dve", lambda: nc.vector.scalar_tensor_tensor(out=Fp[TB][:], in0=Fp[TB][:], scalar=1.0,
                                                                   in1=Fp[K_][:], op0=ALU.add, op1=ALU.mult),
                     reads=[bF[TB], bF[K_]], writes=[bF[TB]])
                k.op("pool", lambda: nc.gpsimd.tensor_tensor(out=tok4[:, 2, :], in0=Fp[TB][:], in1=Fp[PINV][:],
                                                             op=ALU.mult), reads=[bF[TB], bF[PINV]], writes=[btok[2]])
                k.op("dve", lambda: nc.vector.tensor_tensor(out=Fp[TA][:], in0=Fp[R_][:], in1=rkb, op=ALU.mult),
                     reads=[bF[R_], bw], writes=[bF[TA]])
                k.op("pool", lambda: nc.gpsimd.tensor_tensor(out=Fp[TA][:], in0=Fp[TA][:], in1=Fp[TB][:], op=ALU.mult),
                     reads=[bF[TA], bF[TB]], writes=[bF[TA]])
                k.op("dve", lambda: nc.vector.tensor_reduce(out=small[:, 16:24], in_=v3(Fp[TA][:], NHG), axis=AX.X,
                                                            op=ALU.add), reads=[bF[TA]], writes=[bsmall])
                mark(S, "P4")
                for j, (src_i, dst_ap) in enumerate(((0, 0), (1, 1))):
                    for fc in range(FC):
                        k.op("pe", lambda: nc.tensor.transpose(
                            out=S.pT[:, (fc * 2 + j) * 128:(fc * 2 + j + 1) * 128],
                            in_=tok4[:, src_i, fc * 128:(fc + 1) * 128], identity=S.ident[:]),
                            reads=[btok[src_i], S.bident], writes=[S.bpT])
                mark(S, "P5a")
                k.op("act", lambda: nc.scalar.copy(out=KRT[:].rearrange("p f j t -> p (f j t)"), in_=S.pT[:]),
                     reads=[S.bpT], writes=[bKRT])
                mark(S, "P5b")
                for j, src_i in enumerate((2, 3)):
                    for fc in range(FC):
                        k.op("pe", lambda: nc.tensor.transpose(
                            out=S.pT[:, (j * FC + fc) * 128:(j * FC + fc + 1) * 128],
                            in_=tok4[:, src_i, fc * 128:(fc + 1) * 128], identity=S.ident[:]),
                            reads=[btok[src_i], S.bident], writes=[S.bpT])
                mark(S, "P5c")
                k.op("act", lambda: nc.scalar.copy(out=KBT[:].rearrange("p j f t -> p (j f t)"), in_=S.pT[:]),
                     reads=[S.bpT], writes=[bKBT])
                mark(S, "P5")
                for h4 in range(2):
                    b = pbank()
                    for hh in range(4):
                        plo, fc = 64 * h4, hh
                        k.op("pe", lambda: nc.tensor.matmul(out=pb[b][:, hh * 128:(hh + 1) * 128],
                                                            lhsT=KRT[plo:plo + 64, fc, 0, :], rhs=KBT[plo:plo + 64, 1, fc, :],
                                                            start=True, stop=True, skip_group_check=True),
                             reads=[bKRT, bKBT], writes=[bpb[b]])
                    mark(S, "P6m")
                    k.op("dve", lambda: nc.vector.tensor_tensor(out=Pm[0][:, h4 * 4:(h4 + 1) * 4, :], in0=v3(pb[b][:], 4),
                                                                in1=maskL[:].unsqueeze(1).to_broadcast([128, 4, 128]),
                                                                op=ALU.mult), reads=[bpb[b], bconst], writes=[bPm[0][h4]])
                mark(S, "P6a")
                for h2 in range(4):
                    b = pbank()
                    for hh in range(2):
                        s_ = h2 * 2 + hh
                        plo, fc = 64 * (s_ // 4), s_ % 4
                        k.op("pe", lambda: nc.tensor.matmul(out=pb[b][:, hh * 256:(hh + 1) * 256],
                                                            lhsT=KBT[plo:plo + 64, 1, fc, :], rhs=KRT[plo:plo + 64, fc, :, :],
                                                            start=True, stop=True, skip_group_check=True),
                             reads=[bKRT, bKBT], writes=[bpb[b]])
                    pv = pb[b][:].rearrange("p (h j t) -> p h j t", h=2, j=2)
                    k.op("dve", lambda: nc.vector.tensor_tensor(out=PTm[0][:, h2 * 2:(h2 + 1) * 2, :], in0=pv[:, :, 0, :],
                                                                in1=maskU[:].unsqueeze(1).to_broadcast([128, 2, 128]),
                                                                op=ALU.mult), reads=[bpb[b], bconst], writes=[bPTm[0][h2 // 2]])
                    k.op("act", lambda: nc.scalar.copy(out=ArbT[:, h2 * 2:(h2 + 1) * 2, :], in_=pv[:, :, 1, :]),
                         reads=[bpb[b]], writes=[bArb])
                mark(S, "P6b")
                for h2 in range(4):
                    b = pbank()
                    for hh in range(2):
                        s_ = h2 * 2 + hh
                        plo, fc = 64 * (s_ // 4), s_ % 4
                        k.op("pe", lambda: nc.tensor.matmul(out=pb[b][:, hh * 256:(hh + 1) * 256],
                                                            lhsT=KBT[plo:plo + 64, 0, fc, :], rhs=KRT[plo:plo + 64, fc, :, :],
                                                            start=True, stop=True, skip_group_check=True),
                             reads=[bKRT, bKBT], writes=[bpb[b]])
                    pv = pb[b][:].rearrange("p (h j t) -> p h j t", h=2, j=2)
                    k.op("dve", lambda: nc.vector.tensor_tensor(out=AkkT[:, h2 * 2:(h2 + 1) * 2, :], in0=pv[:, :, 0, :],
                                                                in1=maskU[:].unsqueeze(1).to_broadcast([128, 2, 128]),
                                                                op=ALU.mult), reads=[bpb[b], bconst], writes=[bAkk])
                    k.op("act", lambda: nc.scalar.copy(out=ArkT[:, h2 * 2:(h2 + 1) * 2, :], in_=pv[:, :, 1, :]),
                         reads=[bpb[b]], writes=[bArk])
                mark(S, "P6c")
                k.op("pool", lambda: nc.gpsimd.tensor_tensor(out=ArbT[:], in0=ArbT[:],
                                                             in1=maskUi[:].unsqueeze(1).to_broadcast([128, NHG, 128]),
                                                             op=ALU.mult), reads=[bArb, bconst], writes=[bArb])
                k.op("pool", lambda: nc.gpsimd.tensor_tensor(out=ArkT[:], in0=ArkT[:],
                                                             in1=maskUi[:].unsqueeze(1).to_broadcast([128, NHG, 128]),
                                                             op=ALU.mult), reads=[bArk, bconst], writes=[bArk])
                mark(S, "P6")
                Dm, DTm, bD_, bDT_ = Pm[1], PTm[1], bPm[1], bPTm[1]
                for h4 in range(2):
                    hs = slice(h4 * 4, (h4 + 1) * 4)
                    k.op("pool", lambda: nc.gpsimd.tensor_tensor(out=Dm[:, hs, :], in0=Pm[0][:, hs, :],
                                                                 in1=lmask[:, 0, 0, :].unsqueeze(1).to_broadcast([128, 4, 128]),
                                                                 op=ALU.mult), reads=[bPm[0][h4], bconst], writes=[bD_[h4]])
                    k.op("pool", lambda: nc.gpsimd.tensor_tensor(out=Dm[:, hs, :], in0=Dm[:, hs, :],
                                                                 in1=S.ident[:].unsqueeze(1).to_broadcast([128, 4, 128]),
                                                                 op=ALU.add), reads=[bD_[h4], S.bident], writes=[bD_[h4]])
                    k.op("pool", lambda: nc.gpsimd.tensor_tensor(out=DTm[:, hs, :], in0=PTm[0][:, hs, :],
                                                                 in1=lmask[:, 0, 1, :].unsqueeze(1).to_broadcast([128, 4, 128]),
                                                                 op=ALU.mult), reads=[bPTm[0][h4], bconst], writes=[bDT_[h4]])
                    k.op("pool", lambda: nc.gpsimd.tensor_tensor(out=DTm[:, hs, :], in0=DTm[:, hs, :],
                                                                 in1=S.ident[:].unsqueeze(1).to_broadcast([128, 4, 128]),
                                                                 op=ALU.add), reads=[bDT_[h4], S.bident], writes=[bDT_[h4]])
                for l in range(2, NLEVS + 1):
                    last = (l == NLEVS)
                    for h4 in range(2):
                        if not last:
                            b1 = pbank()
                            for hh in range(4):
                                h = h4 * 4 + hh
                                k.op("pe", lambda: nc.tensor.matmul(out=pb[b1][:, hh * 128:(hh + 1) * 128],
                                                                    lhsT=PTm[0][:, h, :], rhs=Dm[:, h, :],
                                                                    start=True, stop=True, skip_group_check=True),
                                     reads=[bPTm[0][h4], bD_[h4]], writes=[bpb[b1]])
                            k.op("dve", lambda: nc.vector.tensor_tensor(
                                out=Ym[0][:, h4 * 4:(h4 + 1) * 4, :], in0=v3(pb[b1][:], 4),
                                in1=lmask[:, l - 1, 0, :].unsqueeze(1).to_broadcast([128, 4, 128]), op=ALU.mult),
                                reads=[bpb[b1], bconst], writes=[bYm[0][h4]])
                        b2 = pbank()
                        for hh in range(4):
                            h = h4 * 4 + hh
                            k.op("pe", lambda: nc.tensor.matmul(out=pb[b2][:, hh * 128:(hh + 1) * 128],
                                                                lhsT=Pm[0][:, h, :], rhs=DTm[:, h, :],
                                                                start=True, stop=True, skip_group_check=True),
                                 reads=[bPm[0][h4], bDT_[h4]], writes=[bpb[b2]])
                        k.op("dve", lambda: nc.vector.tensor_tensor(
                            out=Ym[1][:, h4 * 4:(h4 + 1) * 4, :], in0=v3(pb[b2][:], 4),
                            in1=lmask[:, l - 1, 1, :].unsqueeze(1).to_broadcast([128, 4, 128]), op=ALU.mult),
                            reads=[bpb[b2], bconst], writes=[bYm[1][h4]])
                    zb = []
                    for h4 in range(2):
                        if not last:
                            b1 = pbank()
                            for hh in range(4):
                                h = h4 * 4 + hh
                                k.op("pe", lambda: nc.tensor.matmul(out=pb[b1][:, hh * 128:(hh + 1) * 128],
                                                                    lhsT=DTm[:, h, :], rhs=Ym[0][:, h, :],
                                                                    start=True, stop=True, skip_group_check=True),
                                     reads=[bDT_[h4], bYm[0][h4]], writes=[bpb[b1]])
                            zb.append((0, h4, b1))
                        b2 = pbank()
                        for hh in range(4):
                            h = h4 * 4 + hh
                            k.op("pe", lambda: nc.tensor.matmul(out=pb[b2][:, hh * 128:(hh + 1) * 128],
                                                                lhsT=Dm[:, h, :], rhs=Ym[1][:, h, :],
                                                                start=True, stop=True, skip_group_check=True),
                                 reads=[bD_[h4], bYm[1][h4]], writes=[bpb[b2]])
                        zb.append((1, h4, b2))
                        if len(zb) >= 2 or last:
                            for (which, hq, bb) in zb:
                                tgt, btgt = (Dm, bD_[hq]) if which == 0 else (DTm, bDT_[hq])
                                k.op("dve", lambda: nc.vector.tensor_tensor(
                                    out=tgt[:, hq * 4:(hq + 1) * 4, :], in0=v3(pb[bb][:], 4),
                                    in1=tgt[:, hq * 4:(hq + 1) * 4, :], op=ALU.add),
                                    reads=[bpb[bb], btgt], writes=[btgt])
                            zb = []
                mark(S, "P7")
                bZ, bU, bD, bY = 3, 4, 5, 6
                for s_ in range(NHG):
                    plo, fc = 64 * (s_ // 4), s_ % 4
                    h = 2 * fc + s_ // 4
                    k.op("pe", lambda: nc.tensor.matmul(out=pb[bZ][:, h * 64:(h + 1) * 64], lhsT=KRT[plo:plo + 64, fc, 0, :],
                                                        rhs=Hb[plo:plo + 64, fc * 64:(fc + 1) * 64],
                                                        start=(s_ == 0), stop=False, skip_group_check=True),
                         reads=[bKRT, bHb], writes=[bpb[bZ]])
                    k.op("pe", lambda: nc.tensor.matmul(out=pb[bZ][:, h * 64:(h + 1) * 64], lhsT=AkkT[:, s_, :],
                                                        rhs=Vb[:, h * 64:(h + 1) * 64],
                                                        start=False, stop=True, skip_group_check=True),
                         reads=[bAkk, bVb], writes=[bpb[bZ]])
                k.op("act", lambda: nc.scalar.copy(out=Zs[:], in_=pb[bZ][:]), reads=[bpb[bZ]], writes=[bZs])
                for s_ in range(NHG):
                    h = 2 * (s_ % 4) + s_ // 4
                    k.op("pe", lambda: nc.tensor.matmul(out=pb[bU][:, h * 64:(h + 1) * 64], lhsT=QT[:, s_, :],
                                                        rhs=Zs[:, h * 64:(h + 1) * 64],
                                                        start=(s_ == 0), stop=True, skip_group_check=True),
                         reads=[bPTm[1][0], bPTm[1][1], bZs], writes=[bpb[bU]])
                k.op("act", lambda: nc.scalar.copy(out=Us[:], in_=pb[bU][:]), reads=[bpb[bU]], writes=[bUs])
                for s_ in range(NHG):
                    plo, fc = 64 * (s_ // 4), s_ % 4
                    h = 2 * fc + s_ // 4
                    k.op("pe", lambda: nc.tensor.matmul(out=pb[bY][:, h * 64:(h + 1) * 64], lhsT=KRT[plo:plo + 64, fc, 1, :],
                                                        rhs=Hb[plo:plo + 64, fc * 64:(fc + 1) * 64],
                                                        start=(s_ == 0), stop=False, skip_group_check=True),
                         reads=[bKRT, bHb], writes=[bpb[bY]])
                    k.op("pe", lambda: nc.tensor.matmul(out=pb[bY][:, h * 64:(h + 1) * 64], lhsT=ArkT[:, s_, :],
                                                        rhs=Vb[:, h * 64:(h + 1) * 64],
                                                        start=False, stop=False, skip_group_check=True),
                         reads=[bArk, bVb], writes=[bpb[bY]])
                    k.op("pe", lambda: nc.tensor.matmul(out=pb[bY][:, h * 64:(h + 1) * 64], lhsT=ArbT[:, s_, :],
                                                        rhs=Us[:, h * 64:(h + 1) * 64],
                                                        start=False, stop=True, skip_group_check=True),
                         reads=[bArb, bUs], writes=[bpb[bY]])
                for s_ in range(NHG):
                    plo, fc = 64 * (s_ // 4), s_ % 4
                    h = 2 * fc + s_ // 4
                    bDD = bD if s_ < 4 else bZ
                    k.op("pe", lambda: nc.tensor.matmul(out=pb[bDD][plo:plo + 64, fc * 64:(fc + 1) * 64],
                                                        lhsT=tok4[:, 2, h * 64:(h + 1) * 64], rhs=Vb[:, h * 64:(h + 1) * 64],
                                                        start=(s_ % 4 == 0), stop=False, skip_group_check=True),
                         reads=[btok[2], bVb], writes=[bpb[bDD]])
                    k.op("pe", lambda: nc.tensor.matmul(out=pb[bDD][plo:plo + 64, fc * 64:(fc + 1) * 64],
                                                        lhsT=tok4[:, 3, h * 64:(h + 1) * 64], rhs=Us[:, h * 64:(h + 1) * 64],
                                                        start=False, stop=True, skip_group_check=True),
                         reads=[btok[3], bUs], writes=[bpb[bDD]])
                k.op("dve", lambda: nc.vector.tensor_tensor(out=Hf[0:64, :], in0=pb[bD][0:64, 0:FC * 64], in1=Hf[0:64, :],
                                                            op=ALU.add), reads=[bpb[bD], bHf], writes=[bHf])
                k.op("dve", lambda: nc.vector.tensor_tensor(out=Hf[64:128, :], in0=pb[bZ][64:128, 0:FC * 64],
                                                            in1=Hf[64:128, :], op=ALU.add),
                     reads=[bpb[bZ], bHf], writes=[bHf])
                k.op("dve", lambda: nc.vector.tensor_tensor(out=v3(Hf[:], FC), in0=v3(Hf[:], FC), in1=bc3(pC[:], FC, 64),
                                                            op=ALU.mult), reads=[bHf, bpC], writes=[bHf])
                k.op("act", lambda: nc.scalar.copy(out=Hb[:], in_=Hf[:]), reads=[bHf], writes=[bHb])
                mark(S, "scan")
                YS, YQ = PP, PINV
                k.op("act", lambda: nc.scalar.copy(out=Fp[YS][:], in_=pb[bY][:]), reads=[bpb[bY]], writes=[bF[YS]])
                k.op("dve", lambda: nc.vector.tensor_reduce(out=small[:, 24:32], in_=v3(Fp[YS][:], NHG), axis=AX.X,
                                                            op=ALU.add), reads=[bF[YS]], writes=[bsmall])
                k.op("pool", lambda: nc.gpsimd.tensor_tensor(out=Fp[YQ][:], in0=Fp[YS][:], in1=Fp[YS][:], op=ALU.mult),
                     reads=[bF[YS]], writes=[bF[YQ]])
                k.op("dve", lambda: nc.vector.tensor_reduce(out=small[:, 32:40], in_=v3(Fp[YQ][:], NHG), axis=AX.X,
                                                            op=ALU.add), reads=[bF[YQ]], writes=[bsmall])
                k.op("dve", lambda: nc.vector.tensor_scalar(out=small[:, 24:32], in0=small[:, 24:32], scalar1=1.0 / 64,
                                                            scalar2=None, op0=ALU.mult), reads=[bsmall], writes=[bsmall])
                k.op("dve", lambda: nc.vector.tensor_tensor(out=small[:, 40:48], in0=small[:, 24:32], in1=small[:, 24:32],
                                                            op=ALU.mult), reads=[bsmall], writes=[bsmall])
                k.op("dve", lambda: nc.vector.scalar_tensor_tensor(out=small[:, 32:40], in0=small[:, 32:40], scalar=1.0 / 64,
                                                                   in1=small[:, 40:48], op0=ALU.mult, op1=ALU.subtract),
                     reads=[bsmall], writes=[bsmall])
                k.op("act", lambda: nc.scalar.activation(out=small[:, 32:40], in_=small[:, 32:40], func=AF.Sqrt,
                                                         bias=LNX_EPS, scale=1.0), reads=[bsmall], writes=[bsmall])
                k.op("dve", lambda: nc.vector.reciprocal(out=small[:, 40:48], in_=small[:, 32:40]),
                     reads=[bsmall], writes=[bsmall])
                k.op("dve", lambda: nc.vector.tensor_tensor(out=v3(Fp[YS][:], NHG), in0=v3(Fp[YS][:], NHG),
                                                            in1=bc3(small[:, 24:32], NHG, 64), op=ALU.subtract),
                     reads=[bF[YS], bsmall], writes=[bF[YS]])
                k.op("dve", lambda: nc.vector.tensor_tensor(out=v3(Fp[YS][:], NHG), in0=v3(Fp[YS][:], NHG),
                                                            in1=bc3(small[:, 40:48], NHG, 64), op=ALU.mult),
                     reads=[bF[YS], bsmall], writes=[bF[YS]])
                k.op("pool", lambda: nc.gpsimd.tensor_tensor(out=Fp[YS][:], in0=Fp[YS][:], in1=bcv[:, 3, :],
                                                             op=ALU.mult), reads=[bF[YS], bw], writes=[bF[YS]])
                k.op("pool", lambda: nc.gpsimd.tensor_tensor(out=Fp[YS][:], in0=Fp[YS][:], in1=bcv[:, 4, :],
                                                             op=ALU.add), reads=[bF[YS], bw], writes=[bF[YS]])
                k.op("dve", lambda: nc.vector.tensor_tensor(out=v3(Fp[YQ][:], NHG), in0=v3(Vb[:], NHG),
                                                            in1=bc3(small[:, 16:24], NHG, 64), op=ALU.mult),
                     reads=[bVb, bsmall], writes=[bF[YQ]])
                k.op("pool", lambda: nc.gpsimd.tensor_tensor(out=Fp[YS][:], in0=Fp[YS][:], in1=Fp[YQ][:], op=ALU.add),
                     reads=[bF[YS], bF[YQ]], writes=[bF[YS]])
                if hg < G - 1:
                    k.op("dve", lambda: nc.vector.tensor_tensor(out=yo0[:, n, :], in0=Fp[YS][:], in1=Fp[GG][:], op=ALU.mult),
                         reads=[bF[YS], bF[GG]], writes=[byo0[n]])
                else:
                    k.op("dve", lambda: nc.vector.tensor_tensor(out=yo[:], in0=Fp[YS][:], in1=Fp[GG][:], op=ALU.mult),
                         reads=[bF[YS], bF[GG]], writes=[byo])
                    for fc in range(FC):
                        k.op("pe", lambda: nc.tensor.transpose(out=S.pT[:, fc * 128:(fc + 1) * 128],
                                                               in_=yo0[:, n, fc * 128:(fc + 1) * 128], identity=S.ident[:]),
                             reads=[byo0[n], S.bident], writes=[S.bpT])
                    for fc in range(FC):
                        k.op("pe", lambda: nc.tensor.transpose(out=S.pT[:, (FC + fc) * 128:(FC + fc + 1) * 128],
                                                               in_=yo[:, fc * 128:(fc + 1) * 128], identity=S.ident[:]),
                             reads=[byo, S.bident], writes=[S.bpT])
                    yoT = KBT[:].rearrange("p j f t -> p (j f) t")
                    byoT = bKBT
                    k.op("act", lambda: nc.scalar.copy(out=KBT[:].rearrange("p j f t -> p (j f t)"), in_=S.pT[:]),
                         reads=[S.bpT], writes=[byoT])
                    for half in range(2):
                        b = pbank()
                        for c in range(DC):
                            k.op("pe", lambda: nc.tensor.matmul(out=pb[b][:], lhsT=yoT[:, c, :],
                                                                rhs=wo[:, c, half * 512:(half + 1) * 512],
                                                                start=(c == 0), stop=(c == DC - 1)),
                                 reads=[byoT, bwo], writes=[bpb[b]])
                        xsl = S.X[:, n, half * 512:(half + 1) * 512]
                        k.op("dve", lambda: nc.vector.tensor_tensor(out=xsl, in0=pb[b][:], in1=xsl, op=ALU.add),
                             reads=[bpb[b], S.bX[n]], writes=[S.bX[n]])
        k.barrier()


LAST_K = None
_NC_CACHE = {}


def kernel(**inputs):
    x = np.asarray(inputs["x"], dtype=np.float32)
    B, L, _ = x.shape
    if L not in _NC_CACHE:
        _NC_CACHE[L] = build(L)
    nc = _NC_CACHE[L]
    params = {n: np.ascontiguousarray(np.asarray(inputs[n], dtype=np.float32))
              for n in PARAM_SHAPES}
    in_maps = []
    for b in range(B):
        m = {"x": np.ascontiguousarray(x[b])}
        m.update(params)
        in_maps.append(m)
    res = run_bass_kernel_spmd(nc, in_maps, core_ids=list(range(B)))
    return np.stack([res.results[b]["out"] for b in range(B)], axis=0)
```
